# Optimizing a Trainium2 kernel written in Bass

```python
import math
import jax, jax.numpy as jnp
from jax import lax
import numpy as np

D_MODEL = 1024
BATCH = 8
SEQ = 8192
DEPTH = 2

HEAD_DIM = 64
ROT_DIM = HEAD_DIM // 4
ROPE_THETA = 500000.0
MAX_POS_OFFSET = 4096

DN_HEADS = 4
DN_HEAD_DIM = 128
DN_CHUNK = 64
DN_CONV = 5
DN_WIDTH = DN_HEADS * DN_HEAD_DIM

DIL_PAIRS = ((128, 1), (512, 4), (2048, 16))
DIL_HEADS_PER_GROUP = 4
DIL_HEADS = DIL_HEADS_PER_GROUP * len(DIL_PAIRS)
DIL_WIDTH = DIL_HEADS * HEAD_DIM

SWA_Q_HEADS = 16
SWA_KV_HEADS = 4
SWA_WINDOW = 128

N_EXPERTS = 16
EC_FACTOR = 2
EXPERT_FF = 1024

AB_IN = 4 * DN_WIDTH + 4 * DN_HEADS + 3 * DIL_WIDTH
AB_OUT = DN_WIDTH + DIL_HEADS_PER_GROUP * HEAD_DIM
C_OUT = SWA_Q_HEADS * HEAD_DIM
C_IN = C_OUT + 2 * SWA_KV_HEADS * HEAD_DIM
N_EVEN = (DEPTH + 1) // 2
N_ODD = DEPTH // 2

ALPHA = (2.0 * DEPTH) ** 0.25
BETA = (8.0 * DEPTH) ** -0.25
LN_EPS = 1e-5
NORM_EPS = 1e-6
NEG = -1e30

kernel_name = 'hybrid_deltanet_dilated_swa_ec_moe'


def layer_norm(x, g, b):
    xf = x.astype(jnp.float32)
    mu = xf.mean(-1, keepdims=True)
    var = jnp.square(xf - mu).mean(-1, keepdims=True)
    return ((xf - mu) * lax.rsqrt(var + LN_EPS) * g + b).astype(x.dtype)


def l2_normalize(x):
    xf = x.astype(jnp.float32)
    return xf * lax.rsqrt(jnp.sum(xf * xf, -1, keepdims=True) + NORM_EPS)


def rope_tables(positions):
    inv_freq = jnp.power(ROPE_THETA, -jnp.arange(0, ROT_DIM, 2, dtype=jnp.float32) / ROT_DIM)
    ang = positions.astype(jnp.float32)[..., None] * inv_freq
    return jnp.cos(ang)[:, :, None, :], jnp.sin(ang)[:, :, None, :]


def partial_rope(x, cos, sin):
    half = ROT_DIM // 2
    x1, x2, rest = x[..., :half], x[..., half:ROT_DIM], x[..., ROT_DIM:]
    cos, sin = cos.astype(x.dtype), sin.astype(x.dtype)
    return jnp.concatenate([x1 * cos - x2 * sin, x2 * cos + x1 * sin, rest], axis=-1)


def centred_depthwise_conv(x, w):
    pad = (w.shape[0] - 1) // 2
    return lax.conv_general_dilated(x, w[:, None, :].astype(x.dtype), window_strides=(1,),
                                    padding=[(pad, pad)], dimension_numbers=('NWC', 'WIO', 'NWC'),
                                    feature_group_count=x.shape[-1])


def gated_delta_rule(q, k, v, g, beta):
    Bx, S, H, dk = q.shape
    dv = v.shape[-1]
    C = DN_CHUNK
    N = S // C

    def chunks(a):
        return jnp.moveaxis(a.reshape(Bx, N, C, H, *a.shape[3:]), 3, 2)

    qc, kc, vc = chunks(q), chunks(k), chunks(v)
    gc = jnp.cumsum(chunks(g), axis=-1)
    bc = chunks(beta)
    causal = jnp.tril(jnp.ones((C, C), dtype=bool))
    strict = jnp.tril(jnp.ones((C, C), dtype=bool), -1)
    diff = gc[..., :, None] - gc[..., None, :]
    decay = jnp.where(causal, jnp.exp(jnp.where(causal, diff, 0.0)), 0.0)
    kb = kc * bc[..., None]
    lower = jnp.where(strict, jnp.einsum('bnhik,bnhjk->bnhij', kb, kc) * decay, 0.0)
    eye = jnp.eye(C, dtype=q.dtype)
    rhs = jnp.concatenate([vc * bc[..., None], kb * jnp.exp(gc)[..., None]], axis=-1)
    sol = lax.linalg.triangular_solve(eye + lower, rhs, left_side=True, lower=True, unit_diagonal=True)
    u, w = sol[..., :dv], sol[..., dv:]
    intra = jnp.einsum('bnhik,bnhjk->bnhij', qc, kc) * decay
    q_dec = qc * jnp.exp(gc)[..., None]
    g_last = gc[..., -1]
    k_dec = kc * jnp.exp(g_last[..., None] - gc)[..., None]

    def step(state, xs):
        q_i, k_i, u_i, w_i, a_i, gl_i = xs
        v_new = u_i - jnp.einsum('bhck,bhkv->bhcv', w_i, state)
        o_i = jnp.einsum('bhck,bhkv->bhcv', q_i, state) + jnp.einsum('bhij,bhjv->bhiv', a_i, v_new)
        state = state * jnp.exp(gl_i)[..., None, None] + jnp.einsum('bhck,bhcv->bhkv', k_i, v_new)
        return state, o_i

    xs = tuple(jnp.moveaxis(a, 1, 0) for a in (q_dec, k_dec, u, w, intra, g_last))
    state0 = jnp.zeros((Bx, H, dk, dv), q.dtype)
    _, o = lax.scan(step, state0, xs)
    return jnp.moveaxis(o, 0, 1).swapaxes(2, 3).reshape(Bx, S, H, dv)


def banded_attention(q, k, v, key_valid, half_window, sink=None):
    Bx, T, Hq, dh = q.shape
    Hkv = k.shape[2]
    G = Hq // Hkv
    blk = half_window
    nb = T // blk

    def neighbours(a):
        ab = a.reshape(Bx, nb, blk, *a.shape[2:])
        ap = jnp.pad(ab, [(0, 0), (1, 1)] + [(0, 0)] * (ab.ndim - 2))
        return jnp.concatenate([ap[:, :-2], ap[:, 1:-1], ap[:, 2:]], axis=2)

    kw, vw, valid_w = neighbours(k), neighbours(v), neighbours(key_valid)
    qb = q.reshape(Bx, nb, blk, Hkv, G, dh)
    s = jnp.einsum('bnqhgd,bnkhd->bnhgqk', qb, kw, preferred_element_type=jnp.float32) * (dh ** -0.5)
    rel = jnp.arange(3 * blk)[None, :] - blk - jnp.arange(blk)[:, None]
    mask = (jnp.abs(rel) <= half_window)[None, None, None, None] & valid_w[:, :, None, None, None, :]
    s = jnp.where(mask, s, NEG)
    m = jnp.max(s, axis=-1, keepdims=True)
    if sink is not None:
        sk = sink.astype(jnp.float32).reshape(Hkv, G)[None, None, :, :, None, None]
        m = jnp.maximum(m, sk)
    p = jnp.exp(s - m)
    denom = jnp.sum(p, axis=-1, keepdims=True)
    if sink is not None:
        denom = denom + jnp.exp(sk - m)
    o = jnp.einsum('bnhgqk,bnkhd->bnqhgd', (p / denom).astype(v.dtype), vw)
    lse = (m + jnp.log(denom))[..., 0]
    lse = jnp.transpose(lse, (0, 1, 4, 2, 3)).reshape(Bx, T, Hq)
    return o.reshape(Bx, T, Hq, dh), lse


def dilated_window_attention(q, k, v, window, dil):
    Bx, S, H, dh = q.shape
    steps = window // (2 * dil)
    T = S // dil
    Tp = -(-T // steps) * steps

    def to_strided(a):
        a = jnp.moveaxis(a.reshape(Bx, T, dil, *a.shape[2:]), 2, 1).reshape(Bx * dil, T, *a.shape[2:])
        return jnp.pad(a, [(0, 0), (0, Tp - T)] + [(0, 0)] * (a.ndim - 2))

    def from_strided(a):
        a = a[:, :T].reshape(Bx, dil, T, *a.shape[2:])
        return jnp.moveaxis(a, 1, 2).reshape(Bx, S, *a.shape[3:])

    valid = jnp.broadcast_to(jnp.arange(Tp) < T, (Bx * dil, Tp))
    o, lse = banded_attention(to_strided(q), to_strided(k), to_strided(v), valid, steps)
    return from_strided(o), from_strided(lse)


def mixer_deltanet_dilated(h, cos, sin, w_in, conv_w, a_log, dt_bias, dn_norm, w_out):
    Bx, S, _ = h.shape
    proj = h @ w_in
    qkv_a, z, gates, qkv_b = jnp.split(proj, [3 * DN_WIDTH, 4 * DN_WIDTH, 4 * DN_WIDTH + 4 * DN_HEADS], axis=-1)
    qkv_a = jax.nn.silu(centred_depthwise_conv(qkv_a, conv_w))
    qa, ka, va = (a.reshape(Bx, S, DN_HEADS, DN_HEAD_DIM) for a in jnp.split(qkv_a, 3, axis=-1))
    qa = l2_normalize(qa) * (DN_HEAD_DIM ** -0.5)
    ka = l2_normalize(ka)
    va = va.astype(jnp.float32)
    gates = gates.astype(jnp.float32).reshape(Bx, S, 4, DN_HEADS)
    decay = -jnp.exp(a_log.astype(jnp.float32)) * jax.nn.softplus(gates[:, :, :2] + dt_bias)
    beta = jax.nn.sigmoid(gates[:, :, 2:])
    flip = lambda a: jnp.flip(a, axis=1)
    o_fwd = gated_delta_rule(qa, ka, va, decay[:, :, 0], beta[:, :, 0])
    o_bwd = flip(gated_delta_rule(flip(qa), flip(ka), flip(va), flip(decay[:, :, 1]), flip(beta[:, :, 1])))
    o_dn = o_fwd + o_bwd
    o_dn = o_dn * lax.rsqrt(jnp.mean(o_dn * o_dn, -1, keepdims=True) + NORM_EPS) * dn_norm
    o_dn = o_dn * jax.nn.silu(z.astype(jnp.float32)).reshape(Bx, S, DN_HEADS, DN_HEAD_DIM)
    o_dn = o_dn.astype(h.dtype).reshape(Bx, S, DN_WIDTH)
    qb, kb, vb = (a.reshape(Bx, S, DIL_HEADS, HEAD_DIM) for a in jnp.split(qkv_b, 3, axis=-1))
    qb, kb = partial_rope(qb, cos, sin), partial_rope(kb, cos, sin)
    outs, lses = [], []
    for gi, (window, dil) in enumerate(DIL_PAIRS):
        hs = slice(gi * DIL_HEADS_PER_GROUP, (gi + 1) * DIL_HEADS_PER_GROUP)
        o_g, lse_g = dilated_window_attention(qb[:, :, hs], kb[:, :, hs], vb[:, :, hs], window, dil)
        outs.append(o_g)
        lses.append(lse_g)
    wts = jax.nn.softmax(jnp.stack(lses), axis=0)
    o_dil = jnp.einsum('gbsh,gbshd->bshd', wts.astype(h.dtype), jnp.stack(outs)).reshape(Bx, S, -1)
    return jnp.concatenate([o_dn, o_dil], axis=-1) @ w_out


def mixer_window_gqa_sink(h, cos, sin, w_in, sinks, w_out):
    Bx, S, _ = h.shape
    q, k, v = jnp.split(h @ w_in, [C_OUT, C_OUT + SWA_KV_HEADS * HEAD_DIM], axis=-1)
    q = partial_rope(q.reshape(Bx, S, SWA_Q_HEADS, HEAD_DIM), cos, sin)
    k = partial_rope(k.reshape(Bx, S, SWA_KV_HEADS, HEAD_DIM), cos, sin)
    v = v.reshape(Bx, S, SWA_KV_HEADS, HEAD_DIM)
    o, _ = banded_attention(q, k, v, jnp.ones((Bx, S), dtype=bool), SWA_WINDOW, sinks)
    return o.reshape(Bx, S, C_OUT) @ w_out


def expert_choice_ffn(h, router_w, w_gate, w_up, w_down):
    Bx, S, _ = h.shape
    cap = (EC_FACTOR * S) // N_EXPERTS
    aff = jax.nn.softmax(jnp.einsum('bsd,de->bse', h, router_w, preferred_element_type=jnp.float32), axis=-1)
    gate, idx = lax.top_k(jnp.swapaxes(aff, 1, 2), cap)
    bidx = jnp.arange(Bx)[:, None, None]
    xg = h[bidx, idx]
    hid = jax.nn.silu(jnp.einsum('becd,edf->becf', xg, w_gate)) * jnp.einsum('becd,edf->becf', xg, w_up)
    out = jnp.einsum('becf,efd->becd', hid, w_down) * gate[..., None].astype(h.dtype)
    return jnp.zeros_like(h).at[bidx, idx].add(out)


def setup_inputs(seed: int = 0) -> dict:
    key = jax.random.key(seed)
    ks = iter(jax.random.split(key, 24))

    def normal(shape, scale):
        return jax.random.normal(next(ks), shape, jnp.float32) * scale

    x = normal((BATCH, SEQ, D_MODEL), 1.0)
    c = normal((BATCH, D_MODEL), 1.0)
    offset = jax.random.randint(next(ks), (BATCH, 1), 0, MAX_POS_OFFSET, dtype=jnp.int32)
    positions = offset + jnp.arange(SEQ, dtype=jnp.int32)[None, :]
    ada_w = normal((DEPTH, D_MODEL, 6 * D_MODEL), 0.1 * D_MODEL ** -0.5)
    ada_b = normal((DEPTH, 6 * D_MODEL), 0.01)
    ab_w_in = normal((N_EVEN, D_MODEL, AB_IN), D_MODEL ** -0.5)
    ab_conv_w = normal((N_EVEN, DN_CONV, 3 * DN_WIDTH), DN_CONV ** -0.5)
    ab_a_log = jnp.log(jax.random.uniform(next(ks), (N_EVEN, 2, DN_HEADS), jnp.float32, 1.0, 16.0))
    dt = jnp.exp(jax.random.uniform(next(ks), (N_EVEN, 2, DN_HEADS), jnp.float32, math.log(1e-3), math.log(1e-1)))
    ab_dt_bias = dt + jnp.log(-jnp.expm1(-dt))
    ab_dn_norm = 1.0 + normal((N_EVEN, DN_HEAD_DIM), 0.01)
    ab_w_out = normal((N_EVEN, AB_OUT, D_MODEL), BETA * AB_OUT ** -0.5)
    swa_w_in = normal((N_ODD, D_MODEL, C_IN), D_MODEL ** -0.5)
    swa_sinks = normal((N_ODD, SWA_Q_HEADS), 0.5)
    swa_w_out = normal((N_ODD, C_OUT, D_MODEL), BETA * C_OUT ** -0.5)
    ln_mix_g = 1.0 + normal((DEPTH, D_MODEL), 0.01)
    ln_mix_b = normal((DEPTH, D_MODEL), 0.01)
    router_w = normal((DEPTH, D_MODEL, N_EXPERTS), D_MODEL ** -0.5)
    moe_w_gate = normal((DEPTH, N_EXPERTS, D_MODEL, EXPERT_FF), D_MODEL ** -0.5)
    moe_w_up = normal((DEPTH, N_EXPERTS, D_MODEL, EXPERT_FF), D_MODEL ** -0.5)
    moe_w_down = normal((DEPTH, N_EXPERTS, EXPERT_FF, D_MODEL), BETA * EXPERT_FF ** -0.5)
    ln_ffn_g = 1.0 + normal((DEPTH, D_MODEL), 0.01)
    ln_ffn_b = normal((DEPTH, D_MODEL), 0.01)
    return {'x': x, 'c': c, 'positions': positions, 'ada_w': ada_w, 'ada_b': ada_b,
            'ab_w_in': ab_w_in, 'ab_conv_w': ab_conv_w, 'ab_a_log': ab_a_log, 'ab_dt_bias': ab_dt_bias,
            'ab_dn_norm': ab_dn_norm, 'ab_w_out': ab_w_out, 'swa_w_in': swa_w_in, 'swa_sinks': swa_sinks,
            'swa_w_out': swa_w_out, 'ln_mix_g': ln_mix_g, 'ln_mix_b': ln_mix_b, 'router_w': router_w,
            'moe_w_gate': moe_w_gate, 'moe_w_up': moe_w_up, 'moe_w_down': moe_w_down,
            'ln_ffn_g': ln_ffn_g, 'ln_ffn_b': ln_ffn_b}


def reference(x, c, positions, ada_w, ada_b, ab_w_in, ab_conv_w, ab_a_log, ab_dt_bias, ab_dn_norm,
              ab_w_out, swa_w_in, swa_sinks, swa_w_out, ln_mix_g, ln_mix_b, router_w, moe_w_gate,
              moe_w_up, moe_w_down, ln_ffn_g, ln_ffn_b):
    cos, sin = rope_tables(positions)
    cond = jax.nn.silu(c)
    for i in range(DEPTH):
        mod = cond @ ada_w[i] + ada_b[i]
        sh1, sc1, g1, sh2, sc2, g2 = jnp.split(mod[:, None, :], 6, axis=-1)
        j = i // 2
        h = x * (1 + sc1) + sh1
        if i % 2 == 0:
            y = mixer_deltanet_dilated(h, cos, sin, ab_w_in[j], ab_conv_w[j], ab_a_log[j], ab_dt_bias[j],
                                       ab_dn_norm[j], ab_w_out[j])
        else:
            y = mixer_window_gqa_sink(h, cos, sin, swa_w_in[j], swa_sinks[j], swa_w_out[j])
        x = layer_norm(ALPHA * x + (1 + g1) * y, ln_mix_g[i], ln_mix_b[i])
        h = x * (1 + sc2) + sh2
        y = expert_choice_ffn(h, router_w[i], moe_w_gate[i], moe_w_up[i], moe_w_down[i])
        x = layer_norm(ALPHA * x + (1 + g2) * y, ln_ffn_g[i], ln_ffn_b[i])
    return x
```

```python
import math
import contextlib
import numpy as np
import concourse.bass as bass
import concourse.mybir as mybir
from concourse.bass_utils import run_bass_kernel_spmd

F32 = mybir.dt.float32
BF16 = mybir.dt.bfloat16
I32 = mybir.dt.int32
U32 = mybir.dt.uint32
AF = mybir.ActivationFunctionType
ALU = mybir.AluOpType
AX = mybir.AxisListType

D = 1024
NKC = 8
AB_IN = 4368
ALPHA = 4.0 ** 0.25
LN_EPS = 1e-5
NORM_EPS = 1e-6
BIG = 30000.0
NE = 16


class Buf:
    ALL = []

    def __init__(self, t, name):
        self.t = t
        self.name = name
        self.w = {}
        self.r = {}
        self.regs = {}
        Buf.ALL.append(self)

    def reg(self, key):
        if key not in self.regs:
            self.regs[key] = Buf(self.t, "%s@%s" % (self.name, key))
        return self.regs[key]

    def __getitem__(self, idx):
        return V(self.t[idx], self)


class V:
    def __init__(self, ap, buf):
        self.ap = ap
        self.buf = buf

    def __getitem__(self, idx):
        return V(self.ap[idx], self.buf)

    def rearrange(self, *a, **k):
        return V(self.ap.rearrange(*a, **k), self.buf)

    def bitcast(self, dt):
        return V(self.ap.bitcast(dt), self.buf)

    def broadcast_to(self, *a, **k):
        return V(self.ap.broadcast_to(*a, **k), self.buf)

    def unsqueeze(self, *a, **k):
        return V(self.ap.unsqueeze(*a, **k), self.buf)


WRITE_KEYS = ("out", "accum_out", "ap")


class K:
    NDMA_SLOTS = 8

    def __init__(self, nc):
        self.nc = nc
        Buf.ALL = []
        self.eng = {"pe": nc.tensor, "act": nc.scalar, "dve": nc.vector, "pool": nc.gpsimd, "sp": nc.sync}
        self.sem = {}
        self.cnt = {}
        for e in self.eng:
            self.sem[e] = nc.alloc_semaphore("sem_" + e)
            self.cnt[e] = 0
        self.dsem = {}
        self.dcnt = {}
        for q in ("sp", "act", "pool"):
            self.dsem[q] = [nc.alloc_semaphore("dsem_%s_%d" % (q, i)) for i in range(self.NDMA_SLOTS)]
            self.dcnt[q] = 0
        self.seen = {e: {} for e in self.eng}
        self.n_inst = 0
        self.n_wait = 0
        self.stack = None
        self.uid = 0

    @contextlib.contextmanager
    def scope(self):
        old = self.stack
        with contextlib.ExitStack() as st:
            self.stack = st
            yield
            self.barrier()
        self.stack = old

    def sb(self, name, shape, dt=F32):
        self.uid += 1
        nm = "%s_%d" % (name, self.uid)
        t = self.stack.enter_context(self.nc.sbuf_tensor(nm, list(shape), dt))
        return Buf(t, nm)

    def ps(self, name, shape, dt=F32):
        self.uid += 1
        nm = "%s_%d" % (name, self.uid)
        t = self.stack.enter_context(self.nc.psum_tensor(nm, list(shape), dt))
        return Buf(t, nm)

    def dram(self, name, shape, dt, kind="Internal"):
        t = self.nc.dram_tensor(name, list(shape), dt, kind=kind).ap()
        return Buf(t, name)

    def _semobj(self, key):
        if key[0] == "c":
            return self.sem[key[1]]
        return self.dsem[key[1]][key[2]]

    def _wait(self, e, key, val):
        s = self.seen[e]
        if s.get(key, 0) >= val:
            return
        self.eng[e].wait_ge(self._semobj(key), val)
        self.n_wait += 1
        s[key] = val

    def mark(self, name):
        if not hasattr(self, "marks"):
            self.marks = []
        self.marks.append((name, dict(self.cnt)))

    def barrier(self):
        for e in self.eng:
            for e2 in self.eng:
                if e2 != e and self.cnt[e2] > 0:
                    self._wait(e, ("c", e2), self.cnt[e2])
            for q in self.dsem:
                i = self.dcnt[q]
                for slot in range(self.NDMA_SLOTS):
                    n = (i - slot + self.NDMA_SLOTS - 1) // self.NDMA_SLOTS if i > slot else 0
                    if n > 0:
                        self._wait(e, ("d", q, slot), 16 * n)
        for b in Buf.ALL:
            b.w = {}
            b.r = {}
            b.regs = {}

    def _deps(self, e, reads, writes, is_dma=False):
        need = {}

        def add(key, val):
            if need.get(key, 0) < val:
                need[key] = val

        mykey = ("c", e)
        for b in reads:
            for key, val in b.w.items():
                if key == mykey and e == "pe" and not is_dma:
                    continue
                add(key, val)
        for b in writes:
            for key, val in b.w.items():
                if key == mykey and not is_dma:
                    continue
                add(key, val)
            for key, val in b.r.items():
                if key == mykey and not is_dma:
                    continue
                add(key, val)
        for key, val in need.items():
            self._wait(e, key, val)

    def I(self, e, method, **kw):
        reads, writes = [], []
        extra_r = kw.pop("R", [])
        extra_w = kw.pop("W", [])
        kw2 = {}
        for k_, v in kw.items():
            if isinstance(v, V):
                (writes if k_ in WRITE_KEYS else reads).append(v.buf)
                kw2[k_] = v.ap
            else:
                kw2[k_] = v
        for v in extra_r:
            reads.append(v.buf if isinstance(v, V) else v)
        for v in extra_w:
            writes.append(v.buf if isinstance(v, V) else v)
        self._deps(e, reads, writes)
        ins = getattr(self.eng[e], method)(**kw2)
        self.cnt[e] += 1
        ins.then_inc(self.sem[e], 1)
        key = ("c", e)
        val = self.cnt[e]
        for b in writes:
            b.w = {key: val}
            b.r = {}
        for b in reads:
            if b in writes:
                continue
            b.r[key] = val
        self.n_inst += 1
        return ins

    def dma(self, q, out, in_, method="dma_start", R=(), **kw):
        reads = [in_.buf] + [v.buf if isinstance(v, V) else v for v in R]
        writes = [out.buf]
        i = self.dcnt[q]
        slot = i % self.NDMA_SLOTS
        rnd = i // self.NDMA_SLOTS
        key = ("d", q, slot)
        if rnd > 0:
            self._wait(q, key, 16 * rnd)
        self._deps(q, reads, writes, is_dma=True)
        kw2 = {}
        for k_, v in kw.items():
            kw2[k_] = v
        ins = getattr(self.eng[q], method)(out=out.ap, in_=in_.ap, **kw2)
        ins.then_inc(self.dsem[q][slot], 16)
        self.dcnt[q] += 1
        val = 16 * (rnd + 1)
        for b in writes:
            b.w = {key: val}
            b.r = {}
        for b in reads:
            b.r[key] = val
        self.n_inst += 1
        return ins

    def mm(self, out, lhsT, rhs, start=True, stop=True):
        return self.I("pe", "matmul", out=out, lhsT=lhsT, rhs=rhs, start=start, stop=stop)

    def tp(self, out, in_, ident):
        return self.I("pe", "transpose", out=out, in_=in_, identity=ident)

    def cp(self, i, out, in_):
        if i % 2 == 0:
            return self.I("dve", "tensor_copy", out=out, in_=in_)
        return self.I("act", "activation", out=out, in_=in_, func=AF.Copy)


def run_pipelined(n, make_gen, nstages=None):
    live = []
    t_next = 0
    while t_next < n or live:
        if t_next < n:
            live.insert(0, make_gen(t_next))
            t_next += 1
        nxt = []
        for g in live:
            try:
                next(g)
                nxt.append(g)
            except StopIteration:
                pass
        live = nxt


class Prog:
    def __init__(self, S, dbg=None):
        self.S = S
        self.NT = S // 128
        self.dbg = dbg or []
        self.nc = bass.Bass("TRN2", target_bir_lowering=False)
        self.k = K(self.nc)
        self.outs = []

    def declare(self):
        k, S = self.k, self.S
        ein = lambda n, s, d=F32: k.dram(n, s, d, kind="ExternalInput")
        self.x = ein("x", [S, D])
        self.c = ein("c", [128, 8])
        self.pos = ein("pos", [128, self.NT], I32)
        self.ada_w = ein("ada_w", [2, D, 6 * D])
        self.ada_b = ein("ada_b", [2, 6 * D])
        self.ab_w_in = ein("ab_w_in", [D, AB_IN])
        self.ab_conv_w = ein("ab_conv_w", [128, 12, 5])
        self.ab_a_log = ein("ab_a_log", [1, 8])
        self.ab_dt_bias = ein("ab_dt_bias", [1, 8])
        self.ab_dn_norm = ein("ab_dn_norm", [1, 128])
        self.ab_w_out = ein("ab_w_out", [768, D])
        self.swa_w_in = ein("swa_w_in", [D, 1536])
        self.swa_sinks = ein("swa_sinks", [1, 16])
        self.swa_w_out = ein("swa_w_out", [D, D])
        self.ln_mix_g = ein("ln_mix_g", [2, D])
        self.ln_mix_b = ein("ln_mix_b", [2, D])
        self.router_w = ein("router_w", [2, D, NE])
        self.moe_w_gate = ein("moe_w_gate", [2, NE, D, D])
        self.moe_w_up = ein("moe_w_up", [2, NE, D, D])
        self.moe_w_down = ein("moe_w_down", [2, NE, D, D])
        self.ln_ffn_g = ein("ln_ffn_g", [2, D])
        self.ln_ffn_b = ein("ln_ffn_b", [2, D])
        self.out = k.dram("out", [S, D], F32, kind="ExternalOutput")
        sc = lambda n, s, d: k.dram(n, s, d, kind=("ExternalOutput" if n in self.dbg else "Internal"))
        self.QKVA = sc("QKVA", [1536, S + 4], BF16)
        self.Z = sc("Z", [S, 512], BF16)
        self.QBT = sc("QBT", [768, S], BF16)
        self.KBT = sc("KBT", [768, S], BF16)
        self.VB = sc("VB", [S, 768], BF16)
        self.QAT = sc("QAT", [512, S], BF16)
        self.KAT = sc("KAT", [512, S], BF16)
        self.KA = sc("KA", [S, 512], BF16)
        self.VA = sc("VA", [S, 512], BF16)
        self.OF = sc("OF", [S, 512], F32)
        self.OB = sc("OB", [S, 512], F32)
        self.DIL = sc("DIL", [3, S, 260], F32)
        self.X1 = sc("X1", [S, D], F32)
        self.H2 = sc("H2", [S, D], BF16)
        self.Y = sc("Y", [S, D], F32)
        self.X2 = sc("X2", [S, D], F32)
        self.GATES = sc("GATES", [128, self.NT, 16], F32)
        self.AFF = sc("AFF", [128, self.NT, NE], F32)
        self.Q1T = sc("Q1T", [1024, S], BF16)
        self.K1T = sc("K1T", [512, S], BF16)
        self.V1 = sc("V1", [S, 256], BF16)
        self.ATT = sc("ATT", [S, 16 * 65], F32)

    def consts(self):
        k = self.k
        self.ident_f = k.sb("ident_f", [128, 128], F32)
        self.ident_b = k.sb("ident_b", [128, 128], BF16)
        self.ones_f = k.sb("ones_f", [128, 128], F32)
        self.ones_b = k.sb("ones_b", [128, 128], BF16)
        self.zeros_b = k.sb("zeros_b", [128, 512], BF16)
        k.I("pool", "memset", ap=self.ones_f[:], constant=1.0)
        k.I("pool", "memset", ap=self.ones_b[:], constant=1.0)
        k.I("pool", "memset", ap=self.zeros_b[:], constant=0.0)
        k.I("pool", "affine_select", out=self.ident_f[:], in_=self.ones_f[:], pattern=[[-1, 128]],
            compare_op=ALU.is_equal, fill=0.0, base=0, channel_multiplier=1)
        k.I("pool", "tensor_copy", out=self.ident_b[:], in_=self.ident_f[:])
        self.eps_norm = k.sb("eps_norm", [128, 1])
        k.I("pool", "memset", ap=self.eps_norm[:], constant=NORM_EPS)
        self.eps_ln = k.sb("eps_ln", [128, 1])
        self.one_col = k.sb("one_col", [128, 1])
        k.I("pool", "memset", ap=self.one_col[:], constant=1.0)
        k.I("pool", "memset", ap=self.eps_ln[:], constant=LN_EPS)

    def tri(self, name, dt, val_true, val_false, base, cm, step, op):
        k = self.k
        t = k.sb(name, [128, 128], dt)
        k.I("pool", "memset", ap=t[:], constant=val_true)
        k.I("pool", "affine_select", out=t[:], in_=t[:], pattern=[[step, 128]],
            compare_op=op, fill=val_false, base=base, channel_multiplier=cm)
        return t

    def ada(self, layer, mod):
        k = self.k
        with k.scope():
            csb = k.sb("csb", [128, 8])
            crep = k.sb("crep", [128, 8, 128])
            brow = k.sb("brow", [1, 6 * D])
            k.dma("sp", csb[:], self.c[:])
            k.dma("sp", brow[:], self.ada_b[layer:layer + 1, :])
            for kc in range(8):
                k.I("act", "activation", out=crep[:, kc, :], in_=self.ones_f[:], func=AF.Silu,
                    scale=csb[:, kc:kc + 1])
            wst = [k.sb("adaw", [128, 8, 512]) for _ in range(2)]
            pp = [k.ps("adaps", [128, 512]) for _ in range(2)]
            for cg in range(12):
                w = wst[cg % 2]
                k.dma("sp" if cg % 2 == 0 else "act", w[:],
                      self.ada_w[layer, :, cg * 512:(cg + 1) * 512].rearrange("(kc p) n -> p kc n", p=128))
                p = pp[cg % 2]
                for kc in range(8):
                    k.mm(p[:], crep[:, kc, :], w[:, kc, :], start=(kc == 0), stop=False)
                k.mm(p[:], self.ones_f[0:1, :], brow[0:1, cg * 512:(cg + 1) * 512], start=False, stop=True)
                k.cp(cg, mod[:, cg * 512:(cg + 1) * 512], p[:])
            for part in (1, 2, 4, 5):
                k.I("dve", "tensor_scalar", out=mod[:, part * D:(part + 1) * D], in0=mod[:, part * D:(part + 1) * D],
                    scalar1=1.0, scalar2=None, op0=ALU.add)
        k.barrier()

    def rope_tables(self):
        k, NT = self.k, self.NT
        self.sin_t = k.sb("sin_t", [128, NT, 8])
        self.cos_t = k.sb("cos_t", [128, NT, 8])
        with k.scope():
            pi = k.sb("posi", [128, NT], I32)
            pf = k.sb("posf", [128, NT])
            ang = k.sb("ang", [128, NT, 8])
            t1 = k.sb("rt1", [128, NT, 8])
            ki = k.sb("rki", [128, NT, 8], I32)
            kf = k.sb("rkf", [128, NT, 8])
            r = k.sb("rr", [128, NT, 8])
            m = k.sb("rm", [128, NT, 8])
            k.dma("sp", pi[:], self.pos[:])
            k.I("dve", "tensor_copy", out=pf[:], in_=pi[:])
            for f in range(8):
                inv = float(np.float32(500000.0) ** np.float32(-(2.0 * f) / 16.0))
                k.I("dve", "tensor_scalar", out=ang[:, :, f], in0=pf[:], scalar1=inv, scalar2=None, op0=ALU.mult)
            TWO_PI = 2.0 * math.pi
            C1 = 6.28125
            C2 = TWO_PI - C1
            k.I("dve", "tensor_scalar", out=t1[:], in0=ang[:], scalar1=1.0 / TWO_PI, scalar2=None, op0=ALU.mult)
            k.I("dve", "tensor_copy", out=ki[:], in_=t1[:])
            k.I("dve", "tensor_copy", out=kf[:], in_=ki[:])
            k.I("dve", "scalar_tensor_tensor", out=r[:], in0=kf[:], scalar=-C1, in1=ang[:], op0=ALU.mult, op1=ALU.add)
            k.I("dve", "scalar_tensor_tensor", out=r[:], in0=kf[:], scalar=-C2, in1=r[:], op0=ALU.mult, op1=ALU.add)

            def fold(rr):
                k.I("dve", "tensor_scalar", out=m[:], in0=rr[:], scalar1=math.pi, scalar2=-TWO_PI, op0=ALU.is_gt, op1=ALU.mult)
                k.I("dve", "tensor_tensor", out=rr[:], in0=rr[:], in1=m[:], op=ALU.add)
                k.I("dve", "tensor_scalar", out=m[:], in0=rr[:], scalar1=-math.pi, scalar2=TWO_PI, op0=ALU.is_lt, op1=ALU.mult)
                k.I("dve", "tensor_tensor", out=rr[:], in0=rr[:], in1=m[:], op=ALU.add)

            fold(r)
            k.I("act", "activation", out=self.sin_t[:], in_=r[:], func=AF.Sin)
            k.I("dve", "tensor_scalar", out=r[:], in0=r[:], scalar1=math.pi / 2, scalar2=None, op0=ALU.add)
            fold(r)
            k.I("act", "activation", out=self.cos_t[:], in_=r[:], func=AF.Sin)
        k.barrier()

    def rope(self, qb, nheads, t, rt=None):
        k = self.k
        v = qb[:, 0:nheads * 64].rearrange("p (h d) -> p h d", d=64)
        x1 = v[:, :, 0:8]
        x2 = v[:, :, 8:16]
        cosb = self.cos_t[:, t, :].unsqueeze(1).broadcast_to([128, nheads, 8])
        sinb = self.sin_t[:, t, :].unsqueeze(1).broadcast_to([128, nheads, 8])
        rt = self.rtmp if rt is None else rt
        ta, tb, tc, td = [rt[i][:, 0:nheads, :] for i in range(4)]
        k.I("dve", "tensor_tensor", out=ta, in0=x1, in1=cosb, op=ALU.mult)
        k.I("pool", "tensor_tensor", out=tb, in0=x2, in1=sinb, op=ALU.mult)
        k.I("dve", "tensor_tensor", out=tc, in0=x2, in1=cosb, op=ALU.mult)
        k.I("pool", "tensor_tensor", out=td, in0=x1, in1=sinb, op=ALU.mult)
        k.I("dve", "tensor_tensor", out=x1, in0=ta, in1=tb, op=ALU.subtract)
        k.I("pool", "tensor_tensor", out=x2, in0=tc, in1=td, op=ALU.add)

    def load_w(self, dst, src, ncols, nkc=8, chunk=1024):
        k = self.k
        with k.scope():
            st = [k.sb("wstage", [128, chunk]) for _ in range(3)]
            i = 0
            for kc in range(nkc):
                for c0 in range(0, ncols, chunk):
                    cw = min(chunk, ncols - c0)
                    s = st[i % 3]
                    k.dma("sp" if i % 2 == 0 else "act", s[:, 0:cw], src[kc * 128:(kc + 1) * 128, c0:c0 + cw])
                    if i % 3 == 2:
                        k.I("pool", "tensor_copy", out=dst[:, kc, c0:c0 + cw], in_=s[:, 0:cw])
                    else:
                        k.cp(i, dst[:, kc, c0:c0 + cw], s[:, 0:cw])
                    i += 1

    def p1_proj0(self, mod):
        k, S, NT = self.k, self.S, self.NT
        MT = 2
        MW = MT * 128
        with k.scope():
            w = k.sb("w_in0", [128, 8, AB_IN], BF16)
            self.load_w(w, self.ab_w_in, AB_IN)
            xs = [k.sb("xs", [128, MT, D]) for _ in range(2)]
            hb = k.sb("hb", [128, MT, D], BF16)
            hTs = [k.sb("hT", [128, 8, MW], BF16) for _ in range(2)]
            qa = k.sb("qa_st", [128, 12, MW], BF16)
            zs = [k.sb("zs", [128, 512], BF16) for _ in range(2)]
            gsb = k.sb("gsb", [128, NT, 16])
            qb = [k.sb("qb", [128, 2304]) for _ in range(3)]
            qbb = [k.sb("qbb", [128, 2304], BF16) for _ in range(2)]
            qkT = [k.sb("qkT", [128, 12, 128], BF16) for _ in range(2)]
            rts = [[k.sb("rtmp", [128, 24, 8]) for _ in range(4)] for _ in range(2)]
            pst = [k.ps("pst", [128, 512], BF16) for _ in range(2)]
            psm = [k.ps("psm", [128, 512]) for _ in range(4)]
            for cc in range(12):
                k.dma("pool", self.QKVA.reg(("padl", cc))[cc * 128:(cc + 1) * 128, 0:2], self.zeros_b[:, 0:2])
                k.dma("pool", self.QKVA.reg(("padr", cc))[cc * 128:(cc + 1) * 128, S + 2:S + 4], self.zeros_b[:, 0:2])
            self._ei = 0
            self._pi = 0

            def nextp():
                p = psm[self._pi % 4]; self._pi += 1
                return p

            def cpy(out, in_):
                k.cp(self._ei, out, in_); self._ei += 1

            def tile(t):
                m, j = t // MT, t % MT
                hT = hTs[m % 2]
                if j == 0:
                    x_ = xs[m % 2]
                    k.dma("sp", x_[:], self.x[m * MW:(m + 1) * MW, :].rearrange("(j p) d -> p j d", p=128))
                    for jj in range(MT):
                        k.I("dve", "tensor_tensor", out=x_[:, jj, :], in0=x_[:, jj, :], in1=mod[:, D:2 * D], op=ALU.mult)
                        k.I("pool", "tensor_tensor", out=hb[:, jj, :], in0=x_[:, jj, :], in1=mod[:, 0:D], op=ALU.add)
                yield
                if j == 0:
                    for kc in range(0, 8, 2):
                        p = pst[(kc // 2) % 2]
                        for k2 in range(2):
                            for jj in range(MT):
                                k.tp(p[:, (k2 * MT + jj) * 128:(k2 * MT + jj + 1) * 128], hb[:, jj, (kc + k2) * 128:(kc + k2 + 1) * 128], self.ident_b[:])
                        cpy(hT[:, kc:kc + 2, :], p[:, 0:2 * MW].rearrange("p (a b) -> p a b", b=MW))
                yield
                if j == 0:
                    for cc in range(12):
                        p = nextp()
                        for kc in range(8):
                            k.mm(p[:, 0:MW], w[:, kc, cc * 128:(cc + 1) * 128], hT[:, kc, :], start=(kc == 0), stop=(kc == 7))
                        cpy(qa[:, cc, :], p[:, 0:MW])
                    k.dma("act", V(self.QKVA.t.rearrange("(cc p) t -> p cc t", p=128)[:, :, 2 + m * MW:2 + (m + 1) * MW],
                                   self.QKVA.reg(("m", m))), qa[:])
                p = nextp()
                for kc in range(8):
                    k.mm(p[:], hT[:, kc, j * 128:(j + 1) * 128], w[:, kc, 1536:2048], start=(kc == 0), stop=(kc == 7))
                z_ = zs[t % 2]
                k.I("act", "activation", out=z_[:], in_=p[:], func=AF.Silu)
                k.dma("sp", self.Z.reg(t)[t * 128:(t + 1) * 128, :], z_[:])
                p = nextp()
                for kc in range(8):
                    k.mm(p[:, 0:16], hT[:, kc, j * 128:(j + 1) * 128], w[:, kc, 2048:2064], start=(kc == 0), stop=(kc == 7))
                k.I("dve", "tensor_copy", out=gsb[:, t, :], in_=p[:, 0:16])
                q_ = qb[t % 3]
                for g in range(5):
                    c0 = 2064 + g * 512
                    cw = min(512, AB_IN - c0)
                    p = nextp()
                    for kc in range(8):
                        k.mm(p[:, 0:cw], hT[:, kc, j * 128:(j + 1) * 128], w[:, kc, c0:c0 + cw], start=(kc == 0), stop=(kc == 7))
                    cpy(q_[:, g * 512:g * 512 + cw], p[:, 0:cw])
                yield
                self.rope(q_, 24, t, rts[t % 2])
                yield
                qq = qbb[t % 2]
                k.I("act", "activation", out=qq[:], in_=q_[:], func=AF.Copy)
                k.dma("sp", self.VB.reg(t)[t * 128:(t + 1) * 128, :], qq[:, 1536:2304])
                yield
                qt = qkT[t % 2]
                for g3 in range(3):
                    p = pst[g3 % 2]
                    for i4 in range(4):
                        cc = g3 * 4 + i4
                        k.tp(p[:, i4 * 128:(i4 + 1) * 128], qq[:, cc * 128:(cc + 1) * 128], self.ident_b[:])
                    cpy(qt[:, g3 * 4:(g3 + 1) * 4, :], p[:].rearrange("p (a b) -> p a b", b=128))
                k.dma("act", V(self.QBT.t.rearrange("(cc p) t -> p cc t", p=128)[:, :, t * 128:(t + 1) * 128], self.QBT.reg(t)),
                      qt[:, 0:6, :])
                k.dma("act", V(self.KBT.t.rearrange("(cc p) t -> p cc t", p=128)[:, :, t * 128:(t + 1) * 128], self.KBT.reg(t)),
                      qt[:, 6:12, :])
                yield

            run_pipelined(NT, tile)
            k.dma("sp", self.GATES[:], gsb[:])
        k.barrier()

    def p2_dnprep(self):
        k, S = self.k, self.S
        NM = S // 512
        with k.scope():
            cw = k.sb("convw", [128, 12, 5])
            k.dma("sp", cw[:], self.ab_conv_w[:])
            dW = k.sb("dW", [128, 12, 5, 128], BF16)
            for cc in range(12):
                for j in range(5):
                    k.I("dve" if (cc + j) % 2 == 0 else "pool", "tensor_scalar", out=dW[:, cc, j, :], in0=self.ident_f[:],
                        scalar1=cw[:, cc, j:j + 1], scalar2=0.0, op0=ALU.mult, op1=ALU.add)
            NB = 3
            xin = [[k.sb("xin", [128, 516], BF16) for _ in range(4)] for _ in range(NB)]
            sl = [[k.sb("csl", [128, 512]) for _ in range(4)] for _ in range(NB)]
            sq = [[k.sb("csq", [128, 512]) for _ in range(4)] for _ in range(2)]
            rn = [[k.sb("crn", [128, 512]) for _ in range(4)] for _ in range(2)]
            ob = [[k.sb("cob", [128, 512], BF16) for _ in range(4)] for _ in range(NB)]
            tk = [k.sb("ctk", [128, 4, 128], BF16) for _ in range(3)]
            psc = [k.ps("p2c", [128, 512]) for _ in range(4)]
            pss = [k.ps("p2s", [128, 512]) for _ in range(2)]
            pst = [k.ps("p2t", [128, 512], BF16) for _ in range(2)]
            self._p2i = 0

            def group(gi):
                m, kind = gi // 3, gi % 3
                b3 = gi % NB; b2 = gi % 2
                for h in range(4):
                    cc = kind * 4 + h
                    xi = xin[b3][h]
                    k.dma("sp" if h % 2 == 0 else "act", xi[:], self.QKVA[cc * 128:(cc + 1) * 128, m * 512:m * 512 + 516])
                    pc = psc[h]
                    for j in range(5):
                        k.mm(pc[:], dW[:, cc, j, :], xi[:, j:j + 512], start=(j == 0), stop=(j == 4))
                    if kind == 2:
                        k.I("act", "activation", out=ob[b3][h][:], in_=pc[:], func=AF.Silu)
                    else:
                        k.I("act", "activation", out=sl[b3][h][:], in_=pc[:], func=AF.Silu)
                        k.I("pool", "tensor_tensor", out=sq[b2][h][:], in0=sl[b3][h][:], in1=sl[b3][h][:], op=ALU.mult)
                yield
                if kind != 2:
                    for h in range(4):
                        p = pss[h % 2]
                        k.mm(p[:], self.ones_f[:], sq[b2][h][:])
                        k.I("act", "activation", out=rn[b2][h][:], in_=p[:], func=AF.Ln, bias=self.eps_norm[:, 0:1])
                    for h in range(4):
                        k.I("act", "activation", out=rn[b2][h][:], in_=rn[b2][h][:], func=AF.Exp, scale=-0.5)
                yield
                for h in range(4):
                    o_ = ob[b3][h]
                    if kind == 0:
                        k.I("dve", "scalar_tensor_tensor", out=o_[:], in0=sl[b3][h][:], scalar=float(128 ** -0.5), in1=rn[b2][h][:],
                            op0=ALU.mult, op1=ALU.mult)
                    elif kind == 1:
                        k.I("dve", "tensor_tensor", out=o_[:], in0=sl[b3][h][:], in1=rn[b2][h][:], op=ALU.mult)
                    if kind != 2:
                        dst = self.QAT if kind == 0 else self.KAT
                        k.dma("act", dst.reg((m, h))[h * 128:(h + 1) * 128, m * 512:(m + 1) * 512], o_[:])
                    if kind >= 1:
                        i = self._p2i; self._p2i += 1
                        p = pst[i % 2]
                        for j in range(4):
                            k.tp(p[:, j * 128:(j + 1) * 128], o_[:, j * 128:(j + 1) * 128], self.ident_b[:])
                        t_ = tk[i % 3]
                        k.I("dve", "tensor_copy", out=t_[:], in_=p[:].rearrange("p (j d) -> p j d", d=128))
                        dst = self.KA if kind == 1 else self.VA
                        k.dma("sp", V(dst.t[m * 512:(m + 1) * 512, h * 128:(h + 1) * 128].rearrange("(j p) d -> p j d", p=128),
                                      dst.reg((m, h))), t_[:])
                yield

            run_pipelined(NM * 3, group, 3)

    def p3_deltanet(self):
        k, S, NT = self.k, self.S, self.NT
        with k.scope():
            UC = [self.tri("UCf", F32, 1.0, 0.0, 0, -1, 1, ALU.is_ge),
                  self.tri("UCb", F32, 1.0, 0.0, 0, 1, -1, ALU.is_ge)]
            NUC = [self.tri("NUCf", F32, -1.0, 0.0, 0, -1, 1, ALU.is_ge),
                   self.tri("NUCb", F32, -1.0, 0.0, 0, 1, -1, ALU.is_ge)]
            NM1 = [self.tri("NM1f", F32, 0.0, -BIG, 0, 1, -1, ALU.is_ge),
                   self.tri("NM1b", F32, 0.0, -BIG, 0, -1, 1, ALU.is_ge)]
            NM2 = [NM1[1], NM1[0]]
            NOTI = self.tri("NOTI", F32, 1.0, 0.0, 0, 1, -1, ALU.not_equal)
            nones_f = k.sb("nones_f", [128, 128])
            k.I("pool", "memset", ap=nones_f[:], constant=-1.0)
            gsb = k.sb("gsb3", [128, NT, 16])
            k.dma("sp", gsb[:], self.GATES[:])
            al = k.sb("alog", [128, 8]); dtb = k.sb("dtb", [128, 8]); nA = k.sb("nA", [128, 8])
            k.dma("sp", al[:], self.ab_a_log[0:1, :].broadcast_to([128, 8]))
            k.dma("sp", dtb[:], self.ab_dt_bias[0:1, :].broadcast_to([128, 8]))
            k.I("act", "activation", out=nA[:], in_=al[:], func=AF.Exp)
            k.I("dve", "tensor_scalar", out=nA[:], in0=nA[:], scalar1=-1.0, scalar2=None, op0=ALU.mult)
            xg = k.sb("xg", [128, NT, 8]); ax = k.sb("axg", [128, NT, 8]); mx = k.sb("mxg", [128, NT, 8])
            gall = k.sb("gall", [128, NT, 8])
            beta = k.sb("beta", [128, NT, 8]); nbeta = k.sb("nbeta", [128, NT, 8])
            k.I("dve", "tensor_tensor", out=xg[:], in0=gsb[:, :, 0:8], in1=dtb[:].unsqueeze(1).broadcast_to([128, NT, 8]), op=ALU.add)
            k.I("act", "activation", out=ax[:], in_=xg[:], func=AF.Abs)
            k.I("act", "activation", out=ax[:], in_=ax[:], func=AF.Exp, scale=-1.0)
            k.I("act", "activation", out=ax[:], in_=ax[:], func=AF.Ln, bias=self.one_col[:, 0:1])
            k.I("dve", "tensor_scalar", out=mx[:], in0=xg[:], scalar1=0.0, scalar2=None, op0=ALU.max)
            k.I("dve", "tensor_tensor", out=mx[:], in0=mx[:], in1=ax[:], op=ALU.add)
            k.I("dve", "tensor_tensor", out=gall[:], in0=mx[:], in1=nA[:].unsqueeze(1).broadcast_to([128, NT, 8]), op=ALU.mult)
            k.I("act", "activation", out=beta[:], in_=gsb[:, :, 8:16], func=AF.Sigmoid)
            k.I("dve", "tensor_scalar", out=nbeta[:], in0=beta[:], scalar1=-1.0, scalar2=None, op0=ALU.mult)
            gd, gc, egc, bw, kd, egl, bet, nbet = [], [], [], [], [], [], [], []
            pz = k.ps("pz", [128, 512])
            for d in range(2):
                g_ = k.sb("gd", [128, NT, 4]); gc_ = k.sb("gc", [128, NT, 4]); e_ = k.sb("egc", [128, NT, 4])
                bw_ = k.sb("bw", [128, NT, 4]); kd_ = k.sb("kd", [128, NT, 4]); gl_ = k.sb("egl", [128, NT, 4])
                b_ = k.sb("bet", [128, NT, 4]); nb_ = k.sb("nbet", [128, NT, 4])
                k.I("dve", "tensor_copy", out=g_[:], in_=gall[:, :, d * 4:(d + 1) * 4])
                k.I("dve", "tensor_copy", out=b_[:], in_=beta[:, :, d * 4:(d + 1) * 4])
                k.I("dve", "tensor_copy", out=nb_[:], in_=nbeta[:, :, d * 4:(d + 1) * 4])
                for c0 in range(0, NT, 128):
                    cw = min(128, NT - c0)
                    gv = g_[:, c0:c0 + cw, :].rearrange("p a b -> p (a b)")
                    k.mm(pz[:, 0:cw * 4], UC[d][:], gv)
                    k.I("dve", "tensor_copy", out=gc_[:, c0:c0 + cw, :].rearrange("p a b -> p (a b)"), in_=pz[:, 0:cw * 4])
                    k.mm(pz[:, 0:cw * 4], self.ones_f[:], gv)
                    k.I("dve", "tensor_copy", out=gl_[:, c0:c0 + cw, :].rearrange("p a b -> p (a b)"), in_=pz[:, 0:cw * 4])
                k.I("act", "activation", out=e_[:], in_=gc_[:], func=AF.Exp)
                k.I("dve", "tensor_tensor", out=bw_[:], in0=e_[:], in1=b_[:], op=ALU.mult)
                k.I("dve", "tensor_tensor", out=kd_[:], in0=gl_[:], in1=gc_[:], op=ALU.subtract)
                k.I("act", "activation", out=kd_[:], in_=kd_[:], func=AF.Exp)
                k.I("act", "activation", out=gl_[:], in_=gl_[:], func=AF.Exp)
                gd.append(g_); gc.append(gc_); egc.append(e_); bw.append(bw_); kd.append(kd_); egl.append(gl_)
                bet.append(b_); nbet.append(nb_)
            def mk(name, shape, dt, n=1):
                return [[k.sb(name, shape, dt) for _ in range(n)] for _ in range(2)]
            kT4 = mk("kT4", [128, 4, 128], BF16, 2); qT4 = mk("qT4", [128, 4, 128], BF16, 3)
            ktok = mk("ktok", [128, 4, 128], BF16, 2); vtok = mk("vtok", [128, 4, 128], BF16, 2)
            GB4 = mk("GB4", [128, 4, 128], F32, 2); UCg4 = mk("UCg4", [128, 4, 128], F32, 2)
            E4 = mk("E4", [128, 4, 128], F32, 2); ET4 = mk("ET4", [128, 4, 128], F32, 2)
            Mk = mk("Mk", [128, 4, 128], BF16, 4); Mtk = mk("Mtk", [128, 4, 128], BF16, 4)
            X4 = mk("X4", [128, 4, 128], BF16, 4)
            rv4 = mk("rv4", [128, 4, 128], BF16, 2); rw4 = mk("rw4", [128, 4, 128], BF16, 2); kdec4 = mk("kdec4", [128, 4, 128], BF16, 3)
            u4 = mk("u4", [128, 4, 128], F32, 3); wT4 = mk("wT4", [128, 4, 128], BF16, 3); AT4 = mk("AT4", [128, 4, 128], BF16, 3)
            vn4 = mk("vn4", [128, 4, 128], BF16)
            S32 = mk("S32", [128, 4, 128], F32); Sb = mk("Sb", [128, 4, 128], BF16)
            o2 = mk("o2", [128, 4, 128], F32); o4 = mk("o4", [128, 4, 128], F32, 2)
            banks = [k.ps("dnps", [128, 512]) for _ in range(7)]
            self._bi = 0

            def bank():
                b = banks[self._bi % len(banks)]
                self._bi += 1
                return b

            def v3(b):
                return b[:].rearrange("p (h f) -> p h f", f=128)

            def v3b(b):
                return b[:].bitcast(BF16)[:, 0:512].rearrange("p (h f) -> p h f", f=128)

            for d in range(2):
                k.I("pool", "memset", ap=S32[d][0][:], constant=0.0)
                k.I("pool", "memset", ap=Sb[d][0][:], constant=0.0)

            def bc_f(t, c):
                return t[:, c, :].unsqueeze(2).broadcast_to([128, 4, 128])

            def gen_prep(s):
                b = s % 3
                b2 = s % 2
                cs = [s, NT - 1 - s]
                for d in range(2):
                    c = cs[d]
                    sl = slice(c * 128, (c + 1) * 128)
                    k.dma("sp", kT4[d][b2][:], V(self.KAT.t.rearrange("(h q) t -> q h t", q=128)[:, :, sl], self.KAT))
                    k.dma("sp", qT4[d][b][:], V(self.QAT.t.rearrange("(h q) t -> q h t", q=128)[:, :, sl], self.QAT))
                    k.dma("act", ktok[d][b2][:].rearrange("p h f -> p (h f)"), self.KA[sl, :])
                    k.dma("act", vtok[d][b2][:].rearrange("p h f -> p (h f)"), self.VA[sl, :])
                yield
                for d in range(2):
                    c = cs[d]
                    k.I("pool", "tensor_tensor", out=GB4[d][b2][:], in0=self.ident_f[:].unsqueeze(1).broadcast_to([128, 4, 128]),
                        in1=bc_f(gc[d], c), op=ALU.mult)
                yield
                pA = [bank(), bank()]
                for d in range(2):
                    for h in range(4):
                        k.mm(v3(pA[d])[:, h, :], self.ones_f[:], GB4[d][b2][:, h, :])
                    k.I("dve", "tensor_tensor", out=UCg4[d][b2][:], in0=v3(pA[d]), in1=bc_f(gc[d], cs[d]), op=ALU.subtract)
                yield
                for d in range(2):
                    k.I("dve", "scalar_tensor_tensor", out=E4[d][b2][:], in0=UCg4[d][b2][:], scalar=-1.0,
                        in1=NM1[d][:].unsqueeze(1).broadcast_to([128, 4, 128]), op0=ALU.mult, op1=ALU.add)
                    k.I("pool", "tensor_tensor", out=ET4[d][b2][:], in0=UCg4[d][b2][:],
                        in1=NM2[d][:].unsqueeze(1).broadcast_to([128, 4, 128]), op=ALU.add)
                yield
                for d in range(2):
                    k.I("act", "activation", out=E4[d][b2][:], in_=E4[d][b2][:], func=AF.Exp)
                    k.I("act", "activation", out=ET4[d][b2][:], in_=ET4[d][b2][:], func=AF.Exp)
                yield
                for d in range(2):
                    c = cs[d]
                    kt = kT4[d][b2]; qt = qT4[d][b]
                    pg = bank()
                    for h in range(4):
                        k.mm(v3(pg)[:, h, :], kt[:, h, :], kt[:, h, :])
                    pa = bank()
                    for h in range(4):
                        k.mm(v3(pa)[:, h, :], kt[:, h, :], qt[:, h, :])
                    k.I("dve", "tensor_tensor", out=AT4[d][b][:], in0=v3(pa), in1=ET4[d][b2][:], op=ALU.mult)
                    k.I("pool", "tensor_tensor", out=E4[d][b2][:], in0=E4[d][b2][:], in1=NOTI[:].unsqueeze(1).broadcast_to([128, 4, 128]), op=ALU.mult)
                    k.I("pool", "tensor_tensor", out=E4[d][b2][:], in0=E4[d][b2][:], in1=bc_f(nbet[d], c), op=ALU.mult)
                    k.I("dve", "tensor_tensor", out=Mk[d][2 * b2][:], in0=v3(pg), in1=E4[d][b2][:], op=ALU.mult)
                    pt = bank()
                    for h in range(4):
                        k.tp(v3b(pt)[:, h, :], Mk[d][2 * b2][:, h, :], self.ident_b[:])
                    k.I("act", "activation", out=Mtk[d][2 * b2][:], in_=v3b(pt), func=AF.Copy)
                    k.I("pool", "tensor_tensor", out=X4[d][2 * b2][:], in0=Mtk[d][2 * b2][:], in1=self.ident_b[:].unsqueeze(1).broadcast_to([128, 4, 128]), op=ALU.add)
                    k.I("pool", "tensor_tensor", out=rv4[d][b2][:], in0=vtok[d][b2][:], in1=bc_f(bet[d], c), op=ALU.mult)
                    k.I("pool", "tensor_tensor", out=rw4[d][b2][:], in0=ktok[d][b2][:], in1=bc_f(bw[d], c), op=ALU.mult)
                    k.I("pool", "tensor_tensor", out=kdec4[d][b][:], in0=ktok[d][b2][:], in1=bc_f(kd[d], c), op=ALU.mult)
                yield
                cur = 0
                for lvl in range(1, 7):
                    nxt = 1 - cur
                    for d in range(2):
                        pm = bank()
                        for h in range(4):
                            k.mm(v3(pm)[:, h, :], Mtk[d][2 * b2 + cur][:, h, :], Mk[d][2 * b2 + cur][:, h, :])
                        k.I("act", "activation", out=Mk[d][2 * b2 + nxt][:], in_=v3(pm), func=AF.Copy)
                        if lvl < 6:
                            pmt = bank()
                            for h in range(4):
                                k.mm(v3(pmt)[:, h, :], Mk[d][2 * b2 + cur][:, h, :], Mtk[d][2 * b2 + cur][:, h, :])
                            k.I("dve", "tensor_copy", out=Mtk[d][2 * b2 + nxt][:], in_=v3(pmt))
                    yield
                    for d in range(2):
                        px = bank()
                        for h in range(4):
                            k.mm(v3(px)[:, h, :], Mk[d][2 * b2 + nxt][:, h, :], X4[d][2 * b2 + cur][:, h, :])
                        k.I("dve", "tensor_tensor", out=X4[d][2 * b2 + nxt][:], in0=v3(px), in1=X4[d][2 * b2 + cur][:], op=ALU.add)
                    cur = nxt
                    yield
                for d in range(2):
                    TT = X4[d][2 * b2 + cur]
                    pu = bank()
                    for h in range(4):
                        k.mm(v3(pu)[:, h, :], TT[:, h, :], rv4[d][b2][:, h, :])
                    k.I("act", "activation", out=u4[d][b][:], in_=v3(pu), func=AF.Copy)
                    pw = bank()
                    for h in range(4):
                        k.mm(v3(pw)[:, h, :], rw4[d][b2][:, h, :], TT[:, h, :])
                    k.I("dve", "tensor_copy", out=wT4[d][b][:], in_=v3(pw))
                yield

            def gen_scan(s):
                b = s % 3
                cs = [s, NT - 1 - s]
                for d in range(2):
                    pws = bank()
                    for h in range(4):
                        k.mm(v3(pws)[:, h, :], wT4[d][b][:, h, :], Sb[d][0][:, h, :])
                    k.I("dve", "tensor_tensor", out=vn4[d][0][:], in0=u4[d][b][:], in1=v3(pws), op=ALU.subtract)
                yield
                for d in range(2):
                    c = cs[d]
                    po1 = bank()
                    for h in range(4):
                        k.mm(v3(po1)[:, h, :], qT4[d][b][:, h, :], Sb[d][0][:, h, :])
                    po2 = bank()
                    for h in range(4):
                        k.mm(v3(po2)[:, h, :], AT4[d][b][:, h, :], vn4[d][0][:, h, :])
                    pds = bank()
                    for h in range(4):
                        k.mm(v3(pds)[:, h, :], kdec4[d][b][:, h, :], vn4[d][0][:, h, :])
                    k.I("pool", "tensor_tensor", out=S32[d][0][:], in0=S32[d][0][:], in1=bc_f(egl[d], c), op=ALU.mult)
                    k.I("dve", "tensor_tensor", out=Sb[d][0][:], in0=S32[d][0][:], in1=v3(pds), op=ALU.add)
                    k.I("dve", "tensor_tensor", out=S32[d][0][:], in0=S32[d][0][:], in1=v3(pds), op=ALU.add)
                    oo = o4[d][s % 2]
                    k.I("act", "activation", out=o2[d][0][:], in_=v3(po2), func=AF.Copy)
                    k.I("dve", "tensor_tensor", out=oo[:], in0=v3(po1), in1=bc_f(egc[d], c), op=ALU.mult)
                    k.I("pool", "tensor_tensor", out=oo[:], in0=oo[:], in1=o2[d][0][:], op=ALU.add)
                    dst = self.OF if d == 0 else self.OB
                    k.dma("sp", dst.reg(c)[c * 128:(c + 1) * 128, :], oo[:].rearrange("p h f -> p (h f)"))
                    yield

            def drive():
                from collections import deque
                preps = deque()
                next_p = 0
                prep_done = -1
                finished = set()
                cur = 0
                gx = None
                rnd = 0
                while cur < NT:
                    while len(preps) < 2 and next_p < NT and next_p - 3 < cur:
                        preps.append((next_p, gen_prep(next_p)))
                        next_p += 1
                    for item in list(preps):
                        sp, g = item
                        try:
                            next(g)
                        except StopIteration:
                            preps.remove(item)
                            finished.add(sp)
                    while (prep_done + 1) in finished:
                        prep_done += 1
                    if gx is None and cur <= prep_done:
                        gx = gen_scan(cur)
                    if gx is not None:
                        try:
                            next(gx)
                        except StopIteration:
                            gx = None
                            cur += 1
                    rnd += 1

            drive()
        k.barrier()


    def attn_masks(self, half):
        k = self.k
        NEGM = -30000.0
        prev = self.tri("mprev", BF16, 0.0, NEGM, -(128 - half), 1, -1, ALU.is_ge)
        nxt = self.tri("mnext", BF16, 0.0, NEGM, -(128 - half), -1, 1, ALU.is_ge)
        own = None
        if half < 128:
            own = self.tri("mown", BF16, 0.0, NEGM, half, 1, -1, ALU.is_ge)
            k.I("pool", "affine_select", out=own[:], in_=own[:], pattern=[[1, 128]], compare_op=ALU.is_ge,
                fill=NEGM, base=half, channel_multiplier=-1)
        return prev, own, nxt

    def attn(self, QT, KT, VD, heads, nqc, nkc, vcols, vc0, dil, masks, OUT, qc0=0, kc0=0):
        k, S = self.k, self.S
        T = S // dil
        NA = T // 128
        W = 128 * dil
        nh = len(heads)
        nvh = vcols // 64
        mprev, mown, mnext = masks
        with k.scope():
            qG = [k.sb("qG", [128, nqc, W], BF16) for _ in range(2)]
            kG = [k.sb("kG", [128, nkc, W], BF16) for _ in range(4)]
            vG = [k.sb("vG", [128, dil, nvh, 65], BF16) for _ in range(4)]
            for v_ in vG:
                k.I("pool", "memset", ap=v_[:], constant=1.0)
            ost = [k.sb("ost", [128, dil, nh * 65]) for _ in range(2)]
            PT = [k.sb("PT", [128, 3, 128], BF16) for _ in range(3)]
            pss = [k.ps("aps", [128, 512]) for _ in range(3)]
            pso = [k.ps("apo", [128, 512]) for _ in range(2)]
            QTv = QT.t.rearrange("(c p) t -> p c t", p=128)
            KTv = KT.t.rearrange("(c p) t -> p c t", p=128)

            def load_kv(a):
                sl = slice(a * W, (a + 1) * W)
                k.dma("sp", kG[a % 4][:], V(KTv[:, kc0:kc0 + nkc, sl], KT))
                k.dma("act", vG[a % 4][:, :, :, 0:64],
                      V(VD.t[sl, vc0:vc0 + vcols].rearrange("(j r) (h d) -> j r h d", r=dil, d=64), VD))

            load_kv(0)
            units = [(a, r, hi) for a in range(NA) for r in range(dil) for hi in range(nh)]

            def unit(u):
                a, r, hi = units[u]
                qc, kc, base, vh = heads[hi]
                sl = slice(a * W, (a + 1) * W)
                q_ = qG[a % 2]
                o_ = ost[a % 2]
                if r == 0 and hi == 0:
                    if a + 1 < NA:
                        load_kv(a + 1)
                    k.dma("sp", q_[:], V(QTv[:, qc0:qc0 + nqc, sl], QT))
                kbs = [kb for kb in (a - 1, a, a + 1) if 0 <= kb < NA]
                nk = len(kbs)
                ps = pss[u % 3]; pt = PT[u % 3]
                po = pso[(u // 4) % 2]
                pv = ps[:, 0:384].rearrange("p (a b) -> p a b", b=128)
                bs = slice(base, base + 64)
                for ki, kb in enumerate(kbs):
                    m_ = mprev if kb == a - 1 else (mown if kb == a else mnext)
                    k.mm(pv[:, ki, :], kG[kb % 4][bs, kc, r:W:dil], q_[bs, qc, r:W:dil], start=True, stop=(m_ is None))
                    if m_ is not None:
                        k.mm(pv[:, ki, :], self.ident_b[:], m_[:], start=False, stop=True)
                yield
                k.I("act", "activation", out=pt[:, 0:nk, :], in_=pv[:, 0:nk, :], func=AF.Exp, scale=0.125)
                yield
                for ki, kb in enumerate(kbs):
                    k.mm(po[:, (hi % 4) * 65:(hi % 4 + 1) * 65], pt[:, ki, :], vG[kb % 4][:, r, vh, :], start=(ki == 0), stop=(ki == nk - 1))
                if hi % 4 == 3:
                    k.I("dve", "tensor_copy", out=o_[:, r, (hi - 3) * 65:(hi + 1) * 65], in_=po[:, 0:260])
                    if r == dil - 1 and hi == nh - 1:
                        k.dma("act", V(OUT.t[sl, :].rearrange("(i r) c -> i r c", r=dil), OUT.reg(a)), o_[:])
                yield

            run_pipelined(len(units), unit)

    def p4_dilated(self):
        with self.k.scope():
            masks = self.attn_masks(64)
            for gi, dil in enumerate((1, 4, 16)):
                heads = [(h // 2, h // 2, (h % 2) * 64, h) for h in range(4)]
                OUT = Buf(self.DIL.t[gi], "DIL%d" % gi)
                self.attn(self.QBT, self.KBT, self.VB, heads, 2, 2, 256, gi * 256, dil, masks, OUT, qc0=gi * 2, kc0=gi * 2)

    def ln_setup(self, g_dram, b_dram, layer):
        k = self.k
        g = k.sb("lng", [128, D]); b = k.sb("lnb", [128, D])
        k.dma("sp", g[:], g_dram[layer:layer + 1, :].broadcast_to([128, D]))
        k.dma("sp", b[:], b_dram[layer:layer + 1, :].broadcast_to([128, D]))
        return g, b

    def ln_stats(self, tin, tmp):
        k = self.k
        st = tmp["st"]; mv = tmp["mv"]; rs = tmp["rs"]; nb = tmp["nb"]
        for hh in range(2):
            k.I("dve", "bn_stats", out=st[:, hh, :], in_=tin[:, hh * 512:(hh + 1) * 512])
        k.I("dve", "bn_aggr", out=mv[:], in_=st[:].rearrange("p a b -> p (a b)"))
        k.I("act", "activation", out=rs[:], in_=mv[:, 1:2], func=AF.Ln, bias=self.eps_ln[:, 0:1])
        k.I("act", "activation", out=rs[:], in_=rs[:], func=AF.Exp, scale=-0.5)
        k.I("dve", "scalar_tensor_tensor", out=nb[:], in0=mv[:, 0:1], scalar=-1.0, in1=rs[:], op0=ALU.mult, op1=ALU.mult)

    def ln_apply(self, tin, out, g, b, tmp):
        k = self.k
        rs = tmp["rs"]; nb = tmp["nb"]
        k.I("act", "activation", out=tin[:], in_=tin[:], func=AF.Identity, scale=rs[:, 0:1], bias=nb[:, 0:1])
        k.I("pool", "tensor_tensor", out=tin[:], in0=tin[:], in1=g[:], op=ALU.mult)
        k.I("dve", "tensor_tensor", out=out, in0=tin[:], in1=b[:], op=ALU.add)

    def ln_tile(self, tin, out, g, b, tmp):
        self.ln_stats(tin, tmp)
        self.ln_apply(tin, out, g, b, tmp)

    def ln_tmp(self):
        k = self.k
        return {"st": k.sb("lnst", [128, 2, 6]), "mv": k.sb("lnmv", [128, 2]), "rs": k.sb("lnrs", [128, 1]), "nb": k.sb("lnnb", [128, 1])}

    def mixer_finish(self, layer, mod, xin, w_out_dram, nrows, build_cat, cat_stages):
        k, S, NT = self.k, self.S, self.NT
        ncc = nrows // 128
        with k.scope():
            w = k.sb("w_out", [128, ncc, D], BF16)
            self.load_w(w, w_out_dram, D, nkc=ncc)
            g, b = self.ln_setup(self.ln_mix_g, self.ln_mix_b, layer)
            NTM = 6
            tmps = [self.ln_tmp() for _ in range(NTM)]
            NCAT = cat_stages + 3
            cat = [k.sb("cat", [128, nrows], BF16) for _ in range(NCAT)]
            catT = [k.sb("catT", [128, ncc, 128], BF16) for _ in range(3)]
            xt = [k.sb("xt5", [128, D]) for _ in range(4)]
            NT1 = 8
            t1 = [k.sb("t15", [128, D]) for _ in range(NT1)]
            h2 = [k.sb("h25", [128, D], BF16) for _ in range(3)]
            h2f = [k.sb("h2f", [128, D]) for _ in range(3)]
            h2T = [k.sb("h2T", [128, 8, 128]) for _ in range(2)]
            rw = k.sb("rw", [128, 8, NE])
            k.dma("sp", rw[:], self.router_w[layer].rearrange("(kc p) e -> p kc e", p=128))
            affs = k.sb("affs", [128, NT, NE])
            lgs = [k.sb("lg", [128, NE]) for _ in range(4)]
            mxs = [k.sb("mxr", [128, 1]) for _ in range(4)]
            sms = [k.sb("smr", [128, 1]) for _ in range(4)]
            pst = [k.ps("p5t", [128, 512], BF16) for _ in range(2)]
            psy = [k.ps("p5y", [128, 512]) for _ in range(4)]
            psr = [k.ps("p5r", [128, 512]) for _ in range(2)]

            def tile(t):
                sl = slice(t * 128, (t + 1) * 128)
                c_ = cat[t % NCAT]
                yield from build_cat(t, c_)
                yield
                cT = catT[t % 3]
                ngrp = (ncc + 3) // 4
                for g0 in range(0, ncc, 4):
                    n = min(4, ncc - g0)
                    p = pst[(g0 // 4) % 2]
                    for i in range(n):
                        k.tp(p[:, i * 128:(i + 1) * 128], c_[:, (g0 + i) * 128:(g0 + i + 1) * 128], self.ident_b[:])
                    k.cp(g0 // 4, cT[:, g0:g0 + n, :], p[:, 0:n * 128].rearrange("p (a b) -> p a b", b=128))
                x_ = xt[t % 4]
                k.dma("sp", x_[:], xin[sl, :])
                yield
                t_ = t1[t % NT1]
                pp = [psy[(t * 2 + hh) % 4] for hh in range(2)]
                for hh in range(2):
                    for cc in range(ncc):
                        k.mm(pp[hh][:], cT[:, cc, :], w[:, cc, hh * 512:(hh + 1) * 512], start=(cc == 0), stop=(cc == ncc - 1))
                yield
                for hh in range(2):
                    k.I("dve", "tensor_tensor", out=t_[:, hh * 512:(hh + 1) * 512], in0=pp[hh][:], in1=mod[:, 2 * D + hh * 512:2 * D + (hh + 1) * 512], op=ALU.mult)
                k.I("dve", "scalar_tensor_tensor", out=t_[:], in0=x_[:], scalar=float(ALPHA), in1=t_[:], op0=ALU.mult, op1=ALU.add)
                tm = tmps[t % NTM]
                st = tm["st"]; mv = tm["mv"]; rs = tm["rs"]; nb = tm["nb"]
                for hh in range(2):
                    k.I("dve", "bn_stats", out=st[:, hh, :], in_=t_[:, hh * 512:(hh + 1) * 512])
                k.I("dve", "bn_aggr", out=mv[:], in_=st[:].rearrange("p a b -> p (a b)"))
                yield
                k.I("act", "activation", out=rs[:], in_=mv[:, 1:2], func=AF.Ln, bias=self.eps_ln[:, 0:1])
                k.I("act", "activation", out=rs[:], in_=rs[:], func=AF.Exp, scale=-0.5)
                yield
                k.I("dve", "scalar_tensor_tensor", out=nb[:], in0=mv[:, 0:1], scalar=-1.0, in1=rs[:], op0=ALU.mult, op1=ALU.mult)
                yield
                k.I("act", "activation", out=t_[:], in_=t_[:], func=AF.Identity, scale=rs[:, 0:1], bias=nb[:, 0:1])
                yield
                k.I("pool", "tensor_tensor", out=t_[:], in0=t_[:], in1=g[:], op=ALU.mult)
                yield
                k.I("dve", "tensor_tensor", out=t_[:], in0=t_[:], in1=b[:], op=ALU.add)
                k.dma("sp", self.X1.reg(t)[sl, :], t_[:])
                yield
                hf = h2f[t % 3]
                k.I("pool", "tensor_tensor", out=hf[:], in0=t_[:], in1=mod[:, 4 * D:5 * D], op=ALU.mult)
                yield
                k.I("dve", "tensor_tensor", out=hf[:], in0=hf[:], in1=mod[:, 3 * D:4 * D], op=ALU.add)
                yield
                k.I("act", "activation", out=h2[t % 3][:], in_=hf[:], func=AF.Copy)
                k.dma("act", self.H2.reg(t)[sl, :], h2[t % 3][:])
                hT_ = h2T[t % 2]
                for g0 in range(2):
                    p = psr[g0]
                    for i in range(4):
                        kc = g0 * 4 + i
                        k.tp(p[:, i * 128:(i + 1) * 128], hf[:, kc * 128:(kc + 1) * 128], self.ident_f[:])
                    k.cp(g0, hT_[:, g0 * 4:(g0 + 1) * 4, :], p[:].rearrange("p (a b) -> p a b", b=128))
                yield
                p = psr[t % 2]
                for kc in range(8):
                    k.mm(p[:, 0:NE], hT_[:, kc, :], rw[:, kc, :], start=(kc == 0), stop=(kc == 7))
                lg = lgs[t % 4]; mxr = mxs[t % 4]; smr = sms[t % 4]
                k.I("dve", "tensor_copy", out=lg[:], in_=p[:, 0:NE])
                k.I("dve", "tensor_reduce", out=mxr[:], in_=lg[:], axis=AX.X, op=ALU.max, negate=True)
                yield
                k.I("act", "activation", out=lg[:], in_=lg[:], func=AF.Exp, bias=mxr[:, 0:1], accum_out=smr[:])
                yield
                k.I("dve", "reciprocal", out=smr[:], in_=smr[:])
                k.I("dve", "tensor_scalar", out=affs[:, t, :], in0=lg[:], scalar1=smr[:, 0:1], scalar2=None, op0=ALU.mult)
                yield

            run_pipelined(NT, tile, cat_stages + 17)
            k.dma("sp", self.AFF[:], affs[:])

    def p5_finish0(self, mod):
        k = self.k
        with k.scope():
            dl = [k.sb("dl", [128, 3, 260]) for _ in range(4)]
            of = [k.sb("of", [128, 512]) for _ in range(7)]
            ob = [k.sb("ob5", [128, 512]) for _ in range(3)]
            zz = [k.sb("zz", [128, 512], BF16) for _ in range(7)]
            sqs = [k.sb("sq5", [128, 512]) for _ in range(3)]
            mss = [k.sb("ms5", [128, 4]) for _ in range(5)]
            rds = [k.sb("rd5", [128, 4]) for _ in range(3)]
            dnn = k.sb("dnn", [128, 128])
            k.dma("sp", dnn[:], self.ab_dn_norm[0:1, :].broadcast_to([128, 128]))

            def build_cat(t, c_):
                sl = slice(t * 128, (t + 1) * 128)
                d_ = dl[t % 4]; f_ = of[t % 7]; b_ = ob[t % 3]; z_ = zz[t % 7]
                sq = sqs[t % 3]; ms = mss[t % 5]; rd = rds[t % 3]
                k.dma("sp", d_[:], V(self.DIL.t[:, sl, :].rearrange("g p c -> p g c"), self.DIL))
                k.dma("act", f_[:], self.OF[sl, :])
                k.dma("act", b_[:], self.OB[sl, :])
                k.dma("sp", z_[:], self.Z[sl, :])
                yield
                k.I("pool", "tensor_tensor", out=d_[:, 0, :], in0=d_[:, 0, :], in1=d_[:, 1, :], op=ALU.add)
                k.I("pool", "tensor_tensor", out=d_[:, 0, :], in0=d_[:, 0, :], in1=d_[:, 2, :], op=ALU.add)
                k.I("dve", "tensor_tensor", out=f_[:], in0=f_[:], in1=b_[:], op=ALU.add)
                yield
                dv = d_[:, 0, :].rearrange("p (h e) -> p h e", e=65)
                k.I("dve", "reciprocal", out=rd[:], in_=dv[:, :, 64])
                k.I("dve", "tensor_tensor", out=c_[:, 512:768].rearrange("p (h e) -> p h e", e=64), in0=dv[:, :, 0:64],
                    in1=rd[:].unsqueeze(2).broadcast_to([128, 4, 64]), op=ALU.mult)
                k.I("pool", "tensor_tensor", out=sq[:], in0=f_[:], in1=f_[:], op=ALU.mult)
                yield
                k.I("dve", "tensor_reduce", out=ms[:], in_=sq[:].rearrange("p (h e) -> p h e", e=128), axis=AX.X, op=ALU.add)
                yield
                k.I("act", "activation", out=ms[:], in_=ms[:], func=AF.Ln, scale=1.0 / 128.0, bias=self.eps_norm[:, 0:1])
                k.I("act", "activation", out=ms[:], in_=ms[:], func=AF.Exp, scale=-0.5)
                yield
                fv = f_[:].rearrange("p (h e) -> p h e", e=128)
                k.I("dve", "tensor_tensor", out=fv, in0=fv, in1=ms[:].unsqueeze(2).broadcast_to([128, 4, 128]), op=ALU.mult)
                k.I("dve", "tensor_tensor", out=fv, in0=fv, in1=dnn[:].unsqueeze(1).broadcast_to([128, 4, 128]), op=ALU.mult)
                k.I("dve", "tensor_tensor", out=c_[:, 0:512], in0=f_[:], in1=z_[:], op=ALU.mult)

            self.mixer_finish(0, mod, self.x, self.ab_w_out, 768, build_cat, 5)

    def p5_finish1(self, mod):
        k = self.k
        with k.scope():
            at = [k.sb("at5", [128, 16, 65]) for _ in range(3)]
            sk = k.sb("sk5", [128, 16])
            dens = [k.sb("den5", [128, 16]) for _ in range(3)]
            k.dma("sp", sk[:], self.swa_sinks[0:1, :].broadcast_to([128, 16]))
            k.I("act", "activation", out=sk[:], in_=sk[:], func=AF.Exp)

            def build_cat(t, c_):
                sl = slice(t * 128, (t + 1) * 128)
                a_ = at[t % 3]; den = dens[t % 3]
                k.dma("sp", a_[:].rearrange("p h e -> p (h e)"), self.ATT[sl, :])
                yield
                k.I("dve", "tensor_tensor", out=den[:], in0=a_[:, :, 64], in1=sk[:], op=ALU.add)
                k.I("dve", "reciprocal", out=den[:], in_=den[:])
                k.I("dve", "tensor_tensor", out=c_[:].rearrange("p (h e) -> p h e", e=64), in0=a_[:, :, 0:64],
                    in1=den[:].unsqueeze(2).broadcast_to([128, 16, 64]), op=ALU.mult)

            self.mixer_finish(1, mod, self.X2, self.swa_w_out, 1024, build_cat, 1)

    def moe(self, layer, mod, OUTX):
        k, S, NT = self.k, self.S, self.NT
        cap = S // 8
        NA = cap // 128
        nc = self.nc
        with k.scope():
            idx_i = k.sb("idx_i", [128, NE, NA], I32)
            gate_s = k.sb("gate_s", [128, NE, NA])
            with k.scope():
                affs = k.sb("affs6", [128, NT, NE])
                k.dma("sp", affs[:], self.AFF[:])
                affT = k.sb("affT", [NE, S])
                scr = k.sb("scr6", [NE, S], BF16)
                pt = [k.ps("p6t", [128, 512]) for _ in range(2)]
                for t0 in range(0, NT, 4):
                    p = pt[(t0 // 4) % 2]
                    for i in range(4):
                        k.tp(p[0:NE, i * 128:(i + 1) * 128], affs[:, t0 + i, :], self.ident_f[:])
                    k.cp(t0 // 4, affT[:, t0 * 128:(t0 + 4) * 128], p[0:NE, :])
                lo = k.sb("lo6", [NE, 1]); mid = k.sb("mid6", [NE, 1]); cnt = k.sb("cnt6", [NE, 1]); ge = k.sb("ge6", [NE, 1])
                k.I("dve", "memset", ap=lo[:], constant=0.0)
                for itn in range(34):
                    dlt = 2.0 ** (-(itn + 1))
                    k.I("dve", "tensor_scalar", out=mid[:], in0=lo[:], scalar1=dlt, scalar2=None, op0=ALU.add)
                    k.I("dve", "tensor_scalar", out=scr[:], in0=affT[:], scalar1=mid[:, 0:1], scalar2=None, op0=ALU.is_gt, op1=ALU.add,
                        accum_out=cnt[:])
                    k.I("dve", "tensor_scalar", out=ge[:], in0=cnt[:], scalar1=float(cap) - 0.5, scalar2=dlt, op0=ALU.is_gt, op1=ALU.mult)
                    k.I("dve", "tensor_tensor", out=lo[:], in0=lo[:], in1=ge[:], op=ALU.add)
                maskT = k.sb("maskT", [NE, S])
                posT = affT
                k.I("dve", "tensor_scalar", out=maskT[:], in0=affT[:], scalar1=lo[:, 0:1], scalar2=None, op0=ALU.is_gt)
                k.I("dve", "tensor_tensor_scan", out=posT[:], data0=maskT[:], data1=maskT[:], initial=0.0, op0=ALU.add, op1=ALU.max)
                k.I("dve", "tensor_tensor", out=posT[:], in0=posT[:], in1=maskT[:], op=ALU.subtract)
                posk = k.sb("posk", [128, NT, NE]); mskk = k.sb("mskk", [128, NT, NE])
                for src, dst in ((posT, posk), (maskT, mskk)):
                    for t0 in range(0, NT, 32):
                        n = min(32, NT - t0)
                        p = pt[(t0 // 32) % 2]
                        for i in range(n):
                            k.tp(p[:, i * NE:(i + 1) * NE], src[:, (t0 + i) * 128:(t0 + i + 1) * 128], self.ident_f[0:NE, 0:NE])
                        k.I("dve", "tensor_copy", out=dst[:, t0:t0 + n, :].rearrange("p a b -> p (a b)"), in_=p[:, 0:n * NE])
                posi = k.sb("posi6", [128, NT, NE], I32); hii = k.sb("hii6", [128, NT, NE], I32); loi = k.sb("loi6", [128, NT, NE], I32)
                hif = k.sb("hif6", [128, NT, NE]); lof = k.sb("lof6", [128, NT, NE])
                k.I("dve", "tensor_copy", out=posi[:], in_=posk[:])
                k.I("dve", "tensor_single_scalar", out=hii[:], in_=posi[:], scalar=7, op=ALU.arith_shift_right)
                k.I("dve", "tensor_single_scalar", out=loi[:], in_=posi[:], scalar=127, op=ALU.bitwise_and)
                k.I("dve", "tensor_copy", out=hif[:], in_=hii[:])
                k.I("dve", "tensor_copy", out=lof[:], in_=loi[:])
                io128i = k.sb("io128i", [128, 128], I32); io128 = k.sb("io128", [128, 128])
                k.I("pool", "iota", out=io128i[:], pattern=[[1, 128]], base=0, channel_multiplier=0)
                k.I("dve", "tensor_copy", out=io128[:], in_=io128i[:])
                toki = k.sb("toki", [128, NT], I32); tokf = k.sb("tokf", [128, NT])
                k.I("pool", "iota", out=toki[:], pattern=[[128, NT]], base=0, channel_multiplier=1)
                k.I("dve", "tensor_copy", out=tokf[:], in_=toki[:])
                io128b = k.sb("io128b", [128, 128], BF16)
                k.I("dve", "tensor_copy", out=io128b[:], in_=io128[:])
                lofb = k.sb("lofb", [128, NT, NE], BF16)
                k.I("dve", "tensor_copy", out=lofb[:], in_=lof[:])
                thii = k.sb("thii", [128, NT], I32); tloi = k.sb("tloi", [128, NT], I32)
                thif = k.sb("thif", [128, NT]); tlof = k.sb("tlof", [128, NT])
                k.I("dve", "tensor_single_scalar", out=thii[:], in_=toki[:], scalar=6, op=ALU.arith_shift_right)
                k.I("dve", "tensor_single_scalar", out=tloi[:], in_=toki[:], scalar=63, op=ALU.bitwise_and)
                k.I("dve", "tensor_copy", out=thif[:], in_=thii[:])
                k.I("dve", "tensor_copy", out=tlof[:], in_=tloi[:])
                ghb = k.sb("ghb", [128, NT, NE], BF16); ghf = k.sb("ghf", [128, NT, NE]); glf = k.sb("glf", [128, NT, NE])
                k.I("dve", "tensor_copy", out=ghb[:], in_=affs[:])
                k.I("dve", "tensor_copy", out=ghf[:], in_=ghb[:])
                k.I("dve", "tensor_tensor", out=glf[:], in0=affs[:], in1=ghf[:], op=ALU.subtract)
                A1 = [k.sb("A61", [128, NT, NA]) for _ in range(2)]
                A2 = [k.sb("A62", [128, NT, 4 * NA], BF16) for _ in range(2)]
                TB = 8
                Bblk = [k.sb("B6", [128, TB, 128], BF16) for _ in range(3)]
                pacc = [k.ps("pacc", [128, 512]) for _ in range(2)]
                racc = k.sb("racc", [128, 4 * NA])
                bi = 0
                for e in range(NE):
                    a1 = A1[e % 2]; a2 = A2[e % 2]
                    k.I("dve", "tensor_tensor", out=a1[:], in0=io128[:, 0:NA].unsqueeze(1).broadcast_to([128, NT, NA]),
                        in1=hif[:, :, e].unsqueeze(2).broadcast_to([128, NT, NA]), op=ALU.is_equal)
                    k.I("dve", "tensor_tensor", out=a1[:], in0=a1[:], in1=mskk[:, :, e].unsqueeze(2).broadcast_to([128, NT, NA]), op=ALU.mult)
                    k.I("dve", "tensor_tensor", out=a2[:, :, 0:NA], in0=a1[:], in1=thif[:].unsqueeze(2).broadcast_to([128, NT, NA]), op=ALU.mult)
                    k.I("dve", "tensor_tensor", out=a2[:, :, NA:2 * NA], in0=a1[:], in1=tlof[:].unsqueeze(2).broadcast_to([128, NT, NA]), op=ALU.mult)
                    k.I("dve", "tensor_tensor", out=a2[:, :, 2 * NA:3 * NA], in0=a1[:], in1=ghf[:, :, e].unsqueeze(2).broadcast_to([128, NT, NA]), op=ALU.mult)
                    k.I("dve", "tensor_tensor", out=a2[:, :, 3 * NA:4 * NA], in0=a1[:], in1=glf[:, :, e].unsqueeze(2).broadcast_to([128, NT, NA]), op=ALU.mult)
                    pa = pacc[e % 2]
                    for t0 in range(0, NT, TB):
                        bb = Bblk[bi % 3]; bi += 1
                        k.I("dve", "tensor_tensor", out=bb[:], in0=io128b[:].unsqueeze(1).broadcast_to([128, TB, 128]),
                            in1=lofb[:, t0:t0 + TB, e].unsqueeze(2).broadcast_to([128, TB, 128]), op=ALU.is_equal)
                        for j in range(TB):
                            t = t0 + j
                            k.mm(pa[:, 0:4 * NA], bb[:, j, :], a2[:, t, :], start=(t == 0), stop=(t == NT - 1))
                    k.I("act", "activation", out=racc[:], in_=pa[:, 0:4 * NA], func=AF.Copy)
                    k.I("dve", "scalar_tensor_tensor", out=racc[:, 0:NA], in0=racc[:, 0:NA], scalar=64.0, in1=racc[:, NA:2 * NA], op0=ALU.mult, op1=ALU.add)
                    k.I("dve", "tensor_copy", out=idx_i[:, e, :], in_=racc[:, 0:NA])
                    k.I("dve", "tensor_tensor", out=gate_s[:, e, :], in0=racc[:, 2 * NA:3 * NA], in1=racc[:, 3 * NA:4 * NA], op=ALU.add)
                if "IDX" in self.dbg:
                    self.IDX = k.dram("IDX", [128, NE, NA], I32, kind="ExternalOutput")
                    k.dma("sp", self.IDX[:], idx_i[:])
                    self.GTS = k.dram("GTS", [128, NE, NA], F32, kind="ExternalOutput")
                    k.dma("sp", self.GTS[:], gate_s[:])
            if getattr(self, "stop", None) == "route":
                return
            with k.scope():
                zf = k.sb("zf6", [128, D])
                k.I("pool", "memset", ap=zf[:], constant=0.0)
                for t in range(NT):
                    k.dma("sp" if t % 2 == 0 else "act", self.Y.reg(t)[t * 128:(t + 1) * 128, :], zf[:])
            with k.scope():
                wg = [k.sb("wg", [128, 8, D], BF16) for _ in range(2)]
                wu = [k.sb("wu", [128, 8, D], BF16) for _ in range(2)]
                wd = [k.sb("wd", [128, 8, D], BF16) for _ in range(2)]
                st = [k.sb("wst6", [128, D]) for _ in range(6)]
                xg = [k.sb("xg6", [128, D], BF16) for _ in range(2)]
                xgT = k.sb("xgT", [128, 8, cap], BF16)
                hT = k.sb("hT6", [128, 8, cap], BF16)
                sg = [k.sb("sg6", [128, 512]) for _ in range(2)]
                yst = [k.sb("yst", [128, D]) for _ in range(2)]
                pst = [k.ps("p6x", [128, 512], BF16) for _ in range(2)]
                pgu = [k.ps("p6g", [128, 512]) for _ in range(4)]
                pdn = [k.ps("p6d", [128, 512]) for _ in range(2)]
                self._wi = 0

                def loadw1(dst, src, kc):
                    s_ = st[self._wi % len(st)]
                    k.dma("sp", s_[:], src[kc * 128:(kc + 1) * 128, :])
                    r = self._wi % 4
                    if r == 3:
                        k.I("pool", "tensor_copy", out=dst[:, kc, :], in_=s_[:])
                    elif r == 1:
                        k.I("dve", "tensor_copy", out=dst[:, kc, :], in_=s_[:])
                    else:
                        k.I("act", "activation", out=dst[:, kc, :], in_=s_[:], func=AF.Copy)
                    self._wi += 1

                def loadw_chunk(e, kc):
                    loadw1(wg[e % 2], self.moe_w_gate[layer, e], kc)
                    loadw1(wu[e % 2], self.moe_w_up[layer, e], kc)
                    loadw1(wd[e % 2], self.moe_w_down[layer, e], kc)

                for kc in range(8):
                    loadw_chunk(0, kc)
                self._ci = 0

                def gather(e, a):
                    k.dma("pool", xg[a % 2][:], self.H2[:, :], method="indirect_dma_start", out_offset=None,
                          in_offset=bass.IndirectOffsetOnAxis(ap=idx_i.t[:, e, a:a + 1], axis=0), R=[idx_i])

                def xpose(e, a):
                    x_ = xg[a % 2]
                    for g0 in range(2):
                        p = pst[g0]
                        for i in range(4):
                            kc = g0 * 4 + i
                            k.tp(p[:, i * 128:(i + 1) * 128], x_[:, kc * 128:(kc + 1) * 128], self.ident_b[:])
                        k.cp(self._ci, xgT[:, g0 * 4:(g0 + 1) * 4, a * 128:(a + 1) * 128], p[:].rearrange("p (a b) -> p a b", b=128)); self._ci += 1

                ci = 0
                for e in range(NE):
                    G, U, Dn = wg[e % 2], wu[e % 2], wd[e % 2]
                    if e == 0:
                        for a in range(NA):
                            gather(0, a); xpose(0, a)
                    for fc in range(8):
                        for n0 in range(0, cap, 512):
                            nw = min(512, cap - n0)
                            p1 = pgu[ci % 4]; p2 = pgu[(ci + 1) % 4]; s_ = sg[(ci // 2) % 2]; ci += 2
                            for kc in range(8):
                                k.mm(p1[:, 0:nw], G[:, kc, fc * 128:(fc + 1) * 128], xgT[:, kc, n0:n0 + nw], start=(kc == 0), stop=(kc == 7))
                            for kc in range(8):
                                k.mm(p2[:, 0:nw], U[:, kc, fc * 128:(fc + 1) * 128], xgT[:, kc, n0:n0 + nw], start=(kc == 0), stop=(kc == 7))
                            k.I("act", "activation", out=s_[:, 0:nw], in_=p1[:, 0:nw], func=AF.Silu)
                            k.I("dve", "tensor_tensor", out=hT[:, fc, n0:n0 + nw], in0=s_[:, 0:nw], in1=p2[:, 0:nw], op=ALU.mult)
                        if e + 1 < NE:
                            loadw_chunk(e + 1, fc)
                    for a in range(NA):
                        if e + 1 < NE:
                            gather(e + 1, a)
                            if a >= 1:
                                xpose(e + 1, a - 1)
                        y_ = yst[a % 2]
                        for hh in range(2):
                            p = pdn[hh]
                            for fc in range(8):
                                k.mm(p[:], hT[:, fc, a * 128:(a + 1) * 128], Dn[:, fc, hh * 512:(hh + 1) * 512], start=(fc == 0), stop=(fc == 7))
                            if hh == 0:
                                k.I("dve", "tensor_scalar", out=y_[:, 0:512], in0=p[:], scalar1=gate_s[:, e, a:a + 1], scalar2=None, op0=ALU.mult)
                            else:
                                k.I("act", "activation", out=y_[:, 512:1024], in_=p[:], func=AF.Copy, scale=gate_s[:, e, a:a + 1])
                        k.dma("pool", self.Y[:, :], y_[:], method="indirect_dma_start",
                              out_offset=bass.IndirectOffsetOnAxis(ap=idx_i.t[:, e, a:a + 1], axis=0), in_offset=None,
                              compute_op=ALU.add, R=[idx_i])
                    if e + 1 < NE:
                        xpose(e + 1, NA - 1)
            with k.scope():
                g, b = self.ln_setup(self.ln_ffn_g, self.ln_ffn_b, layer)
                NR = 8
                tmps = [self.ln_tmp() for _ in range(NR)]
                xt = [k.sb("xt7", [128, D]) for _ in range(4)]
                yt = [k.sb("yt7", [128, D]) for _ in range(NR)]

                def tile(t):
                    sl = slice(t * 128, (t + 1) * 128)
                    x_ = xt[t % 4]; y_ = yt[t % NR]; tm = tmps[t % NR]
                    st = tm["st"]; mv = tm["mv"]; rs = tm["rs"]; nb = tm["nb"]
                    k.dma("sp", x_[:], self.X1[sl, :])
                    k.dma("act", y_[:], self.Y[sl, :])
                    yield
                    k.I("pool", "tensor_tensor", out=y_[:], in0=y_[:], in1=mod[:, 5 * D:6 * D], op=ALU.mult)
                    yield
                    k.I("dve", "scalar_tensor_tensor", out=y_[:], in0=x_[:], scalar=float(ALPHA), in1=y_[:], op0=ALU.mult, op1=ALU.add)
                    for hh in range(2):
                        k.I("dve", "bn_stats", out=st[:, hh, :], in_=y_[:, hh * 512:(hh + 1) * 512])
                    k.I("dve", "bn_aggr", out=mv[:], in_=st[:].rearrange("p a b -> p (a b)"))
                    yield
                    k.I("act", "activation", out=rs[:], in_=mv[:, 1:2], func=AF.Ln, bias=self.eps_ln[:, 0:1])
                    k.I("act", "activation", out=rs[:], in_=rs[:], func=AF.Exp, scale=-0.5)
                    yield
                    k.I("dve", "scalar_tensor_tensor", out=nb[:], in0=mv[:, 0:1], scalar=-1.0, in1=rs[:], op0=ALU.mult, op1=ALU.mult)
                    yield
                    k.I("act", "activation", out=y_[:], in_=y_[:], func=AF.Identity, scale=rs[:, 0:1], bias=nb[:, 0:1])
                    yield
                    k.I("pool", "tensor_tensor", out=y_[:], in0=y_[:], in1=g[:], op=ALU.mult)
                    yield
                    k.I("dve", "tensor_tensor", out=y_[:], in0=y_[:], in1=b[:], op=ALU.add)
                    k.dma("sp", OUTX.reg(t)[sl, :], y_[:])
                    yield
                run_pipelined(NT, tile, 8)


    def p1_proj1(self, mod):
        k, S, NT = self.k, self.S, self.NT
        MT = 4
        MW = MT * 128
        with k.scope():
            w = k.sb("w_in1", [128, 8, 1536], BF16)
            self.load_w(w, self.swa_w_in, 1536)
            xs = [k.sb("xs1", [128, MT, D]) for _ in range(2)]
            hb = k.sb("hb1", [128, MT, D], BF16)
            hTs = [k.sb("hT1", [128, 8, MW], BF16) for _ in range(2)]
            qb = [k.sb("qb1", [128, 1536]) for _ in range(3)]
            qbb = [k.sb("qbb1", [128, 1536], BF16) for _ in range(2)]
            kdup = [k.sb("kdup", [128, 4, 2, 64], BF16) for _ in range(2)]
            qkT = [k.sb("qkT1", [128, 12, 128], BF16) for _ in range(2)]
            rts = [[k.sb("rtmp1", [128, 24, 8]) for _ in range(4)] for _ in range(2)]
            pst = [k.ps("pst1", [128, 512], BF16) for _ in range(2)]
            psm = [k.ps("psm1", [128, 512]) for _ in range(4)]
            self._ei = 0
            self._pi = 0

            def nextp():
                p = psm[self._pi % 4]; self._pi += 1
                return p

            def cpy(out, in_):
                k.cp(self._ei, out, in_); self._ei += 1

            def tile(t):
                m, j = t // MT, t % MT
                hT = hTs[m % 2]
                if j == 0:
                    x_ = xs[m % 2]
                    k.dma("sp", x_[:], self.X2[m * MW:(m + 1) * MW, :].rearrange("(j p) d -> p j d", p=128))
                    for jj in range(MT):
                        k.I("dve", "tensor_tensor", out=x_[:, jj, :], in0=x_[:, jj, :], in1=mod[:, D:2 * D], op=ALU.mult)
                        k.I("pool", "tensor_tensor", out=hb[:, jj, :], in0=x_[:, jj, :], in1=mod[:, 0:D], op=ALU.add)
                yield
                if j == 0:
                    for kc in range(8):
                        p = pst[kc % 2]
                        for jj in range(MT):
                            k.tp(p[:, jj * 128:(jj + 1) * 128], hb[:, jj, kc * 128:(kc + 1) * 128], self.ident_b[:])
                        cpy(hT[:, kc, :], p[:])
                yield
                q_ = qb[t % 3]
                for g in range(3):
                    p = nextp()
                    for kc in range(8):
                        k.mm(p[:], hT[:, kc, j * 128:(j + 1) * 128], w[:, kc, g * 512:(g + 1) * 512], start=(kc == 0), stop=(kc == 7))
                    cpy(q_[:, g * 512:(g + 1) * 512], p[:])
                yield
                self.rope(q_, 20, t, rts[t % 2])
                yield
                qq = qbb[t % 2]
                k.I("act", "activation", out=qq[:], in_=q_[:], func=AF.Copy)
                k.dma("sp", self.V1.reg(t)[t * 128:(t + 1) * 128, :], qq[:, 1280:1536])
                kd_ = kdup[t % 2]
                kv = qq[:, 1024:1280].rearrange("p (h d) -> p h d", d=64)
                k.I("pool", "tensor_copy", out=kd_[:, :, 0, :], in_=kv)
                k.I("pool", "tensor_copy", out=kd_[:, :, 1, :], in_=kv)
                yield
                qt = qkT[t % 2]
                for g3 in range(3):
                    p = pst[g3 % 2]
                    for i4 in range(4):
                        cc = g3 * 4 + i4
                        src = qq[:, cc * 128:(cc + 1) * 128] if cc < 8 else kd_[:, cc - 8, :, :].rearrange("p a b -> p (a b)")
                        k.tp(p[:, i4 * 128:(i4 + 1) * 128], src, self.ident_b[:])
                    cpy(qt[:, g3 * 4:(g3 + 1) * 4, :], p[:].rearrange("p (a b) -> p a b", b=128))
                k.dma("act", V(self.Q1T.t.rearrange("(cc p) t -> p cc t", p=128)[:, :, t * 128:(t + 1) * 128], self.Q1T.reg(t)),
                      qt[:, 0:8, :])
                k.dma("act", V(self.K1T.t.rearrange("(cc p) t -> p cc t", p=128)[:, :, t * 128:(t + 1) * 128], self.K1T.reg(t)),
                      qt[:, 8:12, :])
                yield

            run_pipelined(NT, tile)

    def p4_swa(self):
        with self.k.scope():
            masks = self.attn_masks(128)
            heads = [(h // 2, h // 4, (h % 2) * 64, h // 4) for h in range(16)]
            self.attn(self.Q1T, self.K1T, self.V1, heads, 8, 4, 256, 0, 1, masks, self.ATT)


def build(S, dbg=None, stop=None):
    P = Prog(S, dbg)
    k = P.k
    P.declare()
    with k.scope():
        P.consts()
        k.mark("P.consts()")
        P.rope_tables()
        k.mark("P.rope_tables()")
        mod = k.sb("mod", [128, 6 * D])
        P.ada(0, mod)
        k.mark("P.ada(0, mod)")
        if "MOD" in (dbg or []):
            P.MOD = k.dram("MOD", [128, 6 * D], F32, kind="ExternalOutput")
            k.dma("sp", P.MOD[:], mod[:])
        P.p1_proj0(mod)
        k.mark("P.p1_proj0(mod)")
        P.p2_dnprep()
        k.mark("P.p2_dnprep()")
        P.p3_deltanet()
        k.mark("P.p3_deltanet()")
        P.p4_dilated()
        k.mark("P.p4_dilated()")
        P.p5_finish0(mod)
        k.mark("P.p5_finish0(mod)")
        P.stop = stop
        if stop == "p5":
            k.barrier()
            return P
        P.moe(0, mod, P.X2)
        k.mark("P.moe(0, mod, P.X2)")
        if stop:
            k.barrier()
            return P
        P.ada(1, mod)
        k.mark("P.ada(1, mod)")
        P.p1_proj1(mod)
        k.mark("P.p1_proj1(mod)")
        P.p4_swa()
        k.mark("P.p4_swa()")
        P.p5_finish1(mod)
        k.mark("P.p5_finish1(mod)")
        P.moe(1, mod, P.out)
        k.mark("P.moe(1, mod, P.out)")
    return P


def host_inputs(inputs, b, S):
    f = lambda a: np.ascontiguousarray(np.asarray(a))
    m = {}
    m["x"] = f(inputs["x"][b, :S])
    m["c"] = f(np.asarray(inputs["c"][b]).reshape(8, 128).T)
    m["pos"] = f(np.asarray(inputs["positions"][b, :S]).reshape(S // 128, 128).T.astype(np.int32))
    m["ada_w"] = f(inputs["ada_w"])
    m["ada_b"] = f(inputs["ada_b"])
    m["ab_w_in"] = f(inputs["ab_w_in"][0])
    m["ab_conv_w"] = f(np.asarray(inputs["ab_conv_w"][0]).reshape(5, 12, 128).transpose(2, 1, 0))
    m["ab_a_log"] = f(np.asarray(inputs["ab_a_log"][0]).reshape(1, 8))
    m["ab_dt_bias"] = f(np.asarray(inputs["ab_dt_bias"][0]).reshape(1, 8))
    m["ab_dn_norm"] = f(np.asarray(inputs["ab_dn_norm"][0]).reshape(1, 128))
    m["ab_w_out"] = f(inputs["ab_w_out"][0])
    m["swa_w_in"] = f(inputs["swa_w_in"][0])
    m["swa_sinks"] = f(np.asarray(inputs["swa_sinks"][0]).reshape(1, 16))
    m["swa_w_out"] = f(inputs["swa_w_out"][0])
    for n in ("ln_mix_g", "ln_mix_b", "router_w", "moe_w_gate", "moe_w_up", "moe_w_down", "ln_ffn_g", "ln_ffn_b"):
        m[n] = f(inputs[n])
    return m


def kernel(**inputs):
    S = 8192
    P = build(S)
    in_maps = [host_inputs(inputs, b, S) for b in range(8)]
    res = run_bass_kernel_spmd(P.nc, in_maps, core_ids=list(range(8)))
    return np.stack([r["out"] for r in res.results], axis=0)
```

```python
import math
import contextlib
import numpy as np
import concourse.bass as bass
import concourse.mybir as mybir
from concourse.bass_utils import run_bass_kernel_spmd

F32 = mybir.dt.float32
BF16 = mybir.dt.bfloat16
I32 = mybir.dt.int32
U32 = mybir.dt.uint32
AF = mybir.ActivationFunctionType
ALU = mybir.AluOpType
AX = mybir.AxisListType

D = 1024
NKC = 8
AB_IN = 4368
ALPHA = 4.0 ** 0.25
LN_EPS = 1e-5
NORM_EPS = 1e-6
BIG = 30000.0
NE = 16


class Buf:
    ALL = []

    def __init__(self, t, name):
        self.t = t
        self.name = name
        self.w = {}
        self.r = {}
        self.regs = {}
        Buf.ALL.append(self)

    def reg(self, key):
        if key not in self.regs:
            self.regs[key] = Buf(self.t, "%s@%s" % (self.name, key))
        return self.regs[key]

    def __getitem__(self, idx):
        return V(self.t[idx], self)


class V:
    def __init__(self, ap, buf):
        self.ap = ap
        self.buf = buf

    def __getitem__(self, idx):
        return V(self.ap[idx], self.buf)

    def rearrange(self, *a, **k):
        return V(self.ap.rearrange(*a, **k), self.buf)

    def bitcast(self, dt):
        return V(self.ap.bitcast(dt), self.buf)

    def broadcast_to(self, *a, **k):
        return V(self.ap.broadcast_to(*a, **k), self.buf)

    def unsqueeze(self, *a, **k):
        return V(self.ap.unsqueeze(*a, **k), self.buf)


WRITE_KEYS = ("out", "accum_out", "ap")


class K:
    NDMA_SLOTS = 6

    def __init__(self, nc):
        self.nc = nc
        Buf.ALL = []
        self.eng = {"pe": nc.tensor, "act": nc.scalar, "dve": nc.vector, "pool": nc.gpsimd, "sp": nc.sync}
        self.sem = {}
        self.cnt = {}
        for e in self.eng:
            self.sem[e] = nc.alloc_semaphore("sem_" + e)
            self.cnt[e] = 0
        self.dsem = {}
        self.dcnt = {}
        for q in ("sp", "act", "pool"):
            self.dsem[q] = [nc.alloc_semaphore("dsem_%s_%d" % (q, i)) for i in range(self.NDMA_SLOTS)]
            self.dcnt[q] = 0
        self.seen = {e: {} for e in self.eng}
        self.n_inst = 0
        self.n_wait = 0
        self.stack = None
        self.uid = 0

    @contextlib.contextmanager
    def scope(self):
        old = self.stack
        with contextlib.ExitStack() as st:
            self.stack = st
            yield
            self.barrier()
        self.stack = old

    def sb(self, name, shape, dt=F32):
        self.uid += 1
        nm = "%s_%d" % (name, self.uid)
        t = self.stack.enter_context(self.nc.sbuf_tensor(nm, list(shape), dt))
        return Buf(t, nm)

    def ps(self, name, shape, dt=F32):
        self.uid += 1
        nm = "%s_%d" % (name, self.uid)
        t = self.stack.enter_context(self.nc.psum_tensor(nm, list(shape), dt))
        return Buf(t, nm)

    def dram(self, name, shape, dt, kind="Internal"):
        t = self.nc.dram_tensor(name, list(shape), dt, kind=kind).ap()
        return Buf(t, name)

    def _semobj(self, key):
        if key[0] == "c":
            return self.sem[key[1]]
        return self.dsem[key[1]][key[2]]

    def _wait(self, e, key, val):
        s = self.seen[e]
        if s.get(key, 0) >= val:
            return
        self.eng[e].wait_ge(self._semobj(key), val)
        self.n_wait += 1
        s[key] = val

    def mark(self, name):
        if not hasattr(self, "marks"):
            self.marks = []
        self.marks.append((name, dict(self.cnt)))

    def barrier(self):
        for e in self.eng:
            for e2 in self.eng:
                if e2 != e and self.cnt[e2] > 0:
                    self._wait(e, ("c", e2), self.cnt[e2])
            for q in self.dsem:
                i = self.dcnt[q]
                for slot in range(self.NDMA_SLOTS):
                    n = (i - slot + self.NDMA_SLOTS - 1) // self.NDMA_SLOTS if i > slot else 0
                    if n > 0:
                        self._wait(e, ("d", q, slot), 16 * n)
        for b in Buf.ALL:
            b.w = {}
            b.r = {}
            b.regs = {}

    def _deps(self, e, reads, writes, is_dma=False):
        need = {}

        def add(key, val):
            if need.get(key, 0) < val:
                need[key] = val

        mykey = ("c", e)
        for b in reads:
            for key, val in b.w.items():
                if key == mykey and e == "pe" and not is_dma:
                    continue
                add(key, val)
        for b in writes:
            for key, val in b.w.items():
                if key == mykey and not is_dma:
                    continue
                add(key, val)
            for key, val in b.r.items():
                if key == mykey and not is_dma:
                    continue
                add(key, val)
        for key, val in need.items():
            self._wait(e, key, val)

    def I(self, e, method, **kw):
        reads, writes = [], []
        extra_r = kw.pop("R", [])
        extra_w = kw.pop("W", [])
        kw2 = {}
        for k_, v in kw.items():
            if isinstance(v, V):
                (writes if k_ in WRITE_KEYS else reads).append(v.buf)
                kw2[k_] = v.ap
            else:
                kw2[k_] = v
        for v in extra_r:
            reads.append(v.buf if isinstance(v, V) else v)
        for v in extra_w:
            writes.append(v.buf if isinstance(v, V) else v)
        self._deps(e, reads, writes)
        ins = getattr(self.eng[e], method)(**kw2)
        self.cnt[e] += 1
        ins.then_inc(self.sem[e], 1)
        key = ("c", e)
        val = self.cnt[e]
        for b in writes:
            b.w = {key: val}
            b.r = {}
        for b in reads:
            if b in writes:
                continue
            b.r[key] = val
        self.n_inst += 1
        return ins

    def dma(self, q, out, in_, method="dma_start", R=(), **kw):
        reads = [in_.buf] + [v.buf if isinstance(v, V) else v for v in R]
        writes = [out.buf]
        i = self.dcnt[q]
        slot = i % self.NDMA_SLOTS
        rnd = i // self.NDMA_SLOTS
        key = ("d", q, slot)
        if rnd > 0:
            self._wait(q, key, 16 * rnd)
        self._deps(q, reads, writes, is_dma=True)
        kw2 = {}
        for k_, v in kw.items():
            kw2[k_] = v
        ins = getattr(self.eng[q], method)(out=out.ap, in_=in_.ap, **kw2)
        ins.then_inc(self.dsem[q][slot], 16)
        self.dcnt[q] += 1
        val = 16 * (rnd + 1)
        for b in writes:
            b.w = {key: val}
            b.r = {}
        for b in reads:
            b.r[key] = val
        self.n_inst += 1
        return ins

    def mm(self, out, lhsT, rhs, start=True, stop=True):
        return self.I("pe", "matmul", out=out, lhsT=lhsT, rhs=rhs, start=start, stop=stop)

    def tp(self, out, in_, ident):
        return self.I("pe", "transpose", out=out, in_=in_, identity=ident)

    def cp(self, i, out, in_):
        if i % 2 == 0:
            return self.I("dve", "tensor_copy", out=out, in_=in_)
        return self.I("act", "activation", out=out, in_=in_, func=AF.Copy)


def run_pipelined(n, make_gen, nstages=None):
    live = []
    t_next = 0
    while t_next < n or live:
        if t_next < n:
            live.insert(0, make_gen(t_next))
            t_next += 1
        nxt = []
        for g in live:
            try:
                next(g)
                nxt.append(g)
            except StopIteration:
                pass
        live = nxt


class Prog:
    def __init__(self, S, dbg=None):
        self.S = S
        self.NT = S // 128
        self.dbg = dbg or []
        self.nc = bass.Bass("TRN2", target_bir_lowering=False)
        self.k = K(self.nc)
        self.outs = []

    def declare(self):
        k, S = self.k, self.S
        ein = lambda n, s, d=F32: k.dram(n, s, d, kind="ExternalInput")
        self.x = ein("x", [S, D])
        self.c = ein("c", [128, 8])
        self.pos = ein("pos", [128, self.NT], I32)
        self.ada_w = ein("ada_w", [2, D, 6 * D])
        self.ada_b = ein("ada_b", [2, 6 * D])
        self.ab_w_in = ein("ab_w_in", [D, AB_IN])
        self.ab_conv_w = ein("ab_conv_w", [128, 12, 5])
        self.ab_a_log = ein("ab_a_log", [1, 8])
        self.ab_dt_bias = ein("ab_dt_bias", [1, 8])
        self.ab_dn_norm = ein("ab_dn_norm", [1, 128])
        self.ab_w_out = ein("ab_w_out", [768, D])
        self.swa_w_in = ein("swa_w_in", [D, 1536])
        self.swa_sinks = ein("swa_sinks", [1, 16])
        self.swa_w_out = ein("swa_w_out", [D, D])
        self.ln_mix_g = ein("ln_mix_g", [2, D])
        self.ln_mix_b = ein("ln_mix_b", [2, D])
        self.router_w = ein("router_w", [2, D, NE])
        self.moe_w_gate = ein("moe_w_gate", [2, NE, D, D])
        self.moe_w_up = ein("moe_w_up", [2, NE, D, D])
        self.moe_w_down = ein("moe_w_down", [2, NE, D, D])
        self.ln_ffn_g = ein("ln_ffn_g", [2, D])
        self.ln_ffn_b = ein("ln_ffn_b", [2, D])
        self.out = k.dram("out", [S, D], F32, kind="ExternalOutput")
        sc = lambda n, s, d: k.dram(n, s, d, kind=("ExternalOutput" if n in self.dbg else "Internal"))
        self.QKVA = sc("QKVA", [1536, S + 4], BF16)
        self.Z = sc("Z", [S, 512], BF16)
        self.QBT = sc("QBT", [768, S], BF16)
        self.KBT = sc("KBT", [768, S], BF16)
        self.VB = sc("VB", [S, 768], BF16)
        self.QAT = sc("QAT", [512, S], BF16)
        self.KAT = sc("KAT", [512, S], BF16)
        self.KA = sc("KA", [S, 512], BF16)
        self.VA = sc("VA", [S, 512], BF16)
        self.OF = sc("OF", [S, 512], F32)
        self.OB = sc("OB", [S, 512], F32)
        self.DIL = sc("DIL", [3, S, 260], F32)
        self.X1 = sc("X1", [S, D], F32)
        self.H2 = sc("H2", [S, D], BF16)
        self.Y = sc("Y", [S, D], F32)
        self.X2 = sc("X2", [S, D], F32)
        self.GATES = sc("GATES", [128, self.NT, 16], F32)
        self.AFF = sc("AFF", [128, self.NT, NE], F32)
        self.Q1T = sc("Q1T", [1024, S], BF16)
        self.K1T = sc("K1T", [512, S], BF16)
        self.V1 = sc("V1", [S, 256], BF16)
        self.ATT = sc("ATT", [S, 16 * 65], F32)

    def consts(self):
        k = self.k
        self.ident_f = k.sb("ident_f", [128, 128], F32)
        self.ident_b = k.sb("ident_b", [128, 128], BF16)
        self.ones_f = k.sb("ones_f", [128, 128], F32)
        self.ones_b = k.sb("ones_b", [128, 128], BF16)
        self.zeros_b = k.sb("zeros_b", [128, 512], BF16)
        k.I("pool", "memset", ap=self.ones_f[:], constant=1.0)
        k.I("pool", "memset", ap=self.ones_b[:], constant=1.0)
        k.I("pool", "memset", ap=self.zeros_b[:], constant=0.0)
        k.I("pool", "affine_select", out=self.ident_f[:], in_=self.ones_f[:], pattern=[[-1, 128]],
            compare_op=ALU.is_equal, fill=0.0, base=0, channel_multiplier=1)
        k.I("pool", "tensor_copy", out=self.ident_b[:], in_=self.ident_f[:])
        self.eps_norm = k.sb("eps_norm", [128, 1])
        k.I("pool", "memset", ap=self.eps_norm[:], constant=NORM_EPS)
        self.eps_ln = k.sb("eps_ln", [128, 1])
        self.one_col = k.sb("one_col", [128, 1])
        k.I("pool", "memset", ap=self.one_col[:], constant=1.0)
        k.I("pool", "memset", ap=self.eps_ln[:], constant=LN_EPS)

    def tri(self, name, dt, val_true, val_false, base, cm, step, op):
        k = self.k
        t = k.sb(name, [128, 128], dt)
        k.I("pool", "memset", ap=t[:], constant=val_true)
        k.I("pool", "affine_select", out=t[:], in_=t[:], pattern=[[step, 128]],
            compare_op=op, fill=val_false, base=base, channel_multiplier=cm)
        return t

    def ada(self, layer, mod):
        k = self.k
        with k.scope():
            csb = k.sb("csb", [128, 8])
            crep = k.sb("crep", [128, 8, 128])
            brow = k.sb("brow", [1, 6 * D])
            k.dma("sp", csb[:], self.c[:])
            k.dma("sp", brow[:], self.ada_b[layer:layer + 1, :])
            for kc in range(8):
                k.I("act", "activation", out=crep[:, kc, :], in_=self.ones_f[:], func=AF.Silu,
                    scale=csb[:, kc:kc + 1])
            wst = [k.sb("adaw", [128, 8, 512]) for _ in range(2)]
            pp = [k.ps("adaps", [128, 512]) for _ in range(2)]
            for cg in range(12):
                w = wst[cg % 2]
                k.dma("sp" if cg % 2 == 0 else "act", w[:],
                      self.ada_w[layer, :, cg * 512:(cg + 1) * 512].rearrange("(kc p) n -> p kc n", p=128))
                p = pp[cg % 2]
                for kc in range(8):
                    k.mm(p[:], crep[:, kc, :], w[:, kc, :], start=(kc == 0), stop=False)
                k.mm(p[:], self.ones_f[0:1, :], brow[0:1, cg * 512:(cg + 1) * 512], start=False, stop=True)
                k.cp(cg, mod[:, cg * 512:(cg + 1) * 512], p[:])
            for part in (1, 2, 4, 5):
                k.I("dve", "tensor_scalar", out=mod[:, part * D:(part + 1) * D], in0=mod[:, part * D:(part + 1) * D],
                    scalar1=1.0, scalar2=None, op0=ALU.add)
        k.barrier()

    def rope_tables(self):
        k, NT = self.k, self.NT
        self.sin_t = k.sb("sin_t", [128, NT, 8])
        self.cos_t = k.sb("cos_t", [128, NT, 8])
        with k.scope():
            pi = k.sb("posi", [128, NT], I32)
            pf = k.sb("posf", [128, NT])
            ang = k.sb("ang", [128, NT, 8])
            t1 = k.sb("rt1", [128, NT, 8])
            ki = k.sb("rki", [128, NT, 8], I32)
            kf = k.sb("rkf", [128, NT, 8])
            r = k.sb("rr", [128, NT, 8])
            m = k.sb("rm", [128, NT, 8])
            k.dma("sp", pi[:], self.pos[:])
            k.I("dve", "tensor_copy", out=pf[:], in_=pi[:])
            for f in range(8):
                inv = float(np.float32(500000.0) ** np.float32(-(2.0 * f) / 16.0))
                k.I("dve", "tensor_scalar", out=ang[:, :, f], in0=pf[:], scalar1=inv, scalar2=None, op0=ALU.mult)
            TWO_PI = 2.0 * math.pi
            C1 = 6.28125
            C2 = TWO_PI - C1
            k.I("dve", "tensor_scalar", out=t1[:], in0=ang[:], scalar1=1.0 / TWO_PI, scalar2=None, op0=ALU.mult)
            k.I("dve", "tensor_copy", out=ki[:], in_=t1[:])
            k.I("dve", "tensor_copy", out=kf[:], in_=ki[:])
            k.I("dve", "scalar_tensor_tensor", out=r[:], in0=kf[:], scalar=-C1, in1=ang[:], op0=ALU.mult, op1=ALU.add)
            k.I("dve", "scalar_tensor_tensor", out=r[:], in0=kf[:], scalar=-C2, in1=r[:], op0=ALU.mult, op1=ALU.add)

            def fold(rr):
                k.I("dve", "tensor_scalar", out=m[:], in0=rr[:], scalar1=math.pi, scalar2=-TWO_PI, op0=ALU.is_gt, op1=ALU.mult)
                k.I("dve", "tensor_tensor", out=rr[:], in0=rr[:], in1=m[:], op=ALU.add)
                k.I("dve", "tensor_scalar", out=m[:], in0=rr[:], scalar1=-math.pi, scalar2=TWO_PI, op0=ALU.is_lt, op1=ALU.mult)
                k.I("dve", "tensor_tensor", out=rr[:], in0=rr[:], in1=m[:], op=ALU.add)

            fold(r)
            k.I("act", "activation", out=self.sin_t[:], in_=r[:], func=AF.Sin)
            k.I("dve", "tensor_scalar", out=r[:], in0=r[:], scalar1=math.pi / 2, scalar2=None, op0=ALU.add)
            fold(r)
            k.I("act", "activation", out=self.cos_t[:], in_=r[:], func=AF.Sin)
        k.barrier()

    def rope(self, qb, nheads, t, rt=None):
        k = self.k
        v = qb[:, 0:nheads * 64].rearrange("p (h d) -> p h d", d=64)
        x1 = v[:, :, 0:8]
        x2 = v[:, :, 8:16]
        cosb = self.cos_t[:, t, :].unsqueeze(1).broadcast_to([128, nheads, 8])
        sinb = self.sin_t[:, t, :].unsqueeze(1).broadcast_to([128, nheads, 8])
        rt = self.rtmp if rt is None else rt
        ta, tb, tc, td = [rt[i][:, 0:nheads, :] for i in range(4)]
        k.I("dve", "tensor_tensor", out=ta, in0=x1, in1=cosb, op=ALU.mult)
        k.I("pool", "tensor_tensor", out=tb, in0=x2, in1=sinb, op=ALU.mult)
        k.I("dve", "tensor_tensor", out=tc, in0=x2, in1=cosb, op=ALU.mult)
        k.I("pool", "tensor_tensor", out=td, in0=x1, in1=sinb, op=ALU.mult)
        k.I("dve", "tensor_tensor", out=x1, in0=ta, in1=tb, op=ALU.subtract)
        k.I("pool", "tensor_tensor", out=x2, in0=tc, in1=td, op=ALU.add)

    def load_w(self, dst, src, ncols, nkc=8, chunk=1024):
        k = self.k
        with k.scope():
            st = [k.sb("wstage", [128, chunk]) for _ in range(3)]
            i = 0
            for kc in range(nkc):
                for c0 in range(0, ncols, chunk):
                    cw = min(chunk, ncols - c0)
                    s = st[i % 3]
                    k.dma("sp" if i % 2 == 0 else "act", s[:, 0:cw], src[kc * 128:(kc + 1) * 128, c0:c0 + cw])
                    if i % 3 == 2:
                        k.I("pool", "tensor_copy", out=dst[:, kc, c0:c0 + cw], in_=s[:, 0:cw])
                    else:
                        k.cp(i, dst[:, kc, c0:c0 + cw], s[:, 0:cw])
                    i += 1

    def p1_proj0(self, mod):
        k, S, NT = self.k, self.S, self.NT
        MT = 2
        MW = MT * 128
        with k.scope():
            w = k.sb("w_in0", [128, 8, AB_IN], BF16)
            self.load_w(w, self.ab_w_in, AB_IN)
            xs = [k.sb("xs", [128, MT, D]) for _ in range(2)]
            hb = k.sb("hb", [128, MT, D], BF16)
            hTs = [k.sb("hT", [128, 8, MW], BF16) for _ in range(2)]
            qa = k.sb("qa_st", [128, 12, MW], BF16)
            zs = [k.sb("zs", [128, 512], BF16) for _ in range(2)]
            gsb = k.sb("gsb", [128, NT, 16])
            qb = [k.sb("qb", [128, 2304]) for _ in range(3)]
            qbb = [k.sb("qbb", [128, 2304], BF16) for _ in range(2)]
            qkT = [k.sb("qkT", [128, 12, 128], BF16) for _ in range(2)]
            rts = [[k.sb("rtmp", [128, 24, 8]) for _ in range(4)] for _ in range(2)]
            pst = [k.ps("pst", [128, 512], BF16) for _ in range(2)]
            psm = [k.ps("psm", [128, 512]) for _ in range(4)]
            for cc in range(12):
                k.dma("pool", self.QKVA.reg(("padl", cc))[cc * 128:(cc + 1) * 128, 0:2], self.zeros_b[:, 0:2])
                k.dma("pool", self.QKVA.reg(("padr", cc))[cc * 128:(cc + 1) * 128, S + 2:S + 4], self.zeros_b[:, 0:2])
            self._ei = 0
            self._pi = 0

            def nextp():
                p = psm[self._pi % 4]; self._pi += 1
                return p

            def cpy(out, in_):
                k.cp(self._ei, out, in_); self._ei += 1

            def tile(t):
                m, j = t // MT, t % MT
                hT = hTs[m % 2]
                if j == 0:
                    x_ = xs[m % 2]
                    k.dma("sp", x_[:], self.x[m * MW:(m + 1) * MW, :].rearrange("(j p) d -> p j d", p=128))
                    for jj in range(MT):
                        k.I("dve", "tensor_tensor", out=x_[:, jj, :], in0=x_[:, jj, :], in1=mod[:, D:2 * D], op=ALU.mult)
                        k.I("pool", "tensor_tensor", out=hb[:, jj, :], in0=x_[:, jj, :], in1=mod[:, 0:D], op=ALU.add)
                yield
                if j == 0:
                    for kc in range(0, 8, 2):
                        p = pst[(kc // 2) % 2]
                        for k2 in range(2):
                            for jj in range(MT):
                                k.tp(p[:, (k2 * MT + jj) * 128:(k2 * MT + jj + 1) * 128], hb[:, jj, (kc + k2) * 128:(kc + k2 + 1) * 128], self.ident_b[:])
                        cpy(hT[:, kc:kc + 2, :], p[:, 0:2 * MW].rearrange("p (a b) -> p a b", b=MW))
                yield
                if j == 0:
                    for cc in range(12):
                        p = nextp()
                        for kc in range(8):
                            k.mm(p[:, 0:MW], w[:, kc, cc * 128:(cc + 1) * 128], hT[:, kc, :], start=(kc == 0), stop=(kc == 7))
                        cpy(qa[:, cc, :], p[:, 0:MW])
                    k.dma("act", V(self.QKVA.t.rearrange("(cc p) t -> p cc t", p=128)[:, :, 2 + m * MW:2 + (m + 1) * MW],
                                   self.QKVA.reg(("m", m))), qa[:])
                p = nextp()
                for kc in range(8):
                    k.mm(p[:], hT[:, kc, j * 128:(j + 1) * 128], w[:, kc, 1536:2048], start=(kc == 0), stop=(kc == 7))
                z_ = zs[t % 2]
                k.I("act", "activation", out=z_[:], in_=p[:], func=AF.Silu)
                k.dma("sp", self.Z.reg(t)[t * 128:(t + 1) * 128, :], z_[:])
                p = nextp()
                for kc in range(8):
                    k.mm(p[:, 0:16], hT[:, kc, j * 128:(j + 1) * 128], w[:, kc, 2048:2064], start=(kc == 0), stop=(kc == 7))
                k.I("dve", "tensor_copy", out=gsb[:, t, :], in_=p[:, 0:16])
                q_ = qb[t % 3]
                for g in range(5):
                    c0 = 2064 + g * 512
                    cw = min(512, AB_IN - c0)
                    p = nextp()
                    for kc in range(8):
                        k.mm(p[:, 0:cw], hT[:, kc, j * 128:(j + 1) * 128], w[:, kc, c0:c0 + cw], start=(kc == 0), stop=(kc == 7))
                    cpy(q_[:, g * 512:g * 512 + cw], p[:, 0:cw])
                yield
                self.rope(q_, 24, t, rts[t % 2])
                yield
                qq = qbb[t % 2]
                k.I("act", "activation", out=qq[:], in_=q_[:], func=AF.Copy)
                k.dma("sp", self.VB.reg(t)[t * 128:(t + 1) * 128, :], qq[:, 1536:2304])
                yield
                qt = qkT[t % 2]
                for g3 in range(3):
                    p = pst[g3 % 2]
                    for i4 in range(4):
                        cc = g3 * 4 + i4
                        k.tp(p[:, i4 * 128:(i4 + 1) * 128], qq[:, cc * 128:(cc + 1) * 128], self.ident_b[:])
                    cpy(qt[:, g3 * 4:(g3 + 1) * 4, :], p[:].rearrange("p (a b) -> p a b", b=128))
                k.dma("act", V(self.QBT.t.rearrange("(cc p) t -> p cc t", p=128)[:, :, t * 128:(t + 1) * 128], self.QBT.reg(t)),
                      qt[:, 0:6, :])
                k.dma("act", V(self.KBT.t.rearrange("(cc p) t -> p cc t", p=128)[:, :, t * 128:(t + 1) * 128], self.KBT.reg(t)),
                      qt[:, 6:12, :])
                yield

            run_pipelined(NT, tile)
            k.dma("sp", self.GATES[:], gsb[:])
        k.barrier()

    def p2_dnprep(self):
        k, S = self.k, self.S
        NM = S // 512
        with k.scope():
            cw = k.sb("convw", [128, 12, 5])
            k.dma("sp", cw[:], self.ab_conv_w[:])
            dW = k.sb("dW", [128, 12, 5, 128], BF16)
            for cc in range(12):
                for j in range(5):
                    k.I("dve" if (cc + j) % 2 == 0 else "pool", "tensor_scalar", out=dW[:, cc, j, :], in0=self.ident_f[:],
                        scalar1=cw[:, cc, j:j + 1], scalar2=0.0, op0=ALU.mult, op1=ALU.add)
            NB = 3
            xin = [[k.sb("xin", [128, 516], BF16) for _ in range(4)] for _ in range(NB)]
            sl = [[k.sb("csl", [128, 512]) for _ in range(4)] for _ in range(NB)]
            sq = [[k.sb("csq", [128, 512]) for _ in range(4)] for _ in range(2)]
            rn = [[k.sb("crn", [128, 512]) for _ in range(4)] for _ in range(2)]
            ob = [[k.sb("cob", [128, 512], BF16) for _ in range(4)] for _ in range(NB)]
            tk = [k.sb("ctk", [128, 4, 128], BF16) for _ in range(3)]
            psc = [k.ps("p2c", [128, 512]) for _ in range(4)]
            pss = [k.ps("p2s", [128, 512]) for _ in range(2)]
            pst = [k.ps("p2t", [128, 512], BF16) for _ in range(2)]
            self._p2i = 0

            def group(gi):
                m, kind = gi // 3, gi % 3
                b3 = gi % NB; b2 = gi % 2
                for h in range(4):
                    cc = kind * 4 + h
                    xi = xin[b3][h]
                    k.dma("sp" if h % 2 == 0 else "act", xi[:], self.QKVA[cc * 128:(cc + 1) * 128, m * 512:m * 512 + 516])
                    pc = psc[h]
                    for j in range(5):
                        k.mm(pc[:], dW[:, cc, j, :], xi[:, j:j + 512], start=(j == 0), stop=(j == 4))
                    if kind == 2:
                        k.I("act", "activation", out=ob[b3][h][:], in_=pc[:], func=AF.Silu)
                    else:
                        k.I("act", "activation", out=sl[b3][h][:], in_=pc[:], func=AF.Silu)
                        k.I("pool", "tensor_tensor", out=sq[b2][h][:], in0=sl[b3][h][:], in1=sl[b3][h][:], op=ALU.mult)
                yield
                if kind != 2:
                    for h in range(4):
                        p = pss[h % 2]
                        k.mm(p[:], self.ones_f[:], sq[b2][h][:])
                        k.I("act", "activation", out=rn[b2][h][:], in_=p[:], func=AF.Ln, bias=self.eps_norm[:, 0:1])
                    for h in range(4):
                        k.I("act", "activation", out=rn[b2][h][:], in_=rn[b2][h][:], func=AF.Exp, scale=-0.5)
                yield
                for h in range(4):
                    o_ = ob[b3][h]
                    if kind == 0:
                        k.I("dve", "scalar_tensor_tensor", out=o_[:], in0=sl[b3][h][:], scalar=float(128 ** -0.5), in1=rn[b2][h][:],
                            op0=ALU.mult, op1=ALU.mult)
                    elif kind == 1:
                        k.I("dve", "tensor_tensor", out=o_[:], in0=sl[b3][h][:], in1=rn[b2][h][:], op=ALU.mult)
                    if kind != 2:
                        dst = self.QAT if kind == 0 else self.KAT
                        k.dma("act", dst.reg((m, h))[h * 128:(h + 1) * 128, m * 512:(m + 1) * 512], o_[:])
                    if kind >= 1:
                        i = self._p2i; self._p2i += 1
                        p = pst[i % 2]
                        for j in range(4):
                            k.tp(p[:, j * 128:(j + 1) * 128], o_[:, j * 128:(j + 1) * 128], self.ident_b[:])
                        t_ = tk[i % 3]
                        k.I("dve", "tensor_copy", out=t_[:], in_=p[:].rearrange("p (j d) -> p j d", d=128))
                        dst = self.KA if kind == 1 else self.VA
                        k.dma("sp", V(dst.t[m * 512:(m + 1) * 512, h * 128:(h + 1) * 128].rearrange("(j p) d -> p j d", p=128),
                                      dst.reg((m, h))), t_[:])
                yield

            run_pipelined(NM * 3, group, 3)

    def p3_deltanet(self):
        k, S, NT = self.k, self.S, self.NT
        with k.scope():
            UC = [self.tri("UCf", F32, 1.0, 0.0, 0, -1, 1, ALU.is_ge),
                  self.tri("UCb", F32, 1.0, 0.0, 0, 1, -1, ALU.is_ge)]
            NUC = [self.tri("NUCf", F32, -1.0, 0.0, 0, -1, 1, ALU.is_ge),
                   self.tri("NUCb", F32, -1.0, 0.0, 0, 1, -1, ALU.is_ge)]
            NM1 = [self.tri("NM1f", F32, 0.0, -BIG, 0, 1, -1, ALU.is_ge),
                   self.tri("NM1b", F32, 0.0, -BIG, 0, -1, 1, ALU.is_ge)]
            NM2 = [NM1[1], NM1[0]]
            NOTI = self.tri("NOTI", F32, 1.0, 0.0, 0, 1, -1, ALU.not_equal)
            nones_f = k.sb("nones_f", [128, 128])
            k.I("pool", "memset", ap=nones_f[:], constant=-1.0)
            gsb = k.sb("gsb3", [128, NT, 16])
            k.dma("sp", gsb[:], self.GATES[:])
            al = k.sb("alog", [128, 8]); dtb = k.sb("dtb", [128, 8]); nA = k.sb("nA", [128, 8])
            k.dma("sp", al[:], self.ab_a_log[0:1, :].broadcast_to([128, 8]))
            k.dma("sp", dtb[:], self.ab_dt_bias[0:1, :].broadcast_to([128, 8]))
            k.I("act", "activation", out=nA[:], in_=al[:], func=AF.Exp)
            k.I("dve", "tensor_scalar", out=nA[:], in0=nA[:], scalar1=-1.0, scalar2=None, op0=ALU.mult)
            xg = k.sb("xg", [128, NT, 8]); ax = k.sb("axg", [128, NT, 8]); mx = k.sb("mxg", [128, NT, 8])
            gall = k.sb("gall", [128, NT, 8])
            beta = k.sb("beta", [128, NT, 8]); nbeta = k.sb("nbeta", [128, NT, 8])
            k.I("dve", "tensor_tensor", out=xg[:], in0=gsb[:, :, 0:8], in1=dtb[:].unsqueeze(1).broadcast_to([128, NT, 8]), op=ALU.add)
            k.I("act", "activation", out=ax[:], in_=xg[:], func=AF.Abs)
            k.I("act", "activation", out=ax[:], in_=ax[:], func=AF.Exp, scale=-1.0)
            k.I("act", "activation", out=ax[:], in_=ax[:], func=AF.Ln, bias=self.one_col[:, 0:1])
            k.I("dve", "tensor_scalar", out=mx[:], in0=xg[:], scalar1=0.0, scalar2=None, op0=ALU.max)
            k.I("dve", "tensor_tensor", out=mx[:], in0=mx[:], in1=ax[:], op=ALU.add)
            k.I("dve", "tensor_tensor", out=gall[:], in0=mx[:], in1=nA[:].unsqueeze(1).broadcast_to([128, NT, 8]), op=ALU.mult)
            k.I("act", "activation", out=beta[:], in_=gsb[:, :, 8:16], func=AF.Sigmoid)
            k.I("dve", "tensor_scalar", out=nbeta[:], in0=beta[:], scalar1=-1.0, scalar2=None, op0=ALU.mult)
            gd, gc, egc, bw, kd, egl, bet, nbet = [], [], [], [], [], [], [], []
            pz = k.ps("pz", [128, 512])
            for d in range(2):
                g_ = k.sb("gd", [128, NT, 4]); gc_ = k.sb("gc", [128, NT, 4]); e_ = k.sb("egc", [128, NT, 4])
                bw_ = k.sb("bw", [128, NT, 4]); kd_ = k.sb("kd", [128, NT, 4]); gl_ = k.sb("egl", [128, NT, 4])
                b_ = k.sb("bet", [128, NT, 4]); nb_ = k.sb("nbet", [128, NT, 4])
                k.I("dve", "tensor_copy", out=g_[:], in_=gall[:, :, d * 4:(d + 1) * 4])
                k.I("dve", "tensor_copy", out=b_[:], in_=beta[:, :, d * 4:(d + 1) * 4])
                k.I("dve", "tensor_copy", out=nb_[:], in_=nbeta[:, :, d * 4:(d + 1) * 4])
                for c0 in range(0, NT, 128):
                    cw = min(128, NT - c0)
                    gv = g_[:, c0:c0 + cw, :].rearrange("p a b -> p (a b)")
                    k.mm(pz[:, 0:cw * 4], UC[d][:], gv)
                    k.I("dve", "tensor_copy", out=gc_[:, c0:c0 + cw, :].rearrange("p a b -> p (a b)"), in_=pz[:, 0:cw * 4])
                    k.mm(pz[:, 0:cw * 4], self.ones_f[:], gv)
                    k.I("dve", "tensor_copy", out=gl_[:, c0:c0 + cw, :].rearrange("p a b -> p (a b)"), in_=pz[:, 0:cw * 4])
                k.I("act", "activation", out=e_[:], in_=gc_[:], func=AF.Exp)
                k.I("dve", "tensor_tensor", out=bw_[:], in0=e_[:], in1=b_[:], op=ALU.mult)
                k.I("dve", "tensor_tensor", out=kd_[:], in0=gl_[:], in1=gc_[:], op=ALU.subtract)
                k.I("act", "activation", out=kd_[:], in_=kd_[:], func=AF.Exp)
                k.I("act", "activation", out=gl_[:], in_=gl_[:], func=AF.Exp)
                gd.append(g_); gc.append(gc_); egc.append(e_); bw.append(bw_); kd.append(kd_); egl.append(gl_)
                bet.append(b_); nbet.append(nb_)
            def mk(name, shape, dt, n=1):
                return [[k.sb(name, shape, dt) for _ in range(n)] for _ in range(2)]
            kT4 = mk("kT4", [128, 4, 128], BF16, 2); qT4 = mk("qT4", [128, 4, 128], BF16, 3)
            ktok = mk("ktok", [128, 4, 128], BF16, 2); vtok = mk("vtok", [128, 4, 128], BF16, 2)
            GB4 = mk("GB4", [128, 4, 128], F32, 2); UCg4 = mk("UCg4", [128, 4, 128], F32, 2)
            E4 = mk("E4", [128, 4, 128], F32, 2); ET4 = mk("ET4", [128, 4, 128], F32, 2)
            Mk = mk("Mk", [128, 4, 128], BF16, 4); Mtk = mk("Mtk", [128, 4, 128], BF16, 4)
            X4 = mk("X4", [128, 4, 128], BF16, 4)
            rv4 = mk("rv4", [128, 4, 128], BF16, 2); rw4 = mk("rw4", [128, 4, 128], BF16, 2); kdec4 = mk("kdec4", [128, 4, 128], BF16, 3)
            u4 = mk("u4", [128, 4, 128], F32, 3); wT4 = mk("wT4", [128, 4, 128], BF16, 3); AT4 = mk("AT4", [128, 4, 128], BF16, 3)
            vn4 = mk("vn4", [128, 4, 128], BF16)
            S32 = mk("S32", [128, 4, 128], F32); Sb = mk("Sb", [128, 4, 128], BF16)
            o2 = mk("o2", [128, 4, 128], F32); o4 = mk("o4", [128, 4, 128], F32, 2)
            banks = [k.ps("dnps", [128, 512]) for _ in range(7)]
            self._bi = 0

            def bank():
                b = banks[self._bi % len(banks)]
                self._bi += 1
                return b

            def v3(b):
                return b[:].rearrange("p (h f) -> p h f", f=128)

            def v3b(b):
                return b[:].bitcast(BF16)[:, 0:512].rearrange("p (h f) -> p h f", f=128)

            for d in range(2):
                k.I("pool", "memset", ap=S32[d][0][:], constant=0.0)
                k.I("pool", "memset", ap=Sb[d][0][:], constant=0.0)

            def bc_f(t, c):
                return t[:, c, :].unsqueeze(2).broadcast_to([128, 4, 128])

            def gen_prep(s):
                b = s % 3
                b2 = s % 2
                cs = [s, NT - 1 - s]
                for d in range(2):
                    c = cs[d]
                    sl = slice(c * 128, (c + 1) * 128)
                    k.dma("sp", kT4[d][b2][:], V(self.KAT.t.rearrange("(h q) t -> q h t", q=128)[:, :, sl], self.KAT))
                    k.dma("sp", qT4[d][b][:], V(self.QAT.t.rearrange("(h q) t -> q h t", q=128)[:, :, sl], self.QAT))
                    k.dma("act", ktok[d][b2][:].rearrange("p h f -> p (h f)"), self.KA[sl, :])
                    k.dma("act", vtok[d][b2][:].rearrange("p h f -> p (h f)"), self.VA[sl, :])
                yield
                for d in range(2):
                    c = cs[d]
                    k.I("pool", "tensor_tensor", out=GB4[d][b2][:], in0=self.ident_f[:].unsqueeze(1).broadcast_to([128, 4, 128]),
                        in1=bc_f(gc[d], c), op=ALU.mult)
                yield
                pA = [bank(), bank()]
                for d in range(2):
                    for h in range(4):
                        k.mm(v3(pA[d])[:, h, :], self.ones_f[:], GB4[d][b2][:, h, :])
                    k.I("dve", "tensor_tensor", out=UCg4[d][b2][:], in0=v3(pA[d]), in1=bc_f(gc[d], cs[d]), op=ALU.subtract)
                yield
                for d in range(2):
                    k.I("dve", "scalar_tensor_tensor", out=E4[d][b2][:], in0=UCg4[d][b2][:], scalar=-1.0,
                        in1=NM1[d][:].unsqueeze(1).broadcast_to([128, 4, 128]), op0=ALU.mult, op1=ALU.add)
                    k.I("pool", "tensor_tensor", out=ET4[d][b2][:], in0=UCg4[d][b2][:],
                        in1=NM2[d][:].unsqueeze(1).broadcast_to([128, 4, 128]), op=ALU.add)
                yield
                for d in range(2):
                    k.I("act", "activation", out=E4[d][b2][:], in_=E4[d][b2][:], func=AF.Exp)
                    k.I("act", "activation", out=ET4[d][b2][:], in_=ET4[d][b2][:], func=AF.Exp)
                yield
                for d in range(2):
                    c = cs[d]
                    kt = kT4[d][b2]; qt = qT4[d][b]
                    pg = bank()
                    for h in range(4):
                        k.mm(v3(pg)[:, h, :], kt[:, h, :], kt[:, h, :])
                    pa = bank()
                    for h in range(4):
                        k.mm(v3(pa)[:, h, :], kt[:, h, :], qt[:, h, :])
                    k.I("dve", "tensor_tensor", out=AT4[d][b][:], in0=v3(pa), in1=ET4[d][b2][:], op=ALU.mult)
                    k.I("pool", "tensor_tensor", out=E4[d][b2][:], in0=E4[d][b2][:], in1=NOTI[:].unsqueeze(1).broadcast_to([128, 4, 128]), op=ALU.mult)
                    k.I("pool", "tensor_tensor", out=E4[d][b2][:], in0=E4[d][b2][:], in1=bc_f(nbet[d], c), op=ALU.mult)
                    k.I("dve", "tensor_tensor", out=Mk[d][2 * b2][:], in0=v3(pg), in1=E4[d][b2][:], op=ALU.mult)
                    pt = bank()
                    for h in range(4):
                        k.tp(v3b(pt)[:, h, :], Mk[d][2 * b2][:, h, :], self.ident_b[:])
                    k.I("act", "activation", out=Mtk[d][2 * b2][:], in_=v3b(pt), func=AF.Copy)
                    k.I("pool", "tensor_tensor", out=X4[d][2 * b2][:], in0=Mtk[d][2 * b2][:], in1=self.ident_b[:].unsqueeze(1).broadcast_to([128, 4, 128]), op=ALU.add)
                    for h in range(4):
                        k.I("act", "activation", out=rv4[d][b2][:, h, :], in_=vtok[d][b2][:, h, :], func=AF.Copy, scale=bet[d][:, c, h:h + 1])
                        k.I("act", "activation", out=rw4[d][b2][:, h, :], in_=ktok[d][b2][:, h, :], func=AF.Copy, scale=bw[d][:, c, h:h + 1])
                        k.I("act", "activation", out=kdec4[d][b][:, h, :], in_=ktok[d][b2][:, h, :], func=AF.Copy, scale=kd[d][:, c, h:h + 1])
                yield
                cur = 0
                for lvl in range(1, 7):
                    nxt = 1 - cur
                    for d in range(2):
                        pm = bank()
                        for h in range(4):
                            k.mm(v3(pm)[:, h, :], Mtk[d][2 * b2 + cur][:, h, :], Mk[d][2 * b2 + cur][:, h, :])
                        k.I("act", "activation", out=Mk[d][2 * b2 + nxt][:], in_=v3(pm), func=AF.Copy)
                        if lvl < 6:
                            pmt = bank()
                            for h in range(4):
                                k.mm(v3(pmt)[:, h, :], Mk[d][2 * b2 + cur][:, h, :], Mtk[d][2 * b2 + cur][:, h, :])
                            k.I("dve", "tensor_copy", out=Mtk[d][2 * b2 + nxt][:], in_=v3(pmt))
                    yield
                    for d in range(2):
                        px = bank()
                        for h in range(4):
                            k.mm(v3(px)[:, h, :], Mk[d][2 * b2 + nxt][:, h, :], X4[d][2 * b2 + cur][:, h, :])
                        k.I("dve", "tensor_tensor", out=X4[d][2 * b2 + nxt][:], in0=v3(px), in1=X4[d][2 * b2 + cur][:], op=ALU.add)
                    cur = nxt
                    yield
                for d in range(2):
                    TT = X4[d][2 * b2 + cur]
                    pu = bank()
                    for h in range(4):
                        k.mm(v3(pu)[:, h, :], TT[:, h, :], rv4[d][b2][:, h, :])
                    k.I("act", "activation", out=u4[d][b][:], in_=v3(pu), func=AF.Copy)
                    pw = bank()
                    for h in range(4):
                        k.mm(v3(pw)[:, h, :], rw4[d][b2][:, h, :], TT[:, h, :])
                    k.I("dve", "tensor_copy", out=wT4[d][b][:], in_=v3(pw))
                yield

            def gen_scan(s):
                b = s % 3
                cs = [s, NT - 1 - s]
                for d in range(2):
                    pws = bank()
                    for h in range(4):
                        k.mm(v3(pws)[:, h, :], wT4[d][b][:, h, :], Sb[d][0][:, h, :])
                    k.I("dve", "tensor_tensor", out=vn4[d][0][:], in0=u4[d][b][:], in1=v3(pws), op=ALU.subtract)
                yield
                for d in range(2):
                    c = cs[d]
                    po1 = bank()
                    for h in range(4):
                        k.mm(v3(po1)[:, h, :], qT4[d][b][:, h, :], Sb[d][0][:, h, :])
                    po2 = bank()
                    for h in range(4):
                        k.mm(v3(po2)[:, h, :], AT4[d][b][:, h, :], vn4[d][0][:, h, :])
                    pds = bank()
                    for h in range(4):
                        k.mm(v3(pds)[:, h, :], kdec4[d][b][:, h, :], vn4[d][0][:, h, :])
                    k.I("pool", "tensor_tensor", out=S32[d][0][:], in0=S32[d][0][:], in1=bc_f(egl[d], c), op=ALU.mult)
                    k.I("dve", "tensor_tensor", out=Sb[d][0][:], in0=S32[d][0][:], in1=v3(pds), op=ALU.add)
                    k.I("dve", "tensor_tensor", out=S32[d][0][:], in0=S32[d][0][:], in1=v3(pds), op=ALU.add)
                    oo = o4[d][s % 2]
                    k.I("act", "activation", out=o2[d][0][:], in_=v3(po2), func=AF.Copy)
                    k.I("dve", "tensor_tensor", out=oo[:], in0=v3(po1), in1=bc_f(egc[d], c), op=ALU.mult)
                    k.I("pool", "tensor_tensor", out=oo[:], in0=oo[:], in1=o2[d][0][:], op=ALU.add)
                    dst = self.OF if d == 0 else self.OB
                    k.dma("sp", dst.reg(c)[c * 128:(c + 1) * 128, :], oo[:].rearrange("p h f -> p (h f)"))
                    yield

            def drive():
                from collections import deque
                preps = deque()
                next_p = 0
                prep_done = -1
                finished = set()
                cur = 0
                gx = None
                rnd = 0
                while cur < NT:
                    while len(preps) < 2 and next_p < NT and next_p - 3 < cur:
                        preps.append((next_p, gen_prep(next_p)))
                        next_p += 1
                    for item in list(preps):
                        sp, g = item
                        try:
                            next(g)
                        except StopIteration:
                            preps.remove(item)
                            finished.add(sp)
                    while (prep_done + 1) in finished:
                        prep_done += 1
                    if gx is None and cur <= prep_done:
                        gx = gen_scan(cur)
                    if gx is not None and rnd % 2 == 0:
                        try:
                            next(gx)
                        except StopIteration:
                            gx = None
                            cur += 1
                    rnd += 1

            drive()
        k.barrier()


    def attn_masks(self, half):
        k = self.k
        NEGM = -30000.0
        prev = self.tri("mprev", BF16, 0.0, NEGM, -(128 - half), 1, -1, ALU.is_ge)
        nxt = self.tri("mnext", BF16, 0.0, NEGM, -(128 - half), -1, 1, ALU.is_ge)
        own = None
        if half < 128:
            own = self.tri("mown", BF16, 0.0, NEGM, half, 1, -1, ALU.is_ge)
            k.I("pool", "affine_select", out=own[:], in_=own[:], pattern=[[1, 128]], compare_op=ALU.is_ge,
                fill=NEGM, base=half, channel_multiplier=-1)
        return prev, own, nxt

    def attn(self, QT, KT, VD, heads, nqc, nkc, vcols, vc0, dil, masks, OUT, qc0=0, kc0=0):
        k, S = self.k, self.S
        T = S // dil
        NA = T // 128
        W = 128 * dil
        nh = len(heads)
        nvh = vcols // 64
        mprev, mown, mnext = masks
        with k.scope():
            qG = [k.sb("qG", [128, nqc, W], BF16) for _ in range(2)]
            kG = [k.sb("kG", [128, nkc, W], BF16) for _ in range(4)]
            vG = [k.sb("vG", [128, dil, nvh, 65], BF16) for _ in range(4)]
            for v_ in vG:
                k.I("pool", "memset", ap=v_[:], constant=1.0)
            ost = [k.sb("ost", [128, dil, nh * 65]) for _ in range(2)]
            PT = [k.sb("PT", [128, 3, 128], BF16) for _ in range(3)]
            pss = [k.ps("aps", [128, 512]) for _ in range(3)]
            pso = [k.ps("apo", [128, 512]) for _ in range(2)]
            QTv = QT.t.rearrange("(c p) t -> p c t", p=128)
            KTv = KT.t.rearrange("(c p) t -> p c t", p=128)

            def load_kv(a):
                sl = slice(a * W, (a + 1) * W)
                k.dma("sp", kG[a % 4][:], V(KTv[:, kc0:kc0 + nkc, sl], KT))
                k.dma("act", vG[a % 4][:, :, :, 0:64],
                      V(VD.t[sl, vc0:vc0 + vcols].rearrange("(j r) (h d) -> j r h d", r=dil, d=64), VD))

            load_kv(0)
            units = [(a, r, hi) for a in range(NA) for r in range(dil) for hi in range(nh)]

            def unit(u):
                a, r, hi = units[u]
                qc, kc, base, vh = heads[hi]
                sl = slice(a * W, (a + 1) * W)
                q_ = qG[a % 2]
                o_ = ost[a % 2]
                if r == 0 and hi == 0:
                    if a + 1 < NA:
                        load_kv(a + 1)
                    k.dma("sp", q_[:], V(QTv[:, qc0:qc0 + nqc, sl], QT))
                kbs = [kb for kb in (a - 1, a, a + 1) if 0 <= kb < NA]
                nk = len(kbs)
                ps = pss[u % 3]; pt = PT[u % 3]
                po = pso[(u // 4) % 2]
                pv = ps[:, 0:384].rearrange("p (a b) -> p a b", b=128)
                bs = slice(base, base + 64)
                for ki, kb in enumerate(kbs):
                    m_ = mprev if kb == a - 1 else (mown if kb == a else mnext)
                    k.mm(pv[:, ki, :], kG[kb % 4][bs, kc, r:W:dil], q_[bs, qc, r:W:dil], start=True, stop=(m_ is None))
                    if m_ is not None:
                        k.mm(pv[:, ki, :], self.ident_b[:], m_[:], start=False, stop=True)
                yield
                k.I("act", "activation", out=pt[:, 0:nk, :], in_=pv[:, 0:nk, :], func=AF.Exp, scale=0.125)
                yield
                for ki, kb in enumerate(kbs):
                    k.mm(po[:, (hi % 4) * 65:(hi % 4 + 1) * 65], pt[:, ki, :], vG[kb % 4][:, r, vh, :], start=(ki == 0), stop=(ki == nk - 1))
                if hi % 4 == 3:
                    k.I("dve", "tensor_copy", out=o_[:, r, (hi - 3) * 65:(hi + 1) * 65], in_=po[:, 0:260])
                    if r == dil - 1 and hi == nh - 1:
                        k.dma("act", V(OUT.t[sl, :].rearrange("(i r) c -> i r c", r=dil), OUT.reg(a)), o_[:])
                yield

            run_pipelined(len(units), unit)

    def p4_dilated(self):
        with self.k.scope():
            masks = self.attn_masks(64)
            for gi, dil in enumerate((1, 4, 16)):
                heads = [(h // 2, h // 2, (h % 2) * 64, h) for h in range(4)]
                OUT = Buf(self.DIL.t[gi], "DIL%d" % gi)
                self.attn(self.QBT, self.KBT, self.VB, heads, 2, 2, 256, gi * 256, dil, masks, OUT, qc0=gi * 2, kc0=gi * 2)

    def ln_setup(self, g_dram, b_dram, layer):
        k = self.k
        g = k.sb("lng", [128, D]); b = k.sb("lnb", [128, D])
        k.dma("sp", g[:], g_dram[layer:layer + 1, :].broadcast_to([128, D]))
        k.dma("sp", b[:], b_dram[layer:layer + 1, :].broadcast_to([128, D]))
        return g, b

    def ln_stats(self, tin, tmp):
        k = self.k
        st = tmp["st"]; mv = tmp["mv"]; rs = tmp["rs"]; nb = tmp["nb"]
        for hh in range(2):
            k.I("dve", "bn_stats", out=st[:, hh, :], in_=tin[:, hh * 512:(hh + 1) * 512])
        k.I("dve", "bn_aggr", out=mv[:], in_=st[:].rearrange("p a b -> p (a b)"))
        k.I("act", "activation", out=rs[:], in_=mv[:, 1:2], func=AF.Ln, bias=self.eps_ln[:, 0:1])
        k.I("act", "activation", out=rs[:], in_=rs[:], func=AF.Exp, scale=-0.5)
        k.I("dve", "scalar_tensor_tensor", out=nb[:], in0=mv[:, 0:1], scalar=-1.0, in1=rs[:], op0=ALU.mult, op1=ALU.mult)

    def ln_apply(self, tin, out, g, b, tmp):
        k = self.k
        rs = tmp["rs"]; nb = tmp["nb"]
        k.I("act", "activation", out=tin[:], in_=tin[:], func=AF.Identity, scale=rs[:, 0:1], bias=nb[:, 0:1])
        k.I("pool", "tensor_tensor", out=tin[:], in0=tin[:], in1=g[:], op=ALU.mult)
        k.I("dve", "tensor_tensor", out=out, in0=tin[:], in1=b[:], op=ALU.add)

    def ln_tile(self, tin, out, g, b, tmp):
        self.ln_stats(tin, tmp)
        self.ln_apply(tin, out, g, b, tmp)

    def ln_tmp(self):
        k = self.k
        return {"st": k.sb("lnst", [128, 2, 6]), "mv": k.sb("lnmv", [128, 2]), "rs": k.sb("lnrs", [128, 1]), "nb": k.sb("lnnb", [128, 1])}

    def mixer_finish(self, layer, mod, xin, w_out_dram, nrows, build_cat, cat_stages):
        k, S, NT = self.k, self.S, self.NT
        ncc = nrows // 128
        with k.scope():
            w = k.sb("w_out", [128, ncc, D], BF16)
            self.load_w(w, w_out_dram, D, nkc=ncc)
            g, b = self.ln_setup(self.ln_mix_g, self.ln_mix_b, layer)
            NTM = 6
            tmps = [self.ln_tmp() for _ in range(NTM)]
            NCAT = cat_stages + 3
            cat = [k.sb("cat", [128, nrows], BF16) for _ in range(NCAT)]
            catT = [k.sb("catT", [128, ncc, 128], BF16) for _ in range(3)]
            xt = [k.sb("xt5", [128, D]) for _ in range(4)]
            NT1 = 8
            t1 = [k.sb("t15", [128, D]) for _ in range(NT1)]
            h2 = [k.sb("h25", [128, D], BF16) for _ in range(3)]
            h2f = [k.sb("h2f", [128, D]) for _ in range(3)]
            h2T = [k.sb("h2T", [128, 8, 128]) for _ in range(2)]
            rw = k.sb("rw", [128, 8, NE])
            k.dma("sp", rw[:], self.router_w[layer].rearrange("(kc p) e -> p kc e", p=128))
            affs = k.sb("affs", [128, NT, NE])
            lgs = [k.sb("lg", [128, NE]) for _ in range(4)]
            mxs = [k.sb("mxr", [128, 1]) for _ in range(4)]
            sms = [k.sb("smr", [128, 1]) for _ in range(4)]
            pst = [k.ps("p5t", [128, 512], BF16) for _ in range(2)]
            psy = [k.ps("p5y", [128, 512]) for _ in range(4)]
            psr = [k.ps("p5r", [128, 512]) for _ in range(2)]

            def tile(t):
                sl = slice(t * 128, (t + 1) * 128)
                c_ = cat[t % NCAT]
                yield from build_cat(t, c_)
                yield
                cT = catT[t % 3]
                ngrp = (ncc + 3) // 4
                for g0 in range(0, ncc, 4):
                    n = min(4, ncc - g0)
                    p = pst[(g0 // 4) % 2]
                    for i in range(n):
                        k.tp(p[:, i * 128:(i + 1) * 128], c_[:, (g0 + i) * 128:(g0 + i + 1) * 128], self.ident_b[:])
                    k.cp(g0 // 4, cT[:, g0:g0 + n, :], p[:, 0:n * 128].rearrange("p (a b) -> p a b", b=128))
                x_ = xt[t % 4]
                k.dma("sp", x_[:], xin[sl, :])
                yield
                t_ = t1[t % NT1]
                pp = [psy[(t * 2 + hh) % 4] for hh in range(2)]
                for hh in range(2):
                    for cc in range(ncc):
                        k.mm(pp[hh][:], cT[:, cc, :], w[:, cc, hh * 512:(hh + 1) * 512], start=(cc == 0), stop=(cc == ncc - 1))
                yield
                for hh in range(2):
                    k.I("dve", "tensor_tensor", out=t_[:, hh * 512:(hh + 1) * 512], in0=pp[hh][:], in1=mod[:, 2 * D + hh * 512:2 * D + (hh + 1) * 512], op=ALU.mult)
                k.I("dve", "scalar_tensor_tensor", out=t_[:], in0=x_[:], scalar=float(ALPHA), in1=t_[:], op0=ALU.mult, op1=ALU.add)
                tm = tmps[t % NTM]
                st = tm["st"]; mv = tm["mv"]; rs = tm["rs"]; nb = tm["nb"]
                for hh in range(2):
                    k.I("dve", "bn_stats", out=st[:, hh, :], in_=t_[:, hh * 512:(hh + 1) * 512])
                k.I("dve", "bn_aggr", out=mv[:], in_=st[:].rearrange("p a b -> p (a b)"))
                yield
                k.I("act", "activation", out=rs[:], in_=mv[:, 1:2], func=AF.Ln, bias=self.eps_ln[:, 0:1])
                k.I("act", "activation", out=rs[:], in_=rs[:], func=AF.Exp, scale=-0.5)
                yield
                k.I("dve", "scalar_tensor_tensor", out=nb[:], in0=mv[:, 0:1], scalar=-1.0, in1=rs[:], op0=ALU.mult, op1=ALU.mult)
                yield
                k.I("act", "activation", out=t_[:], in_=t_[:], func=AF.Identity, scale=rs[:, 0:1], bias=nb[:, 0:1])
                yield
                k.I("pool", "tensor_tensor", out=t_[:], in0=t_[:], in1=g[:], op=ALU.mult)
                yield
                k.I("dve", "tensor_tensor", out=t_[:], in0=t_[:], in1=b[:], op=ALU.add)
                k.dma("sp", self.X1.reg(t)[sl, :], t_[:])
                yield
                hf = h2f[t % 3]
                k.I("pool", "tensor_tensor", out=hf[:], in0=t_[:], in1=mod[:, 4 * D:5 * D], op=ALU.mult)
                yield
                k.I("dve", "tensor_tensor", out=hf[:], in0=hf[:], in1=mod[:, 3 * D:4 * D], op=ALU.add)
                yield
                k.I("act", "activation", out=h2[t % 3][:], in_=hf[:], func=AF.Copy)
                k.dma("act", self.H2.reg(t)[sl, :], h2[t % 3][:])
                hT_ = h2T[t % 2]
                for g0 in range(2):
                    p = psr[g0]
                    for i in range(4):
                        kc = g0 * 4 + i
                        k.tp(p[:, i * 128:(i + 1) * 128], hf[:, kc * 128:(kc + 1) * 128], self.ident_f[:])
                    k.cp(g0, hT_[:, g0 * 4:(g0 + 1) * 4, :], p[:].rearrange("p (a b) -> p a b", b=128))
                yield
                p = psr[t % 2]
                for kc in range(8):
                    k.mm(p[:, 0:NE], hT_[:, kc, :], rw[:, kc, :], start=(kc == 0), stop=(kc == 7))
                lg = lgs[t % 4]; mxr = mxs[t % 4]; smr = sms[t % 4]
                k.I("dve", "tensor_copy", out=lg[:], in_=p[:, 0:NE])
                k.I("dve", "tensor_reduce", out=mxr[:], in_=lg[:], axis=AX.X, op=ALU.max, negate=True)
                yield
                k.I("act", "activation", out=lg[:], in_=lg[:], func=AF.Exp, bias=mxr[:, 0:1], accum_out=smr[:])
                yield
                k.I("dve", "reciprocal", out=smr[:], in_=smr[:])
                k.I("dve", "tensor_scalar", out=affs[:, t, :], in0=lg[:], scalar1=smr[:, 0:1], scalar2=None, op0=ALU.mult)
                yield

            run_pipelined(NT, tile, cat_stages + 17)
            k.dma("sp", self.AFF[:], affs[:])

    def p5_finish0(self, mod):
        k = self.k
        with k.scope():
            dl = [k.sb("dl", [128, 3, 260]) for _ in range(4)]
            of = [k.sb("of", [128, 512]) for _ in range(7)]
            ob = [k.sb("ob5", [128, 512]) for _ in range(3)]
            zz = [k.sb("zz", [128, 512], BF16) for _ in range(7)]
            sqs = [k.sb("sq5", [128, 512]) for _ in range(3)]
            mss = [k.sb("ms5", [128, 4]) for _ in range(5)]
            rds = [k.sb("rd5", [128, 4]) for _ in range(3)]
            dnn = k.sb("dnn", [128, 128])
            k.dma("sp", dnn[:], self.ab_dn_norm[0:1, :].broadcast_to([128, 128]))

            def build_cat(t, c_):
                sl = slice(t * 128, (t + 1) * 128)
                d_ = dl[t % 4]; f_ = of[t % 7]; b_ = ob[t % 3]; z_ = zz[t % 7]
                sq = sqs[t % 3]; ms = mss[t % 5]; rd = rds[t % 3]
                k.dma("sp", d_[:], V(self.DIL.t[:, sl, :].rearrange("g p c -> p g c"), self.DIL))
                k.dma("act", f_[:], self.OF[sl, :])
                k.dma("act", b_[:], self.OB[sl, :])
                k.dma("sp", z_[:], self.Z[sl, :])
                yield
                k.I("pool", "tensor_tensor", out=d_[:, 0, :], in0=d_[:, 0, :], in1=d_[:, 1, :], op=ALU.add)
                k.I("pool", "tensor_tensor", out=d_[:, 0, :], in0=d_[:, 0, :], in1=d_[:, 2, :], op=ALU.add)
                k.I("dve", "tensor_tensor", out=f_[:], in0=f_[:], in1=b_[:], op=ALU.add)
                yield
                dv = d_[:, 0, :].rearrange("p (h e) -> p h e", e=65)
                k.I("dve", "reciprocal", out=rd[:], in_=dv[:, :, 64])
                k.I("dve", "tensor_tensor", out=c_[:, 512:768].rearrange("p (h e) -> p h e", e=64), in0=dv[:, :, 0:64],
                    in1=rd[:].unsqueeze(2).broadcast_to([128, 4, 64]), op=ALU.mult)
                k.I("pool", "tensor_tensor", out=sq[:], in0=f_[:], in1=f_[:], op=ALU.mult)
                yield
                k.I("dve", "tensor_reduce", out=ms[:], in_=sq[:].rearrange("p (h e) -> p h e", e=128), axis=AX.X, op=ALU.add)
                yield
                k.I("act", "activation", out=ms[:], in_=ms[:], func=AF.Ln, scale=1.0 / 128.0, bias=self.eps_norm[:, 0:1])
                k.I("act", "activation", out=ms[:], in_=ms[:], func=AF.Exp, scale=-0.5)
                yield
                fv = f_[:].rearrange("p (h e) -> p h e", e=128)
                k.I("dve", "tensor_tensor", out=fv, in0=fv, in1=ms[:].unsqueeze(2).broadcast_to([128, 4, 128]), op=ALU.mult)
                k.I("dve", "tensor_tensor", out=fv, in0=fv, in1=dnn[:].unsqueeze(1).broadcast_to([128, 4, 128]), op=ALU.mult)
                k.I("dve", "tensor_tensor", out=c_[:, 0:512], in0=f_[:], in1=z_[:], op=ALU.mult)

            self.mixer_finish(0, mod, self.x, self.ab_w_out, 768, build_cat, 5)

    def p5_finish1(self, mod):
        k = self.k
        with k.scope():
            at = [k.sb("at5", [128, 16, 65]) for _ in range(3)]
            sk = k.sb("sk5", [128, 16])
            dens = [k.sb("den5", [128, 16]) for _ in range(3)]
            k.dma("sp", sk[:], self.swa_sinks[0:1, :].broadcast_to([128, 16]))
            k.I("act", "activation", out=sk[:], in_=sk[:], func=AF.Exp)

            def build_cat(t, c_):
                sl = slice(t * 128, (t + 1) * 128)
                a_ = at[t % 3]; den = dens[t % 3]
                k.dma("sp", a_[:].rearrange("p h e -> p (h e)"), self.ATT[sl, :])
                yield
                k.I("dve", "tensor_tensor", out=den[:], in0=a_[:, :, 64], in1=sk[:], op=ALU.add)
                k.I("dve", "reciprocal", out=den[:], in_=den[:])
                k.I("dve", "tensor_tensor", out=c_[:].rearrange("p (h e) -> p h e", e=64), in0=a_[:, :, 0:64],
                    in1=den[:].unsqueeze(2).broadcast_to([128, 16, 64]), op=ALU.mult)

            self.mixer_finish(1, mod, self.X2, self.swa_w_out, 1024, build_cat, 1)

    def moe(self, layer, mod, OUTX):
        k, S, NT = self.k, self.S, self.NT
        cap = S // 8
        NA = cap // 128
        nc = self.nc
        with k.scope():
            idx_i = k.sb("idx_i", [128, NE, NA], I32)
            gate_s = k.sb("gate_s", [128, NE, NA])
            with k.scope():
                affs = k.sb("affs6", [128, NT, NE])
                k.dma("sp", affs[:], self.AFF[:])
                affT = k.sb("affT", [NE, S])
                scr = k.sb("scr6", [NE, S], BF16)
                pt = [k.ps("p6t", [128, 512]) for _ in range(2)]
                for t0 in range(0, NT, 4):
                    p = pt[(t0 // 4) % 2]
                    for i in range(4):
                        k.tp(p[0:NE, i * 128:(i + 1) * 128], affs[:, t0 + i, :], self.ident_f[:])
                    k.cp(t0 // 4, affT[:, t0 * 128:(t0 + 4) * 128], p[0:NE, :])
                lo = k.sb("lo6", [NE, 1]); mid = k.sb("mid6", [NE, 1]); cnt = k.sb("cnt6", [NE, 1]); ge = k.sb("ge6", [NE, 1])
                k.I("dve", "memset", ap=lo[:], constant=0.0)
                for itn in range(34):
                    dlt = 2.0 ** (-(itn + 1))
                    k.I("dve", "tensor_scalar", out=mid[:], in0=lo[:], scalar1=dlt, scalar2=None, op0=ALU.add)
                    k.I("dve", "tensor_scalar", out=scr[:], in0=affT[:], scalar1=mid[:, 0:1], scalar2=None, op0=ALU.is_gt, op1=ALU.add,
                        accum_out=cnt[:])
                    k.I("dve", "tensor_scalar", out=ge[:], in0=cnt[:], scalar1=float(cap) - 0.5, scalar2=dlt, op0=ALU.is_gt, op1=ALU.mult)
                    k.I("dve", "tensor_tensor", out=lo[:], in0=lo[:], in1=ge[:], op=ALU.add)
                maskT = k.sb("maskT", [NE, S])
                posT = affT
                k.I("dve", "tensor_scalar", out=maskT[:], in0=affT[:], scalar1=lo[:, 0:1], scalar2=None, op0=ALU.is_gt)
                k.I("dve", "tensor_tensor_scan", out=posT[:], data0=maskT[:], data1=maskT[:], initial=0.0, op0=ALU.add, op1=ALU.max)
                k.I("dve", "tensor_tensor", out=posT[:], in0=posT[:], in1=maskT[:], op=ALU.subtract)
                posk = k.sb("posk", [128, NT, NE]); mskk = k.sb("mskk", [128, NT, NE])
                for src, dst in ((posT, posk), (maskT, mskk)):
                    for t0 in range(0, NT, 32):
                        n = min(32, NT - t0)
                        p = pt[(t0 // 32) % 2]
                        for i in range(n):
                            k.tp(p[:, i * NE:(i + 1) * NE], src[:, (t0 + i) * 128:(t0 + i + 1) * 128], self.ident_f[0:NE, 0:NE])
                        k.I("dve", "tensor_copy", out=dst[:, t0:t0 + n, :].rearrange("p a b -> p (a b)"), in_=p[:, 0:n * NE])
                posi = k.sb("posi6", [128, NT, NE], I32); hii = k.sb("hii6", [128, NT, NE], I32); loi = k.sb("loi6", [128, NT, NE], I32)
                hif = k.sb("hif6", [128, NT, NE]); lof = k.sb("lof6", [128, NT, NE])
                k.I("dve", "tensor_copy", out=posi[:], in_=posk[:])
                k.I("dve", "tensor_single_scalar", out=hii[:], in_=posi[:], scalar=7, op=ALU.arith_shift_right)
                k.I("dve", "tensor_single_scalar", out=loi[:], in_=posi[:], scalar=127, op=ALU.bitwise_and)
                k.I("dve", "tensor_copy", out=hif[:], in_=hii[:])
                k.I("dve", "tensor_copy", out=lof[:], in_=loi[:])
                io128i = k.sb("io128i", [128, 128], I32); io128 = k.sb("io128", [128, 128])
                k.I("pool", "iota", out=io128i[:], pattern=[[1, 128]], base=0, channel_multiplier=0)
                k.I("dve", "tensor_copy", out=io128[:], in_=io128i[:])
                toki = k.sb("toki", [128, NT], I32); tokf = k.sb("tokf", [128, NT])
                k.I("pool", "iota", out=toki[:], pattern=[[128, NT]], base=0, channel_multiplier=1)
                k.I("dve", "tensor_copy", out=tokf[:], in_=toki[:])
                io128b = k.sb("io128b", [128, 128], BF16)
                k.I("dve", "tensor_copy", out=io128b[:], in_=io128[:])
                lofb = k.sb("lofb", [128, NT, NE], BF16)
                k.I("dve", "tensor_copy", out=lofb[:], in_=lof[:])
                thii = k.sb("thii", [128, NT], I32); tloi = k.sb("tloi", [128, NT], I32)
                thif = k.sb("thif", [128, NT]); tlof = k.sb("tlof", [128, NT])
                k.I("dve", "tensor_single_scalar", out=thii[:], in_=toki[:], scalar=6, op=ALU.arith_shift_right)
                k.I("dve", "tensor_single_scalar", out=tloi[:], in_=toki[:], scalar=63, op=ALU.bitwise_and)
                k.I("dve", "tensor_copy", out=thif[:], in_=thii[:])
                k.I("dve", "tensor_copy", out=tlof[:], in_=tloi[:])
                ghb = k.sb("ghb", [128, NT, NE], BF16); ghf = k.sb("ghf", [128, NT, NE]); glf = k.sb("glf", [128, NT, NE])
                k.I("dve", "tensor_copy", out=ghb[:], in_=affs[:])
                k.I("dve", "tensor_copy", out=ghf[:], in_=ghb[:])
                k.I("dve", "tensor_tensor", out=glf[:], in0=affs[:], in1=ghf[:], op=ALU.subtract)
                A1 = [k.sb("A61", [128, NT, NA]) for _ in range(2)]
                A2 = [k.sb("A62", [128, NT, 4 * NA], BF16) for _ in range(2)]
                TB = 8
                Bblk = [k.sb("B6", [128, TB, 128], BF16) for _ in range(3)]
                pacc = [k.ps("pacc", [128, 512]) for _ in range(2)]
                racc = k.sb("racc", [128, 4 * NA])
                bi = 0
                for e in range(NE):
                    a1 = A1[e % 2]; a2 = A2[e % 2]
                    k.I("dve", "tensor_tensor", out=a1[:], in0=io128[:, 0:NA].unsqueeze(1).broadcast_to([128, NT, NA]),
                        in1=hif[:, :, e].unsqueeze(2).broadcast_to([128, NT, NA]), op=ALU.is_equal)
                    k.I("dve", "tensor_tensor", out=a1[:], in0=a1[:], in1=mskk[:, :, e].unsqueeze(2).broadcast_to([128, NT, NA]), op=ALU.mult)
                    k.I("dve", "tensor_tensor", out=a2[:, :, 0:NA], in0=a1[:], in1=thif[:].unsqueeze(2).broadcast_to([128, NT, NA]), op=ALU.mult)
                    k.I("dve", "tensor_tensor", out=a2[:, :, NA:2 * NA], in0=a1[:], in1=tlof[:].unsqueeze(2).broadcast_to([128, NT, NA]), op=ALU.mult)
                    k.I("dve", "tensor_tensor", out=a2[:, :, 2 * NA:3 * NA], in0=a1[:], in1=ghf[:, :, e].unsqueeze(2).broadcast_to([128, NT, NA]), op=ALU.mult)
                    k.I("dve", "tensor_tensor", out=a2[:, :, 3 * NA:4 * NA], in0=a1[:], in1=glf[:, :, e].unsqueeze(2).broadcast_to([128, NT, NA]), op=ALU.mult)
                    pa = pacc[e % 2]
                    for t0 in range(0, NT, TB):
                        bb = Bblk[bi % 3]; bi += 1
                        k.I("dve", "tensor_tensor", out=bb[:], in0=io128b[:].unsqueeze(1).broadcast_to([128, TB, 128]),
                            in1=lofb[:, t0:t0 + TB, e].unsqueeze(2).broadcast_to([128, TB, 128]), op=ALU.is_equal)
                        for j in range(TB):
                            t = t0 + j
                            k.mm(pa[:, 0:4 * NA], bb[:, j, :], a2[:, t, :], start=(t == 0), stop=(t == NT - 1))
                    k.I("act", "activation", out=racc[:], in_=pa[:, 0:4 * NA], func=AF.Copy)
                    k.I("dve", "scalar_tensor_tensor", out=racc[:, 0:NA], in0=racc[:, 0:NA], scalar=64.0, in1=racc[:, NA:2 * NA], op0=ALU.mult, op1=ALU.add)
                    k.I("dve", "tensor_copy", out=idx_i[:, e, :], in_=racc[:, 0:NA])
                    k.I("dve", "tensor_tensor", out=gate_s[:, e, :], in0=racc[:, 2 * NA:3 * NA], in1=racc[:, 3 * NA:4 * NA], op=ALU.add)
                if "IDX" in self.dbg:
                    self.IDX = k.dram("IDX", [128, NE, NA], I32, kind="ExternalOutput")
                    k.dma("sp", self.IDX[:], idx_i[:])
                    self.GTS = k.dram("GTS", [128, NE, NA], F32, kind="ExternalOutput")
                    k.dma("sp", self.GTS[:], gate_s[:])
            if getattr(self, "stop", None) == "route":
                return
            with k.scope():
                zf = k.sb("zf6", [128, D])
                k.I("pool", "memset", ap=zf[:], constant=0.0)
                for t in range(NT):
                    k.dma("sp" if t % 2 == 0 else "act", self.Y.reg(t)[t * 128:(t + 1) * 128, :], zf[:])
            with k.scope():
                wg = [k.sb("wg", [128, 8, D], BF16) for _ in range(2)]
                wu = [k.sb("wu", [128, 8, D], BF16) for _ in range(2)]
                wd = [k.sb("wd", [128, 8, D], BF16) for _ in range(2)]
                st = [k.sb("wst6", [128, D]) for _ in range(6)]
                xg = [k.sb("xg6", [128, D], BF16) for _ in range(2)]
                xgT = k.sb("xgT", [128, 8, cap], BF16)
                hT = k.sb("hT6", [128, 8, cap], BF16)
                sg = [k.sb("sg6", [128, 512]) for _ in range(2)]
                yst = [k.sb("yst", [128, D]) for _ in range(2)]
                pst = [k.ps("p6x", [128, 512], BF16) for _ in range(2)]
                pgu = [k.ps("p6g", [128, 512]) for _ in range(4)]
                pdn = [k.ps("p6d", [128, 512]) for _ in range(2)]
                self._wi = 0

                def loadw1(dst, src, kc):
                    s_ = st[self._wi % len(st)]
                    k.dma("sp", s_[:], src[kc * 128:(kc + 1) * 128, :])
                    r = self._wi % 4
                    if r == 3:
                        k.I("pool", "tensor_copy", out=dst[:, kc, :], in_=s_[:])
                    elif r == 1:
                        k.I("dve", "tensor_copy", out=dst[:, kc, :], in_=s_[:])
                    else:
                        k.I("act", "activation", out=dst[:, kc, :], in_=s_[:], func=AF.Copy)
                    self._wi += 1

                def loadw_chunk(e, kc):
                    loadw1(wg[e % 2], self.moe_w_gate[layer, e], kc)
                    loadw1(wu[e % 2], self.moe_w_up[layer, e], kc)
                    loadw1(wd[e % 2], self.moe_w_down[layer, e], kc)

                for kc in range(8):
                    loadw_chunk(0, kc)
                self._ci = 0

                def gather(e, a):
                    k.dma("pool", xg[a % 2][:], self.H2[:, :], method="indirect_dma_start", out_offset=None,
                          in_offset=bass.IndirectOffsetOnAxis(ap=idx_i.t[:, e, a:a + 1], axis=0), R=[idx_i])

                def xpose(e, a):
                    x_ = xg[a % 2]
                    for g0 in range(2):
                        p = pst[g0]
                        for i in range(4):
                            kc = g0 * 4 + i
                            k.tp(p[:, i * 128:(i + 1) * 128], x_[:, kc * 128:(kc + 1) * 128], self.ident_b[:])
                        k.cp(self._ci, xgT[:, g0 * 4:(g0 + 1) * 4, a * 128:(a + 1) * 128], p[:].rearrange("p (a b) -> p a b", b=128)); self._ci += 1

                ci = 0
                for e in range(NE):
                    G, U, Dn = wg[e % 2], wu[e % 2], wd[e % 2]
                    if e == 0:
                        for a in range(NA):
                            gather(0, a); xpose(0, a)
                    for fc in range(8):
                        for n0 in range(0, cap, 512):
                            nw = min(512, cap - n0)
                            p1 = pgu[ci % 4]; p2 = pgu[(ci + 1) % 4]; s_ = sg[(ci // 2) % 2]; ci += 2
                            for kc in range(8):
                                k.mm(p1[:, 0:nw], G[:, kc, fc * 128:(fc + 1) * 128], xgT[:, kc, n0:n0 + nw], start=(kc == 0), stop=(kc == 7))
                            for kc in range(8):
                                k.mm(p2[:, 0:nw], U[:, kc, fc * 128:(fc + 1) * 128], xgT[:, kc, n0:n0 + nw], start=(kc == 0), stop=(kc == 7))
                            k.I("act", "activation", out=s_[:, 0:nw], in_=p1[:, 0:nw], func=AF.Silu)
                            k.I("dve", "tensor_tensor", out=hT[:, fc, n0:n0 + nw], in0=s_[:, 0:nw], in1=p2[:, 0:nw], op=ALU.mult)
                        if e + 1 < NE:
                            loadw_chunk(e + 1, fc)
                    for a in range(NA):
                        if e + 1 < NE:
                            gather(e + 1, a)
                            if a >= 1:
                                xpose(e + 1, a - 1)
                        y_ = yst[a % 2]
                        for hh in range(2):
                            p = pdn[hh]
                            for fc in range(8):
                                k.mm(p[:], hT[:, fc, a * 128:(a + 1) * 128], Dn[:, fc, hh * 512:(hh + 1) * 512], start=(fc == 0), stop=(fc == 7))
                            if hh == 0:
                                k.I("dve", "tensor_scalar", out=y_[:, 0:512], in0=p[:], scalar1=gate_s[:, e, a:a + 1], scalar2=None, op0=ALU.mult)
                            else:
                                k.I("act", "activation", out=y_[:, 512:1024], in_=p[:], func=AF.Copy, scale=gate_s[:, e, a:a + 1])
                        k.dma("pool", self.Y[:, :], y_[:], method="indirect_dma_start",
                              out_offset=bass.IndirectOffsetOnAxis(ap=idx_i.t[:, e, a:a + 1], axis=0), in_offset=None,
                              compute_op=ALU.add, R=[idx_i])
                    if e + 1 < NE:
                        xpose(e + 1, NA - 1)
            with k.scope():
                g, b = self.ln_setup(self.ln_ffn_g, self.ln_ffn_b, layer)
                NR = 8
                tmps = [self.ln_tmp() for _ in range(NR)]
                xt = [k.sb("xt7", [128, D]) for _ in range(4)]
                yt = [k.sb("yt7", [128, D]) for _ in range(NR)]

                def tile(t):
                    sl = slice(t * 128, (t + 1) * 128)
                    x_ = xt[t % 4]; y_ = yt[t % NR]; tm = tmps[t % NR]
                    st = tm["st"]; mv = tm["mv"]; rs = tm["rs"]; nb = tm["nb"]
                    k.dma("sp", x_[:], self.X1[sl, :])
                    k.dma("act", y_[:], self.Y[sl, :])
                    yield
                    k.I("pool", "tensor_tensor", out=y_[:], in0=y_[:], in1=mod[:, 5 * D:6 * D], op=ALU.mult)
                    yield
                    k.I("dve", "scalar_tensor_tensor", out=y_[:], in0=x_[:], scalar=float(ALPHA), in1=y_[:], op0=ALU.mult, op1=ALU.add)
                    for hh in range(2):
                        k.I("dve", "bn_stats", out=st[:, hh, :], in_=y_[:, hh * 512:(hh + 1) * 512])
                    k.I("dve", "bn_aggr", out=mv[:], in_=st[:].rearrange("p a b -> p (a b)"))
                    yield
                    k.I("act", "activation", out=rs[:], in_=mv[:, 1:2], func=AF.Ln, bias=self.eps_ln[:, 0:1])
                    k.I("act", "activation", out=rs[:], in_=rs[:], func=AF.Exp, scale=-0.5)
                    yield
                    k.I("dve", "scalar_tensor_tensor", out=nb[:], in0=mv[:, 0:1], scalar=-1.0, in1=rs[:], op0=ALU.mult, op1=ALU.mult)
                    yield
                    k.I("act", "activation", out=y_[:], in_=y_[:], func=AF.Identity, scale=rs[:, 0:1], bias=nb[:, 0:1])
                    yield
                    k.I("pool", "tensor_tensor", out=y_[:], in0=y_[:], in1=g[:], op=ALU.mult)
                    yield
                    k.I("dve", "tensor_tensor", out=y_[:], in0=y_[:], in1=b[:], op=ALU.add)
                    k.dma("sp", OUTX.reg(t)[sl, :], y_[:])
                    yield
                run_pipelined(NT, tile, 8)


    def p1_proj1(self, mod):
        k, S, NT = self.k, self.S, self.NT
        MT = 4
        MW = MT * 128
        with k.scope():
            w = k.sb("w_in1", [128, 8, 1536], BF16)
            self.load_w(w, self.swa_w_in, 1536)
            xs = [k.sb("xs1", [128, MT, D]) for _ in range(2)]
            hb = k.sb("hb1", [128, MT, D], BF16)
            hTs = [k.sb("hT1", [128, 8, MW], BF16) for _ in range(2)]
            qb = [k.sb("qb1", [128, 1536]) for _ in range(3)]
            qbb = [k.sb("qbb1", [128, 1536], BF16) for _ in range(2)]
            kdup = [k.sb("kdup", [128, 4, 2, 64], BF16) for _ in range(2)]
            qkT = [k.sb("qkT1", [128, 12, 128], BF16) for _ in range(2)]
            rts = [[k.sb("rtmp1", [128, 24, 8]) for _ in range(4)] for _ in range(2)]
            pst = [k.ps("pst1", [128, 512], BF16) for _ in range(2)]
            psm = [k.ps("psm1", [128, 512]) for _ in range(4)]
            self._ei = 0
            self._pi = 0

            def nextp():
                p = psm[self._pi % 4]; self._pi += 1
                return p

            def cpy(out, in_):
                k.cp(self._ei, out, in_); self._ei += 1

            def tile(t):
                m, j = t // MT, t % MT
                hT = hTs[m % 2]
                if j == 0:
                    x_ = xs[m % 2]
                    k.dma("sp", x_[:], self.X2[m * MW:(m + 1) * MW, :].rearrange("(j p) d -> p j d", p=128))
                    for jj in range(MT):
                        k.I("dve", "tensor_tensor", out=x_[:, jj, :], in0=x_[:, jj, :], in1=mod[:, D:2 * D], op=ALU.mult)
                        k.I("pool", "tensor_tensor", out=hb[:, jj, :], in0=x_[:, jj, :], in1=mod[:, 0:D], op=ALU.add)
                yield
                if j == 0:
                    for kc in range(8):
                        p = pst[kc % 2]
                        for jj in range(MT):
                            k.tp(p[:, jj * 128:(jj + 1) * 128], hb[:, jj, kc * 128:(kc + 1) * 128], self.ident_b[:])
                        cpy(hT[:, kc, :], p[:])
                yield
                q_ = qb[t % 3]
                for g in range(3):
                    p = nextp()
                    for kc in range(8):
                        k.mm(p[:], hT[:, kc, j * 128:(j + 1) * 128], w[:, kc, g * 512:(g + 1) * 512], start=(kc == 0), stop=(kc == 7))
                    cpy(q_[:, g * 512:(g + 1) * 512], p[:])
                yield
                self.rope(q_, 20, t, rts[t % 2])
                yield
                qq = qbb[t % 2]
                k.I("act", "activation", out=qq[:], in_=q_[:], func=AF.Copy)
                k.dma("sp", self.V1.reg(t)[t * 128:(t + 1) * 128, :], qq[:, 1280:1536])
                kd_ = kdup[t % 2]
                kv = qq[:, 1024:1280].rearrange("p (h d) -> p h d", d=64)
                k.I("pool", "tensor_copy", out=kd_[:, :, 0, :], in_=kv)
                k.I("pool", "tensor_copy", out=kd_[:, :, 1, :], in_=kv)
                yield
                qt = qkT[t % 2]
                for g3 in range(3):
                    p = pst[g3 % 2]
                    for i4 in range(4):
                        cc = g3 * 4 + i4
                        src = qq[:, cc * 128:(cc + 1) * 128] if cc < 8 else kd_[:, cc - 8, :, :].rearrange("p a b -> p (a b)")
                        k.tp(p[:, i4 * 128:(i4 + 1) * 128], src, self.ident_b[:])
                    cpy(qt[:, g3 * 4:(g3 + 1) * 4, :], p[:].rearrange("p (a b) -> p a b", b=128))
                k.dma("act", V(self.Q1T.t.rearrange("(cc p) t -> p cc t", p=128)[:, :, t * 128:(t + 1) * 128], self.Q1T.reg(t)),
                      qt[:, 0:8, :])
                k.dma("act", V(self.K1T.t.rearrange("(cc p) t -> p cc t", p=128)[:, :, t * 128:(t + 1) * 128], self.K1T.reg(t)),
                      qt[:, 8:12, :])
                yield

            run_pipelined(NT, tile)

    def p4_swa(self):
        with self.k.scope():
            masks = self.attn_masks(128)
            heads = [(h // 2, h // 4, (h % 2) * 64, h // 4) for h in range(16)]
            self.attn(self.Q1T, self.K1T, self.V1, heads, 8, 4, 256, 0, 1, masks, self.ATT)


def build(S, dbg=None, stop=None):
    P = Prog(S, dbg)
    k = P.k
    P.declare()
    with k.scope():
        P.consts()
        k.mark("P.consts()")
        P.rope_tables()
        k.mark("P.rope_tables()")
        mod = k.sb("mod", [128, 6 * D])
        P.ada(0, mod)
        k.mark("P.ada(0, mod)")
        if "MOD" in (dbg or []):
            P.MOD = k.dram("MOD", [128, 6 * D], F32, kind="ExternalOutput")
            k.dma("sp", P.MOD[:], mod[:])
        P.p1_proj0(mod)
        k.mark("P.p1_proj0(mod)")
        P.p2_dnprep()
        k.mark("P.p2_dnprep()")
        P.p3_deltanet()
        k.mark("P.p3_deltanet()")
        P.p4_dilated()
        k.mark("P.p4_dilated()")
        P.p5_finish0(mod)
        k.mark("P.p5_finish0(mod)")
        P.stop = stop
        if stop == "p5":
            k.barrier()
            return P
        P.moe(0, mod, P.X2)
        k.mark("P.moe(0, mod, P.X2)")
        if stop:
            k.barrier()
            return P
        P.ada(1, mod)
        k.mark("P.ada(1, mod)")
        P.p1_proj1(mod)
        k.mark("P.p1_proj1(mod)")
        P.p4_swa()
        k.mark("P.p4_swa()")
        P.p5_finish1(mod)
        k.mark("P.p5_finish1(mod)")
        P.moe(1, mod, P.out)
        k.mark("P.moe(1, mod, P.out)")
    return P


def host_inputs(inputs, b, S):
    f = lambda a: np.ascontiguousarray(np.asarray(a))
    m = {}
    m["x"] = f(inputs["x"][b, :S])
    m["c"] = f(np.asarray(inputs["c"][b]).reshape(8, 128).T)
    m["pos"] = f(np.asarray(inputs["positions"][b, :S]).reshape(S // 128, 128).T.astype(np.int32))
    m["ada_w"] = f(inputs["ada_w"])
    m["ada_b"] = f(inputs["ada_b"])
    m["ab_w_in"] = f(inputs["ab_w_in"][0])
    m["ab_conv_w"] = f(np.asarray(inputs["ab_conv_w"][0]).reshape(5, 12, 128).transpose(2, 1, 0))
    m["ab_a_log"] = f(np.asarray(inputs["ab_a_log"][0]).reshape(1, 8))
    m["ab_dt_bias"] = f(np.asarray(inputs["ab_dt_bias"][0]).reshape(1, 8))
    m["ab_dn_norm"] = f(np.asarray(inputs["ab_dn_norm"][0]).reshape(1, 128))
    m["ab_w_out"] = f(inputs["ab_w_out"][0])
    m["swa_w_in"] = f(inputs["swa_w_in"][0])
    m["swa_sinks"] = f(np.asarray(inputs["swa_sinks"][0]).reshape(1, 16))
    m["swa_w_out"] = f(inputs["swa_w_out"][0])
    for n in ("ln_mix_g", "ln_mix_b", "router_w", "moe_w_gate", "moe_w_up", "moe_w_down", "ln_ffn_g", "ln_ffn_b"):
        m[n] = f(inputs[n])
    return m


def kernel(**inputs):
    S = 8192
    P = build(S)
    in_maps = [host_inputs(inputs, b, S) for b in range(8)]
    res = run_bass_kernel_spmd(P.nc, in_maps, core_ids=list(range(8)))
    return np.stack([r["out"] for r in res.results], axis=0)
```

```python
import math
import contextlib
import numpy as np
import concourse.bass as bass
import concourse.mybir as mybir
from concourse.bass_utils import run_bass_kernel_spmd

F32 = mybir.dt.float32
BF16 = mybir.dt.bfloat16
I32 = mybir.dt.int32
U32 = mybir.dt.uint32
AF = mybir.ActivationFunctionType
ALU = mybir.AluOpType
AX = mybir.AxisListType

D = 1024
NKC = 8
AB_IN = 4368
ALPHA = 4.0 ** 0.25
LN_EPS = 1e-5
NORM_EPS = 1e-6
BIG = 30000.0
NE = 16


class Buf:
    ALL = []

    def __init__(self, t, name):
        self.t = t
        self.name = name
        self.w = {}
        self.r = {}
        self.regs = {}
        Buf.ALL.append(self)

    def reg(self, key):
        if key not in self.regs:
            self.regs[key] = Buf(self.t, "%s@%s" % (self.name, key))
        return self.regs[key]

    def __getitem__(self, idx):
        return V(self.t[idx], self)


class V:
    def __init__(self, ap, buf):
        self.ap = ap
        self.buf = buf

    def __getitem__(self, idx):
        return V(self.ap[idx], self.buf)

    def rearrange(self, *a, **k):
        return V(self.ap.rearrange(*a, **k), self.buf)

    def bitcast(self, dt):
        return V(self.ap.bitcast(dt), self.buf)

    def broadcast_to(self, *a, **k):
        return V(self.ap.broadcast_to(*a, **k), self.buf)

    def unsqueeze(self, *a, **k):
        return V(self.ap.unsqueeze(*a, **k), self.buf)


WRITE_KEYS = ("out", "accum_out", "ap")


class K:
    NDMA_SLOTS = 6

    def __init__(self, nc):
        self.nc = nc
        Buf.ALL = []
        self.eng = {"pe": nc.tensor, "act": nc.scalar, "dve": nc.vector, "pool": nc.gpsimd, "sp": nc.sync}
        self.sem = {}
        self.cnt = {}
        for e in self.eng:
            self.sem[e] = nc.alloc_semaphore("sem_" + e)
            self.cnt[e] = 0
        self.dsem = {}
        self.dcnt = {}
        for q in ("sp", "act", "pool"):
            self.dsem[q] = [nc.alloc_semaphore("dsem_%s_%d" % (q, i)) for i in range(self.NDMA_SLOTS)]
            self.dcnt[q] = 0
        self.seen = {e: {} for e in self.eng}
        self.n_inst = 0
        self.n_wait = 0
        self.stack = None
        self.uid = 0

    @contextlib.contextmanager
    def scope(self):
        old = self.stack
        with contextlib.ExitStack() as st:
            self.stack = st
            yield
            self.barrier()
        self.stack = old

    def sb(self, name, shape, dt=F32):
        self.uid += 1
        nm = "%s_%d" % (name, self.uid)
        t = self.stack.enter_context(self.nc.sbuf_tensor(nm, list(shape), dt))
        return Buf(t, nm)

    def ps(self, name, shape, dt=F32):
        self.uid += 1
        nm = "%s_%d" % (name, self.uid)
        t = self.stack.enter_context(self.nc.psum_tensor(nm, list(shape), dt))
        return Buf(t, nm)

    def dram(self, name, shape, dt, kind="Internal"):
        t = self.nc.dram_tensor(name, list(shape), dt, kind=kind).ap()
        return Buf(t, name)

    def _semobj(self, key):
        if key[0] == "c":
            return self.sem[key[1]]
        return self.dsem[key[1]][key[2]]

    def _wait(self, e, key, val):
        s = self.seen[e]
        if s.get(key, 0) >= val:
            return
        self.eng[e].wait_ge(self._semobj(key), val)
        self.n_wait += 1
        s[key] = val

    def mark(self, name):
        if not hasattr(self, "marks"):
            self.marks = []
        self.marks.append((name, dict(self.cnt)))

    def barrier(self):
        for e in self.eng:
            for e2 in self.eng:
                if e2 != e and self.cnt[e2] > 0:
                    self._wait(e, ("c", e2), self.cnt[e2])
            for q in self.dsem:
                i = self.dcnt[q]
                for slot in range(self.NDMA_SLOTS):
                    n = (i - slot + self.NDMA_SLOTS - 1) // self.NDMA_SLOTS if i > slot else 0
                    if n > 0:
                        self._wait(e, ("d", q, slot), 16 * n)
        for b in Buf.ALL:
            b.w = {}
            b.r = {}
            b.regs = {}

    def _deps(self, e, reads, writes, is_dma=False):
        need = {}

        def add(key, val):
            if need.get(key, 0) < val:
                need[key] = val

        mykey = ("c", e)
        for b in reads:
            for key, val in b.w.items():
                if key == mykey and e == "pe" and not is_dma:
                    continue
                add(key, val)
        for b in writes:
            for key, val in b.w.items():
                if key == mykey and not is_dma:
                    continue
                add(key, val)
            for key, val in b.r.items():
                if key == mykey and not is_dma:
                    continue
                add(key, val)
        for key, val in need.items():
            self._wait(e, key, val)

    def I(self, e, method, **kw):
        reads, writes = [], []
        extra_r = kw.pop("R", [])
        extra_w = kw.pop("W", [])
        kw2 = {}
        for k_, v in kw.items():
            if isinstance(v, V):
                (writes if k_ in WRITE_KEYS else reads).append(v.buf)
                kw2[k_] = v.ap
            else:
                kw2[k_] = v
        for v in extra_r:
            reads.append(v.buf if isinstance(v, V) else v)
        for v in extra_w:
            writes.append(v.buf if isinstance(v, V) else v)
        self._deps(e, reads, writes)
        ins = getattr(self.eng[e], method)(**kw2)
        self.cnt[e] += 1
        ins.then_inc(self.sem[e], 1)
        key = ("c", e)
        val = self.cnt[e]
        for b in writes:
            b.w = {key: val}
            b.r = {}
        for b in reads:
            if b in writes:
                continue
            b.r[key] = val
        self.n_inst += 1
        return ins

    def dma(self, q, out, in_, method="dma_start", R=(), **kw):
        reads = [in_.buf] + [v.buf if isinstance(v, V) else v for v in R]
        writes = [out.buf]
        i = self.dcnt[q]
        slot = i % self.NDMA_SLOTS
        rnd = i // self.NDMA_SLOTS
        key = ("d", q, slot)
        if rnd > 0:
            self._wait(q, key, 16 * rnd)
        self._deps(q, reads, writes, is_dma=True)
        kw2 = {}
        for k_, v in kw.items():
            kw2[k_] = v
        ins = getattr(self.eng[q], method)(out=out.ap, in_=in_.ap, **kw2)
        ins.then_inc(self.dsem[q][slot], 16)
        self.dcnt[q] += 1
        val = 16 * (rnd + 1)
        for b in writes:
            b.w = {key: val}
            b.r = {}
        for b in reads:
            b.r[key] = val
        self.n_inst += 1
        return ins

    def mm(self, out, lhsT, rhs, start=True, stop=True):
        return self.I("pe", "matmul", out=out, lhsT=lhsT, rhs=rhs, start=start, stop=stop)

    def tp(self, out, in_, ident):
        return self.I("pe", "transpose", out=out, in_=in_, identity=ident)

    def cp(self, i, out, in_):
        if i % 2 == 0:
            return self.I("dve", "tensor_copy", out=out, in_=in_)
        return self.I("act", "activation", out=out, in_=in_, func=AF.Copy)


def run_pipelined(n, make_gen, nstages=None):
    live = []
    t_next = 0
    while t_next < n or live:
        if t_next < n:
            live.insert(0, make_gen(t_next))
            t_next += 1
        nxt = []
        for g in live:
            try:
                next(g)
                nxt.append(g)
            except StopIteration:
                pass
        live = nxt


class Prog:
    def __init__(self, S, dbg=None):
        self.S = S
        self.NT = S // 128
        self.dbg = dbg or []
        self.nc = bass.Bass("TRN2", target_bir_lowering=False)
        self.k = K(self.nc)
        self.outs = []

    def declare(self):
        k, S = self.k, self.S
        ein = lambda n, s, d=F32: k.dram(n, s, d, kind="ExternalInput")
        self.x = ein("x", [S, D])
        self.c = ein("c", [128, 8])
        self.pos = ein("pos", [128, self.NT], I32)
        self.ada_w = ein("ada_w", [2, D, 6 * D])
        self.ada_b = ein("ada_b", [2, 6 * D])
        self.ab_w_in = ein("ab_w_in", [D, AB_IN])
        self.ab_conv_w = ein("ab_conv_w", [128, 12, 5])
        self.ab_a_log = ein("ab_a_log", [1, 8])
        self.ab_dt_bias = ein("ab_dt_bias", [1, 8])
        self.ab_dn_norm = ein("ab_dn_norm", [1, 128])
        self.ab_w_out = ein("ab_w_out", [768, D])
        self.swa_w_in = ein("swa_w_in", [D, 1536])
        self.swa_sinks = ein("swa_sinks", [1, 16])
        self.swa_w_out = ein("swa_w_out", [D, D])
        self.ln_mix_g = ein("ln_mix_g", [2, D])
        self.ln_mix_b = ein("ln_mix_b", [2, D])
        self.router_w = ein("router_w", [2, D, NE])
        self.moe_w_gate = ein("moe_w_gate", [2, NE, D, D])
        self.moe_w_up = ein("moe_w_up", [2, NE, D, D])
        self.moe_w_down = ein("moe_w_down", [2, NE, D, D])
        self.ln_ffn_g = ein("ln_ffn_g", [2, D])
        self.ln_ffn_b = ein("ln_ffn_b", [2, D])
        self.out = k.dram("out", [S, D], F32, kind="ExternalOutput")
        sc = lambda n, s, d: k.dram(n, s, d, kind=("ExternalOutput" if n in self.dbg else "Internal"))
        self.QKVA = sc("QKVA", [1536, S + 4], BF16)
        self.Z = sc("Z", [S, 512], BF16)
        self.QBT = sc("QBT", [768, S], BF16)
        self.KBT = sc("KBT", [768, S], BF16)
        self.VB = sc("VB", [S, 768], BF16)
        self.QAT = sc("QAT", [512, S], BF16)
        self.KAT = sc("KAT", [512, S], BF16)
        self.KA = sc("KA", [S, 512], BF16)
        self.VA = sc("VA", [S, 512], BF16)
        self.OF = sc("OF", [S, 512], F32)
        self.OB = sc("OB", [S, 512], F32)
        self.DIL = sc("DIL", [3, S, 260], F32)
        self.X1 = sc("X1", [S, D], F32)
        self.H2 = sc("H2", [S, D], BF16)
        self.Y = sc("Y", [S, D], F32)
        self.X2 = sc("X2", [S, D], F32)
        self.GATES = sc("GATES", [128, self.NT, 16], F32)
        self.AFF = sc("AFF", [128, self.NT, NE], F32)
        self.Q1T = sc("Q1T", [1024, S], BF16)
        self.K1T = sc("K1T", [512, S], BF16)
        self.V1 = sc("V1", [S, 256], BF16)
        self.ATT = sc("ATT", [S, 16 * 65], F32)

    def consts(self):
        k = self.k
        self.ident_f = k.sb("ident_f", [128, 128], F32)
        self.ident_b = k.sb("ident_b", [128, 128], BF16)
        self.ones_f = k.sb("ones_f", [128, 128], F32)
        self.ones_b = k.sb("ones_b", [128, 128], BF16)
        self.zeros_b = k.sb("zeros_b", [128, 512], BF16)
        k.I("pool", "memset", ap=self.ones_f[:], constant=1.0)
        k.I("pool", "memset", ap=self.ones_b[:], constant=1.0)
        k.I("pool", "memset", ap=self.zeros_b[:], constant=0.0)
        k.I("pool", "affine_select", out=self.ident_f[:], in_=self.ones_f[:], pattern=[[-1, 128]],
            compare_op=ALU.is_equal, fill=0.0, base=0, channel_multiplier=1)
        k.I("pool", "tensor_copy", out=self.ident_b[:], in_=self.ident_f[:])
        self.eps_norm = k.sb("eps_norm", [128, 1])
        k.I("pool", "memset", ap=self.eps_norm[:], constant=NORM_EPS)
        self.eps_ln = k.sb("eps_ln", [128, 1])
        self.one_col = k.sb("one_col", [128, 1])
        k.I("pool", "memset", ap=self.one_col[:], constant=1.0)
        k.I("pool", "memset", ap=self.eps_ln[:], constant=LN_EPS)

    def tri(self, name, dt, val_true, val_false, base, cm, step, op):
        k = self.k
        t = k.sb(name, [128, 128], dt)
        k.I("pool", "memset", ap=t[:], constant=val_true)
        k.I("pool", "affine_select", out=t[:], in_=t[:], pattern=[[step, 128]],
            compare_op=op, fill=val_false, base=base, channel_multiplier=cm)
        return t

    def ada(self, layer, mod):
        k = self.k
        with k.scope():
            csb = k.sb("csb", [128, 8])
            crep = k.sb("crep", [128, 8, 128])
            brow = k.sb("brow", [1, 6 * D])
            k.dma("sp", csb[:], self.c[:])
            k.dma("sp", brow[:], self.ada_b[layer:layer + 1, :])
            for kc in range(8):
                k.I("act", "activation", out=crep[:, kc, :], in_=self.ones_f[:], func=AF.Silu,
                    scale=csb[:, kc:kc + 1])
            wst = [k.sb("adaw", [128, 8, 512]) for _ in range(2)]
            pp = [k.ps("adaps", [128, 512]) for _ in range(2)]
            for cg in range(12):
                w = wst[cg % 2]
                k.dma("sp" if cg % 2 == 0 else "act", w[:],
                      self.ada_w[layer, :, cg * 512:(cg + 1) * 512].rearrange("(kc p) n -> p kc n", p=128))
                p = pp[cg % 2]
                for kc in range(8):
                    k.mm(p[:], crep[:, kc, :], w[:, kc, :], start=(kc == 0), stop=False)
                k.mm(p[:], self.ones_f[0:1, :], brow[0:1, cg * 512:(cg + 1) * 512], start=False, stop=True)
                k.cp(cg, mod[:, cg * 512:(cg + 1) * 512], p[:])
            for part in (1, 2, 4, 5):
                k.I("dve", "tensor_scalar", out=mod[:, part * D:(part + 1) * D], in0=mod[:, part * D:(part + 1) * D],
                    scalar1=1.0, scalar2=None, op0=ALU.add)
        k.barrier()

    def rope_tables(self):
        k, NT = self.k, self.NT
        self.sin_t = k.sb("sin_t", [128, NT, 8])
        self.cos_t = k.sb("cos_t", [128, NT, 8])
        with k.scope():
            pi = k.sb("posi", [128, NT], I32)
            pf = k.sb("posf", [128, NT])
            ang = k.sb("ang", [128, NT, 8])
            t1 = k.sb("rt1", [128, NT, 8])
            ki = k.sb("rki", [128, NT, 8], I32)
            kf = k.sb("rkf", [128, NT, 8])
            r = k.sb("rr", [128, NT, 8])
            m = k.sb("rm", [128, NT, 8])
            k.dma("sp", pi[:], self.pos[:])
            k.I("dve", "tensor_copy", out=pf[:], in_=pi[:])
            for f in range(8):
                inv = float(np.float32(500000.0) ** np.float32(-(2.0 * f) / 16.0))
                k.I("dve", "tensor_scalar", out=ang[:, :, f], in0=pf[:], scalar1=inv, scalar2=None, op0=ALU.mult)
            TWO_PI = 2.0 * math.pi
            C1 = 6.28125
            C2 = TWO_PI - C1
            k.I("dve", "tensor_scalar", out=t1[:], in0=ang[:], scalar1=1.0 / TWO_PI, scalar2=None, op0=ALU.mult)
            k.I("dve", "tensor_copy", out=ki[:], in_=t1[:])
            k.I("dve", "tensor_copy", out=kf[:], in_=ki[:])
            k.I("dve", "scalar_tensor_tensor", out=r[:], in0=kf[:], scalar=-C1, in1=ang[:], op0=ALU.mult, op1=ALU.add)
            k.I("dve", "scalar_tensor_tensor", out=r[:], in0=kf[:], scalar=-C2, in1=r[:], op0=ALU.mult, op1=ALU.add)

            def fold(rr):
                k.I("dve", "tensor_scalar", out=m[:], in0=rr[:], scalar1=math.pi, scalar2=-TWO_PI, op0=ALU.is_gt, op1=ALU.mult)
                k.I("dve", "tensor_tensor", out=rr[:], in0=rr[:], in1=m[:], op=ALU.add)
                k.I("dve", "tensor_scalar", out=m[:], in0=rr[:], scalar1=-math.pi, scalar2=TWO_PI, op0=ALU.is_lt, op1=ALU.mult)
                k.I("dve", "tensor_tensor", out=rr[:], in0=rr[:], in1=m[:], op=ALU.add)

            fold(r)
            k.I("act", "activation", out=self.sin_t[:], in_=r[:], func=AF.Sin)
            k.I("dve", "tensor_scalar", out=r[:], in0=r[:], scalar1=math.pi / 2, scalar2=None, op0=ALU.add)
            fold(r)
            k.I("act", "activation", out=self.cos_t[:], in_=r[:], func=AF.Sin)
        k.barrier()

    def rope(self, qb, nheads, t, rt=None):
        k = self.k
        v = qb[:, 0:nheads * 64].rearrange("p (h d) -> p h d", d=64)
        x1 = v[:, :, 0:8]
        x2 = v[:, :, 8:16]
        cosb = self.cos_t[:, t, :].unsqueeze(1).broadcast_to([128, nheads, 8])
        sinb = self.sin_t[:, t, :].unsqueeze(1).broadcast_to([128, nheads, 8])
        rt = self.rtmp if rt is None else rt
        ta, tb, tc, td = [rt[i][:, 0:nheads, :] for i in range(4)]
        k.I("dve", "tensor_tensor", out=ta, in0=x1, in1=cosb, op=ALU.mult)
        k.I("pool", "tensor_tensor", out=tb, in0=x2, in1=sinb, op=ALU.mult)
        k.I("dve", "tensor_tensor", out=tc, in0=x2, in1=cosb, op=ALU.mult)
        k.I("pool", "tensor_tensor", out=td, in0=x1, in1=sinb, op=ALU.mult)
        k.I("dve", "tensor_tensor", out=x1, in0=ta, in1=tb, op=ALU.subtract)
        k.I("pool", "tensor_tensor", out=x2, in0=tc, in1=td, op=ALU.add)

    def load_w(self, dst, src, ncols, nkc=8, chunk=1024):
        k = self.k
        with k.scope():
            st = [k.sb("wstage", [128, chunk]) for _ in range(3)]
            i = 0
            for kc in range(nkc):
                for c0 in range(0, ncols, chunk):
                    cw = min(chunk, ncols - c0)
                    s = st[i % 3]
                    k.dma("sp" if i % 2 == 0 else "act", s[:, 0:cw], src[kc * 128:(kc + 1) * 128, c0:c0 + cw])
                    if i % 3 == 2:
                        k.I("pool", "tensor_copy", out=dst[:, kc, c0:c0 + cw], in_=s[:, 0:cw])
                    else:
                        k.cp(i, dst[:, kc, c0:c0 + cw], s[:, 0:cw])
                    i += 1

    def p1_proj0(self, mod):
        k, S, NT = self.k, self.S, self.NT
        MT = 2
        MW = MT * 128
        with k.scope():
            w = k.sb("w_in0", [128, 8, AB_IN], BF16)
            self.load_w(w, self.ab_w_in, AB_IN)
            xs = [k.sb("xs", [128, MT, D]) for _ in range(2)]
            hb = k.sb("hb", [128, MT, D], BF16)
            hTs = [k.sb("hT", [128, 8, MW], BF16) for _ in range(2)]
            qa = k.sb("qa_st", [128, 12, MW], BF16)
            zs = [k.sb("zs", [128, 512], BF16) for _ in range(2)]
            gsb = k.sb("gsb", [128, NT, 16])
            qb = [k.sb("qb", [128, 2304]) for _ in range(3)]
            qbb = [k.sb("qbb", [128, 2304], BF16) for _ in range(2)]
            qkT = [k.sb("qkT", [128, 12, 128], BF16) for _ in range(2)]
            rts = [[k.sb("rtmp", [128, 24, 8]) for _ in range(4)] for _ in range(2)]
            pst = [k.ps("pst", [128, 512], BF16) for _ in range(2)]
            psm = [k.ps("psm", [128, 512]) for _ in range(4)]
            for cc in range(12):
                k.dma("pool", self.QKVA.reg(("padl", cc))[cc * 128:(cc + 1) * 128, 0:2], self.zeros_b[:, 0:2])
                k.dma("pool", self.QKVA.reg(("padr", cc))[cc * 128:(cc + 1) * 128, S + 2:S + 4], self.zeros_b[:, 0:2])
            self._ei = 0
            self._pi = 0

            def nextp():
                p = psm[self._pi % 4]; self._pi += 1
                return p

            def cpy(out, in_):
                k.cp(self._ei, out, in_); self._ei += 1

            def tile(t):
                m, j = t // MT, t % MT
                hT = hTs[m % 2]
                if j == 0:
                    x_ = xs[m % 2]
                    k.dma("sp", x_[:], self.x[m * MW:(m + 1) * MW, :].rearrange("(j p) d -> p j d", p=128))
                    for jj in range(MT):
                        k.I("dve", "tensor_tensor", out=x_[:, jj, :], in0=x_[:, jj, :], in1=mod[:, D:2 * D], op=ALU.mult)
                        k.I("pool", "tensor_tensor", out=hb[:, jj, :], in0=x_[:, jj, :], in1=mod[:, 0:D], op=ALU.add)
                yield
                if j == 0:
                    for kc in range(0, 8, 2):
                        p = pst[(kc // 2) % 2]
                        for k2 in range(2):
                            for jj in range(MT):
                                k.tp(p[:, (k2 * MT + jj) * 128:(k2 * MT + jj + 1) * 128], hb[:, jj, (kc + k2) * 128:(kc + k2 + 1) * 128], self.ident_b[:])
                        cpy(hT[:, kc:kc + 2, :], p[:, 0:2 * MW].rearrange("p (a b) -> p a b", b=MW))
                yield
                if j == 0:
                    for cc in range(12):
                        p = nextp()
                        for kc in range(8):
                            k.mm(p[:, 0:MW], w[:, kc, cc * 128:(cc + 1) * 128], hT[:, kc, :], start=(kc == 0), stop=(kc == 7))
                        cpy(qa[:, cc, :], p[:, 0:MW])
                    k.dma("act", V(self.QKVA.t.rearrange("(cc p) t -> p cc t", p=128)[:, :, 2 + m * MW:2 + (m + 1) * MW],
                                   self.QKVA.reg(("m", m))), qa[:])
                p = nextp()
                for kc in range(8):
                    k.mm(p[:], hT[:, kc, j * 128:(j + 1) * 128], w[:, kc, 1536:2048], start=(kc == 0), stop=(kc == 7))
                z_ = zs[t % 2]
                k.I("act", "activation", out=z_[:], in_=p[:], func=AF.Silu)
                k.dma("sp", self.Z.reg(t)[t * 128:(t + 1) * 128, :], z_[:])
                p = nextp()
                for kc in range(8):
                    k.mm(p[:, 0:16], hT[:, kc, j * 128:(j + 1) * 128], w[:, kc, 2048:2064], start=(kc == 0), stop=(kc == 7))
                k.I("dve", "tensor_copy", out=gsb[:, t, :], in_=p[:, 0:16])
                q_ = qb[t % 3]
                for g in range(5):
                    c0 = 2064 + g * 512
                    cw = min(512, AB_IN - c0)
                    p = nextp()
                    for kc in range(8):
                        k.mm(p[:, 0:cw], hT[:, kc, j * 128:(j + 1) * 128], w[:, kc, c0:c0 + cw], start=(kc == 0), stop=(kc == 7))
                    cpy(q_[:, g * 512:g * 512 + cw], p[:, 0:cw])
                yield
                self.rope(q_, 24, t, rts[t % 2])
                yield
                qq = qbb[t % 2]
                k.I("act", "activation", out=qq[:], in_=q_[:], func=AF.Copy)
                k.dma("sp", self.VB.reg(t)[t * 128:(t + 1) * 128, :], qq[:, 1536:2304])
                yield
                qt = qkT[t % 2]
                for g3 in range(3):
                    p = pst[g3 % 2]
                    for i4 in range(4):
                        cc = g3 * 4 + i4
                        k.tp(p[:, i4 * 128:(i4 + 1) * 128], qq[:, cc * 128:(cc + 1) * 128], self.ident_b[:])
                    cpy(qt[:, g3 * 4:(g3 + 1) * 4, :], p[:].rearrange("p (a b) -> p a b", b=128))
                k.dma("act", V(self.QBT.t.rearrange("(cc p) t -> p cc t", p=128)[:, :, t * 128:(t + 1) * 128], self.QBT.reg(t)),
                      qt[:, 0:6, :])
                k.dma("act", V(self.KBT.t.rearrange("(cc p) t -> p cc t", p=128)[:, :, t * 128:(t + 1) * 128], self.KBT.reg(t)),
                      qt[:, 6:12, :])
                yield

            run_pipelined(NT, tile)
            k.dma("sp", self.GATES[:], gsb[:])
        k.barrier()

    def p2_dnprep(self):
        k, S = self.k, self.S
        NM = S // 512
        with k.scope():
            cw = k.sb("convw", [128, 12, 5])
            k.dma("sp", cw[:], self.ab_conv_w[:])
            dW = k.sb("dW", [128, 12, 5, 128], BF16)
            for cc in range(12):
                for j in range(5):
                    k.I("dve" if (cc + j) % 2 == 0 else "pool", "tensor_scalar", out=dW[:, cc, j, :], in0=self.ident_f[:],
                        scalar1=cw[:, cc, j:j + 1], scalar2=0.0, op0=ALU.mult, op1=ALU.add)
            NB = 3
            xin = [[k.sb("xin", [128, 516], BF16) for _ in range(4)] for _ in range(NB)]
            sl = [[k.sb("csl", [128, 512]) for _ in range(4)] for _ in range(NB)]
            sq = [[k.sb("csq", [128, 512]) for _ in range(4)] for _ in range(2)]
            rn = [[k.sb("crn", [128, 512]) for _ in range(4)] for _ in range(2)]
            ob = [[k.sb("cob", [128, 512], BF16) for _ in range(4)] for _ in range(NB)]
            tk = [k.sb("ctk", [128, 4, 128], BF16) for _ in range(3)]
            psc = [k.ps("p2c", [128, 512]) for _ in range(4)]
            pss = [k.ps("p2s", [128, 512]) for _ in range(2)]
            pst = [k.ps("p2t", [128, 512], BF16) for _ in range(2)]
            self._p2i = 0

            def group(gi):
                m, kind = gi // 3, gi % 3
                b3 = gi % NB; b2 = gi % 2
                for h in range(4):
                    cc = kind * 4 + h
                    xi = xin[b3][h]
                    k.dma("sp" if h % 2 == 0 else "act", xi[:], self.QKVA[cc * 128:(cc + 1) * 128, m * 512:m * 512 + 516])
                    pc = psc[h]
                    for j in range(5):
                        k.mm(pc[:], dW[:, cc, j, :], xi[:, j:j + 512], start=(j == 0), stop=(j == 4))
                    if kind == 2:
                        k.I("act", "activation", out=ob[b3][h][:], in_=pc[:], func=AF.Silu)
                    else:
                        k.I("act", "activation", out=sl[b3][h][:], in_=pc[:], func=AF.Silu)
                        k.I("pool", "tensor_tensor", out=sq[b2][h][:], in0=sl[b3][h][:], in1=sl[b3][h][:], op=ALU.mult)
                yield
                if kind != 2:
                    for h in range(4):
                        p = pss[h % 2]
                        k.mm(p[:], self.ones_f[:], sq[b2][h][:])
                        k.I("act", "activation", out=rn[b2][h][:], in_=p[:], func=AF.Ln, bias=self.eps_norm[:, 0:1])
                    for h in range(4):
                        k.I("act", "activation", out=rn[b2][h][:], in_=rn[b2][h][:], func=AF.Exp, scale=-0.5)
                yield
                for h in range(4):
                    o_ = ob[b3][h]
                    if kind == 0:
                        k.I("dve", "scalar_tensor_tensor", out=o_[:], in0=sl[b3][h][:], scalar=float(128 ** -0.5), in1=rn[b2][h][:],
                            op0=ALU.mult, op1=ALU.mult)
                    elif kind == 1:
                        k.I("dve", "tensor_tensor", out=o_[:], in0=sl[b3][h][:], in1=rn[b2][h][:], op=ALU.mult)
                    if kind != 2:
                        dst = self.QAT if kind == 0 else self.KAT
                        k.dma("act", dst.reg((m, h))[h * 128:(h + 1) * 128, m * 512:(m + 1) * 512], o_[:])
                    if kind >= 1:
                        i = self._p2i; self._p2i += 1
                        p = pst[i % 2]
                        for j in range(4):
                            k.tp(p[:, j * 128:(j + 1) * 128], o_[:, j * 128:(j + 1) * 128], self.ident_b[:])
                        t_ = tk[i % 3]
                        k.I("dve", "tensor_copy", out=t_[:], in_=p[:].rearrange("p (j d) -> p j d", d=128))
                        dst = self.KA if kind == 1 else self.VA
                        k.dma("sp", V(dst.t[m * 512:(m + 1) * 512, h * 128:(h + 1) * 128].rearrange("(j p) d -> p j d", p=128),
                                      dst.reg((m, h))), t_[:])
                yield

            run_pipelined(NM * 3, group, 3)

    def p3_deltanet(self):
        k, S, NT = self.k, self.S, self.NT
        with k.scope():
            UC = [self.tri("UCf", F32, 1.0, 0.0, 0, -1, 1, ALU.is_ge),
                  self.tri("UCb", F32, 1.0, 0.0, 0, 1, -1, ALU.is_ge)]
            NUC = [self.tri("NUCf", F32, -1.0, 0.0, 0, -1, 1, ALU.is_ge),
                   self.tri("NUCb", F32, -1.0, 0.0, 0, 1, -1, ALU.is_ge)]
            NM1 = [self.tri("NM1f", F32, 0.0, -BIG, 0, 1, -1, ALU.is_ge),
                   self.tri("NM1b", F32, 0.0, -BIG, 0, -1, 1, ALU.is_ge)]
            NM2 = [NM1[1], NM1[0]]
            NOTI = self.tri("NOTI", F32, 1.0, 0.0, 0, 1, -1, ALU.not_equal)
            nones_f = k.sb("nones_f", [128, 128])
            k.I("pool", "memset", ap=nones_f[:], constant=-1.0)
            gsb = k.sb("gsb3", [128, NT, 16])
            k.dma("sp", gsb[:], self.GATES[:])
            al = k.sb("alog", [128, 8]); dtb = k.sb("dtb", [128, 8]); nA = k.sb("nA", [128, 8])
            k.dma("sp", al[:], self.ab_a_log[0:1, :].broadcast_to([128, 8]))
            k.dma("sp", dtb[:], self.ab_dt_bias[0:1, :].broadcast_to([128, 8]))
            k.I("act", "activation", out=nA[:], in_=al[:], func=AF.Exp)
            k.I("dve", "tensor_scalar", out=nA[:], in0=nA[:], scalar1=-1.0, scalar2=None, op0=ALU.mult)
            xg = k.sb("xg", [128, NT, 8]); ax = k.sb("axg", [128, NT, 8]); mx = k.sb("mxg", [128, NT, 8])
            gall = k.sb("gall", [128, NT, 8])
            beta = k.sb("beta", [128, NT, 8]); nbeta = k.sb("nbeta", [128, NT, 8])
            k.I("dve", "tensor_tensor", out=xg[:], in0=gsb[:, :, 0:8], in1=dtb[:].unsqueeze(1).broadcast_to([128, NT, 8]), op=ALU.add)
            k.I("act", "activation", out=ax[:], in_=xg[:], func=AF.Abs)
            k.I("act", "activation", out=ax[:], in_=ax[:], func=AF.Exp, scale=-1.0)
            k.I("act", "activation", out=ax[:], in_=ax[:], func=AF.Ln, bias=self.one_col[:, 0:1])
            k.I("dve", "tensor_scalar", out=mx[:], in0=xg[:], scalar1=0.0, scalar2=None, op0=ALU.max)
            k.I("dve", "tensor_tensor", out=mx[:], in0=mx[:], in1=ax[:], op=ALU.add)
            k.I("dve", "tensor_tensor", out=gall[:], in0=mx[:], in1=nA[:].unsqueeze(1).broadcast_to([128, NT, 8]), op=ALU.mult)
            k.I("act", "activation", out=beta[:], in_=gsb[:, :, 8:16], func=AF.Sigmoid)
            k.I("dve", "tensor_scalar", out=nbeta[:], in0=beta[:], scalar1=-1.0, scalar2=None, op0=ALU.mult)
            gd, gc, egc, bw, kd, egl, bet, nbet = [], [], [], [], [], [], [], []
            pz = k.ps("pz", [128, 512])
            for d in range(2):
                g_ = k.sb("gd", [128, NT, 4]); gc_ = k.sb("gc", [128, NT, 4]); e_ = k.sb("egc", [128, NT, 4])
                bw_ = k.sb("bw", [128, NT, 4]); kd_ = k.sb("kd", [128, NT, 4]); gl_ = k.sb("egl", [128, NT, 4])
                b_ = k.sb("bet", [128, NT, 4]); nb_ = k.sb("nbet", [128, NT, 4])
                k.I("dve", "tensor_copy", out=g_[:], in_=gall[:, :, d * 4:(d + 1) * 4])
                k.I("dve", "tensor_copy", out=b_[:], in_=beta[:, :, d * 4:(d + 1) * 4])
                k.I("dve", "tensor_copy", out=nb_[:], in_=nbeta[:, :, d * 4:(d + 1) * 4])
                for c0 in range(0, NT, 128):
                    cw = min(128, NT - c0)
                    gv = g_[:, c0:c0 + cw, :].rearrange("p a b -> p (a b)")
                    k.mm(pz[:, 0:cw * 4], UC[d][:], gv)
                    k.I("dve", "tensor_copy", out=gc_[:, c0:c0 + cw, :].rearrange("p a b -> p (a b)"), in_=pz[:, 0:cw * 4])
                    k.mm(pz[:, 0:cw * 4], self.ones_f[:], gv)
                    k.I("dve", "tensor_copy", out=gl_[:, c0:c0 + cw, :].rearrange("p a b -> p (a b)"), in_=pz[:, 0:cw * 4])
                k.I("act", "activation", out=e_[:], in_=gc_[:], func=AF.Exp)
                k.I("dve", "tensor_tensor", out=bw_[:], in0=e_[:], in1=b_[:], op=ALU.mult)
                k.I("dve", "tensor_tensor", out=kd_[:], in0=gl_[:], in1=gc_[:], op=ALU.subtract)
                k.I("act", "activation", out=kd_[:], in_=kd_[:], func=AF.Exp)
                k.I("act", "activation", out=gl_[:], in_=gl_[:], func=AF.Exp)
                gd.append(g_); gc.append(gc_); egc.append(e_); bw.append(bw_); kd.append(kd_); egl.append(gl_)
                bet.append(b_); nbet.append(nb_)
            def mk(name, shape, dt, n=1):
                return [[k.sb(name, shape, dt) for _ in range(n)] for _ in range(2)]
            kT4 = mk("kT4", [128, 4, 128], BF16, 2); qT4 = mk("qT4", [128, 4, 128], BF16, 3)
            ktok = mk("ktok", [128, 4, 128], BF16, 2); vtok = mk("vtok", [128, 4, 128], BF16, 2)
            GB4 = mk("GB4", [128, 4, 128], F32, 2); UCg4 = mk("UCg4", [128, 4, 128], F32, 2)
            E4 = mk("E4", [128, 4, 128], F32, 2); ET4 = mk("ET4", [128, 4, 128], F32, 2)
            Mk = mk("Mk", [128, 4, 128], BF16, 4); Mtk = mk("Mtk", [128, 4, 128], BF16, 4)
            X4 = mk("X4", [128, 4, 128], BF16, 4)
            rv4 = mk("rv4", [128, 4, 128], BF16, 2); rw4 = mk("rw4", [128, 4, 128], BF16, 2); kdec4 = mk("kdec4", [128, 4, 128], BF16, 3)
            u4 = mk("u4", [128, 4, 128], F32, 3); wT4 = mk("wT4", [128, 4, 128], BF16, 3); AT4 = mk("AT4", [128, 4, 128], BF16, 3)
            vn4 = mk("vn4", [128, 4, 128], BF16)
            S32 = mk("S32", [128, 4, 128], F32); Sb = mk("Sb", [128, 4, 128], BF16)
            o2 = mk("o2", [128, 4, 128], F32); o4 = mk("o4", [128, 4, 128], F32, 2)
            banks = [k.ps("dnps", [128, 512]) for _ in range(7)]
            self._bi = 0

            def bank():
                b = banks[self._bi % len(banks)]
                self._bi += 1
                return b

            def v3(b):
                return b[:].rearrange("p (h f) -> p h f", f=128)

            def v3b(b):
                return b[:].bitcast(BF16)[:, 0:512].rearrange("p (h f) -> p h f", f=128)

            for d in range(2):
                k.I("pool", "memset", ap=S32[d][0][:], constant=0.0)
                k.I("pool", "memset", ap=Sb[d][0][:], constant=0.0)

            def bc_f(t, c):
                return t[:, c, :].unsqueeze(2).broadcast_to([128, 4, 128])

            def gen_prep(s):
                b = s % 3
                b2 = s % 2
                cs = [s, NT - 1 - s]
                for d in range(2):
                    c = cs[d]
                    sl = slice(c * 128, (c + 1) * 128)
                    k.dma("sp", kT4[d][b2][:], V(self.KAT.t.rearrange("(h q) t -> q h t", q=128)[:, :, sl], self.KAT))
                    k.dma("sp", qT4[d][b][:], V(self.QAT.t.rearrange("(h q) t -> q h t", q=128)[:, :, sl], self.QAT))
                    k.dma("act", ktok[d][b2][:].rearrange("p h f -> p (h f)"), self.KA[sl, :])
                    k.dma("act", vtok[d][b2][:].rearrange("p h f -> p (h f)"), self.VA[sl, :])
                yield
                for d in range(2):
                    c = cs[d]
                    k.I("pool", "tensor_tensor", out=GB4[d][b2][:], in0=self.ident_f[:].unsqueeze(1).broadcast_to([128, 4, 128]),
                        in1=bc_f(gc[d], c), op=ALU.mult)
                yield
                pA = [bank(), bank()]
                for d in range(2):
                    for h in range(4):
                        k.mm(v3(pA[d])[:, h, :], self.ones_f[:], GB4[d][b2][:, h, :])
                    k.I("dve", "tensor_tensor", out=UCg4[d][b2][:], in0=v3(pA[d]), in1=bc_f(gc[d], cs[d]), op=ALU.subtract)
                yield
                for d in range(2):
                    k.I("dve", "scalar_tensor_tensor", out=E4[d][b2][:], in0=UCg4[d][b2][:], scalar=-1.0,
                        in1=NM1[d][:].unsqueeze(1).broadcast_to([128, 4, 128]), op0=ALU.mult, op1=ALU.add)
                    k.I("pool", "tensor_tensor", out=ET4[d][b2][:], in0=UCg4[d][b2][:],
                        in1=NM2[d][:].unsqueeze(1).broadcast_to([128, 4, 128]), op=ALU.add)
                yield
                for d in range(2):
                    k.I("act", "activation", out=E4[d][b2][:], in_=E4[d][b2][:], func=AF.Exp)
                    k.I("act", "activation", out=ET4[d][b2][:], in_=ET4[d][b2][:], func=AF.Exp)
                yield
                for d in range(2):
                    c = cs[d]
                    kt = kT4[d][b2]; qt = qT4[d][b]
                    pg = bank()
                    for h in range(4):
                        k.mm(v3(pg)[:, h, :], kt[:, h, :], kt[:, h, :])
                    pa = bank()
                    for h in range(4):
                        k.mm(v3(pa)[:, h, :], kt[:, h, :], qt[:, h, :])
                    k.I("dve", "tensor_tensor", out=AT4[d][b][:], in0=v3(pa), in1=ET4[d][b2][:], op=ALU.mult)
                    k.I("pool", "tensor_tensor", out=E4[d][b2][:], in0=E4[d][b2][:], in1=NOTI[:].unsqueeze(1).broadcast_to([128, 4, 128]), op=ALU.mult)
                    k.I("pool", "tensor_tensor", out=E4[d][b2][:], in0=E4[d][b2][:], in1=bc_f(nbet[d], c), op=ALU.mult)
                    k.I("dve", "tensor_tensor", out=Mk[d][2 * b2][:], in0=v3(pg), in1=E4[d][b2][:], op=ALU.mult)
                    pt = bank()
                    for h in range(4):
                        k.tp(v3b(pt)[:, h, :], Mk[d][2 * b2][:, h, :], self.ident_b[:])
                    k.I("act", "activation", out=Mtk[d][2 * b2][:], in_=v3b(pt), func=AF.Copy)
                    k.I("pool", "tensor_tensor", out=X4[d][2 * b2][:], in0=Mtk[d][2 * b2][:], in1=self.ident_b[:].unsqueeze(1).broadcast_to([128, 4, 128]), op=ALU.add)
                    for h in range(4):
                        k.I("act", "activation", out=rv4[d][b2][:, h, :], in_=vtok[d][b2][:, h, :], func=AF.Copy, scale=bet[d][:, c, h:h + 1])
                        k.I("act", "activation", out=rw4[d][b2][:, h, :], in_=ktok[d][b2][:, h, :], func=AF.Copy, scale=bw[d][:, c, h:h + 1])
                        k.I("act", "activation", out=kdec4[d][b][:, h, :], in_=ktok[d][b2][:, h, :], func=AF.Copy, scale=kd[d][:, c, h:h + 1])
                yield
                cur = 0
                for lvl in range(1, 7):
                    nxt = 1 - cur
                    for d in range(2):
                        pm = bank()
                        for h in range(4):
                            k.mm(v3(pm)[:, h, :], Mtk[d][2 * b2 + cur][:, h, :], Mk[d][2 * b2 + cur][:, h, :])
                        k.I("act", "activation", out=Mk[d][2 * b2 + nxt][:], in_=v3(pm), func=AF.Copy)
                        if lvl < 6:
                            pmt = bank()
                            for h in range(4):
                                k.mm(v3(pmt)[:, h, :], Mk[d][2 * b2 + cur][:, h, :], Mtk[d][2 * b2 + cur][:, h, :])
                            k.I("dve", "tensor_copy", out=Mtk[d][2 * b2 + nxt][:], in_=v3(pmt))
                    yield
                    for d in range(2):
                        px = bank()
                        for h in range(4):
                            k.mm(v3(px)[:, h, :], Mk[d][2 * b2 + nxt][:, h, :], X4[d][2 * b2 + cur][:, h, :])
                        k.I("dve", "tensor_tensor", out=X4[d][2 * b2 + nxt][:], in0=v3(px), in1=X4[d][2 * b2 + cur][:], op=ALU.add)
                    cur = nxt
                    yield
                for d in range(2):
                    TT = X4[d][2 * b2 + cur]
                    pu = bank()
                    for h in range(4):
                        k.mm(v3(pu)[:, h, :], TT[:, h, :], rv4[d][b2][:, h, :])
                    k.I("act", "activation", out=u4[d][b][:], in_=v3(pu), func=AF.Copy)
                    pw = bank()
                    for h in range(4):
                        k.mm(v3(pw)[:, h, :], rw4[d][b2][:, h, :], TT[:, h, :])
                    k.I("dve", "tensor_copy", out=wT4[d][b][:], in_=v3(pw))
                yield

            def gen_scan(s):
                b = s % 3
                cs = [s, NT - 1 - s]
                for d in range(2):
                    pws = bank()
                    for h in range(4):
                        k.mm(v3(pws)[:, h, :], wT4[d][b][:, h, :], Sb[d][0][:, h, :])
                    k.I("dve", "tensor_tensor", out=vn4[d][0][:], in0=u4[d][b][:], in1=v3(pws), op=ALU.subtract)
                yield
                for d in range(2):
                    c = cs[d]
                    po1 = bank()
                    for h in range(4):
                        k.mm(v3(po1)[:, h, :], qT4[d][b][:, h, :], Sb[d][0][:, h, :])
                    po2 = bank()
                    for h in range(4):
                        k.mm(v3(po2)[:, h, :], AT4[d][b][:, h, :], vn4[d][0][:, h, :])
                    pds = bank()
                    for h in range(4):
                        k.mm(v3(pds)[:, h, :], kdec4[d][b][:, h, :], vn4[d][0][:, h, :])
                    k.I("pool", "tensor_tensor", out=S32[d][0][:], in0=S32[d][0][:], in1=bc_f(egl[d], c), op=ALU.mult)
                    k.I("dve", "tensor_tensor", out=Sb[d][0][:], in0=S32[d][0][:], in1=v3(pds), op=ALU.add)
                    k.I("dve", "tensor_tensor", out=S32[d][0][:], in0=S32[d][0][:], in1=v3(pds), op=ALU.add)
                    oo = o4[d][s % 2]
                    k.I("act", "activation", out=o2[d][0][:], in_=v3(po2), func=AF.Copy)
                    k.I("dve", "tensor_tensor", out=oo[:], in0=v3(po1), in1=bc_f(egc[d], c), op=ALU.mult)
                    k.I("pool", "tensor_tensor", out=oo[:], in0=oo[:], in1=o2[d][0][:], op=ALU.add)
                    dst = self.OF if d == 0 else self.OB
                    k.dma("sp", dst.reg(c)[c * 128:(c + 1) * 128, :], oo[:].rearrange("p h f -> p (h f)"))
                    yield

            def drive():
                from collections import deque
                preps = deque()
                next_p = 0
                prep_done = -1
                finished = set()
                cur = 0
                gx = None
                rnd = 0
                while cur < NT:
                    while len(preps) < 2 and next_p < NT and next_p - 3 < cur:
                        preps.append((next_p, gen_prep(next_p)))
                        next_p += 1
                    for item in list(preps):
                        sp, g = item
                        try:
                            next(g)
                        except StopIteration:
                            preps.remove(item)
                            finished.add(sp)
                    while (prep_done + 1) in finished:
                        prep_done += 1
                    if gx is None and cur <= prep_done:
                        gx = gen_scan(cur)
                    if gx is not None and rnd % 2 == 0:
                        try:
                            next(gx)
                        except StopIteration:
                            gx = None
                            cur += 1
                    rnd += 1

            drive()
        k.barrier()


    def attn_masks(self, half):
        k = self.k
        NEGM = -30000.0
        prev = self.tri("mprev", BF16, 0.0, NEGM, -(128 - half), 1, -1, ALU.is_ge)
        nxt = self.tri("mnext", BF16, 0.0, NEGM, -(128 - half), -1, 1, ALU.is_ge)
        own = None
        if half < 128:
            own = self.tri("mown", BF16, 0.0, NEGM, half, 1, -1, ALU.is_ge)
            k.I("pool", "affine_select", out=own[:], in_=own[:], pattern=[[1, 128]], compare_op=ALU.is_ge,
                fill=NEGM, base=half, channel_multiplier=-1)
        return prev, own, nxt

    def attn(self, QT, KT, VD, heads, nqc, nkc, vcols, vc0, dil, masks, OUT, qc0=0, kc0=0):
        k, S = self.k, self.S
        T = S // dil
        NA = T // 128
        W = 128 * dil
        nh = len(heads)
        nvh = vcols // 64
        mprev, mown, mnext = masks
        with k.scope():
            qG = [k.sb("qG", [128, nqc, W], BF16) for _ in range(2)]
            kG = [k.sb("kG", [128, nkc, W], BF16) for _ in range(4)]
            vG = [k.sb("vG", [128, dil, nvh, 65], BF16) for _ in range(4)]
            for v_ in vG:
                k.I("pool", "memset", ap=v_[:], constant=1.0)
            ost = [k.sb("ost", [128, dil, nh * 65]) for _ in range(2)]
            PT = [k.sb("PT", [128, 3, 128], BF16) for _ in range(3)]
            pss = [k.ps("aps", [128, 512]) for _ in range(3)]
            pso = [k.ps("apo", [128, 512]) for _ in range(2)]
            QTv = QT.t.rearrange("(c p) t -> p c t", p=128)
            KTv = KT.t.rearrange("(c p) t -> p c t", p=128)

            def load_kv(a):
                sl = slice(a * W, (a + 1) * W)
                k.dma("sp", kG[a % 4][:], V(KTv[:, kc0:kc0 + nkc, sl], KT))
                k.dma("act", vG[a % 4][:, :, :, 0:64],
                      V(VD.t[sl, vc0:vc0 + vcols].rearrange("(j r) (h d) -> j r h d", r=dil, d=64), VD))

            load_kv(0)
            units = [(a, r, hi) for a in range(NA) for r in range(dil) for hi in range(nh)]

            def unit(u):
                a, r, hi = units[u]
                qc, kc, base, vh = heads[hi]
                sl = slice(a * W, (a + 1) * W)
                q_ = qG[a % 2]
                o_ = ost[a % 2]
                if r == 0 and hi == 0:
                    if a + 1 < NA:
                        load_kv(a + 1)
                    k.dma("sp", q_[:], V(QTv[:, qc0:qc0 + nqc, sl], QT))
                kbs = [kb for kb in (a - 1, a, a + 1) if 0 <= kb < NA]
                nk = len(kbs)
                ps = pss[u % 3]; pt = PT[u % 3]
                po = pso[(u // 4) % 2]
                pv = ps[:, 0:384].rearrange("p (a b) -> p a b", b=128)
                bs = slice(base, base + 64)
                for ki, kb in enumerate(kbs):
                    m_ = mprev if kb == a - 1 else (mown if kb == a else mnext)
                    k.mm(pv[:, ki, :], kG[kb % 4][bs, kc, r:W:dil], q_[bs, qc, r:W:dil], start=True, stop=(m_ is None))
                    if m_ is not None:
                        k.mm(pv[:, ki, :], self.ident_b[:], m_[:], start=False, stop=True)
                yield
                k.I("act", "activation", out=pt[:, 0:nk, :], in_=pv[:, 0:nk, :], func=AF.Exp, scale=0.125)
                yield
                for ki, kb in enumerate(kbs):
                    k.mm(po[:, (hi % 4) * 65:(hi % 4 + 1) * 65], pt[:, ki, :], vG[kb % 4][:, r, vh, :], start=(ki == 0), stop=(ki == nk - 1))
                if hi % 4 == 3:
                    k.I("dve", "tensor_copy", out=o_[:, r, (hi - 3) * 65:(hi + 1) * 65], in_=po[:, 0:260])
                    if r == dil - 1 and hi == nh - 1:
                        k.dma("act", V(OUT.t[sl, :].rearrange("(i r) c -> i r c", r=dil), OUT.reg(a)), o_[:])
                yield

            run_pipelined(len(units), unit)

    def p4_dilated(self):
        with self.k.scope():
            masks = self.attn_masks(64)
            for gi, dil in enumerate((1, 4, 16)):
                heads = [(h // 2, h // 2, (h % 2) * 64, h) for h in range(4)]
                OUT = Buf(self.DIL.t[gi], "DIL%d" % gi)
                self.attn(self.QBT, self.KBT, self.VB, heads, 2, 2, 256, gi * 256, dil, masks, OUT, qc0=gi * 2, kc0=gi * 2)

    def ln_setup(self, g_dram, b_dram, layer):
        k = self.k
        g = k.sb("lng", [128, D]); b = k.sb("lnb", [128, D])
        k.dma("sp", g[:], g_dram[layer:layer + 1, :].broadcast_to([128, D]))
        k.dma("sp", b[:], b_dram[layer:layer + 1, :].broadcast_to([128, D]))
        return g, b

    def ln_stats(self, tin, tmp):
        k = self.k
        st = tmp["st"]; mv = tmp["mv"]; rs = tmp["rs"]; nb = tmp["nb"]
        for hh in range(2):
            k.I("dve", "bn_stats", out=st[:, hh, :], in_=tin[:, hh * 512:(hh + 1) * 512])
        k.I("dve", "bn_aggr", out=mv[:], in_=st[:].rearrange("p a b -> p (a b)"))
        k.I("act", "activation", out=rs[:], in_=mv[:, 1:2], func=AF.Ln, bias=self.eps_ln[:, 0:1])
        k.I("act", "activation", out=rs[:], in_=rs[:], func=AF.Exp, scale=-0.5)
        k.I("dve", "scalar_tensor_tensor", out=nb[:], in0=mv[:, 0:1], scalar=-1.0, in1=rs[:], op0=ALU.mult, op1=ALU.mult)

    def ln_apply(self, tin, out, g, b, tmp):
        k = self.k
        rs = tmp["rs"]; nb = tmp["nb"]
        k.I("act", "activation", out=tin[:], in_=tin[:], func=AF.Identity, scale=rs[:, 0:1], bias=nb[:, 0:1])
        k.I("pool", "tensor_tensor", out=tin[:], in0=tin[:], in1=g[:], op=ALU.mult)
        k.I("dve", "tensor_tensor", out=out, in0=tin[:], in1=b[:], op=ALU.add)

    def ln_tile(self, tin, out, g, b, tmp):
        self.ln_stats(tin, tmp)
        self.ln_apply(tin, out, g, b, tmp)

    def ln_tmp(self):
        k = self.k
        return {"st": k.sb("lnst", [128, 2, 6]), "mv": k.sb("lnmv", [128, 2]), "rs": k.sb("lnrs", [128, 1]), "nb": k.sb("lnnb", [128, 1])}

    def mixer_finish(self, layer, mod, xin, w_out_dram, nrows, build_cat, cat_stages):
        k, S, NT = self.k, self.S, self.NT
        ncc = nrows // 128
        with k.scope():
            w = k.sb("w_out", [128, ncc, D], BF16)
            self.load_w(w, w_out_dram, D, nkc=ncc)
            g, b = self.ln_setup(self.ln_mix_g, self.ln_mix_b, layer)
            NTM = 6
            tmps = [self.ln_tmp() for _ in range(NTM)]
            NCAT = cat_stages + 3
            cat = [k.sb("cat", [128, nrows], BF16) for _ in range(NCAT)]
            catT = [k.sb("catT", [128, ncc, 128], BF16) for _ in range(3)]
            xt = [k.sb("xt5", [128, D]) for _ in range(4)]
            NT1 = 8
            t1 = [k.sb("t15", [128, D]) for _ in range(NT1)]
            h2 = [k.sb("h25", [128, D], BF16) for _ in range(3)]
            h2f = [k.sb("h2f", [128, D]) for _ in range(3)]
            h2T = [k.sb("h2T", [128, 8, 128]) for _ in range(2)]
            rw = k.sb("rw", [128, 8, NE])
            k.dma("sp", rw[:], self.router_w[layer].rearrange("(kc p) e -> p kc e", p=128))
            affs = k.sb("affs", [128, NT, NE])
            lgs = [k.sb("lg", [128, NE]) for _ in range(4)]
            mxs = [k.sb("mxr", [128, 1]) for _ in range(4)]
            sms = [k.sb("smr", [128, 1]) for _ in range(4)]
            pst = [k.ps("p5t", [128, 512], BF16) for _ in range(2)]
            psy = [k.ps("p5y", [128, 512]) for _ in range(4)]
            psr = [k.ps("p5r", [128, 512]) for _ in range(2)]

            def tile(t):
                sl = slice(t * 128, (t + 1) * 128)
                c_ = cat[t % NCAT]
                yield from build_cat(t, c_)
                yield
                cT = catT[t % 3]
                ngrp = (ncc + 3) // 4
                for g0 in range(0, ncc, 4):
                    n = min(4, ncc - g0)
                    p = pst[(g0 // 4) % 2]
                    for i in range(n):
                        k.tp(p[:, i * 128:(i + 1) * 128], c_[:, (g0 + i) * 128:(g0 + i + 1) * 128], self.ident_b[:])
                    k.cp(g0 // 4, cT[:, g0:g0 + n, :], p[:, 0:n * 128].rearrange("p (a b) -> p a b", b=128))
                x_ = xt[t % 4]
                k.dma("sp", x_[:], xin[sl, :])
                yield
                t_ = t1[t % NT1]
                pp = [psy[(t * 2 + hh) % 4] for hh in range(2)]
                for hh in range(2):
                    for cc in range(ncc):
                        k.mm(pp[hh][:], cT[:, cc, :], w[:, cc, hh * 512:(hh + 1) * 512], start=(cc == 0), stop=(cc == ncc - 1))
                yield
                for hh in range(2):
                    k.I("dve", "tensor_tensor", out=t_[:, hh * 512:(hh + 1) * 512], in0=pp[hh][:], in1=mod[:, 2 * D + hh * 512:2 * D + (hh + 1) * 512], op=ALU.mult)
                k.I("dve", "scalar_tensor_tensor", out=t_[:], in0=x_[:], scalar=float(ALPHA), in1=t_[:], op0=ALU.mult, op1=ALU.add)
                tm = tmps[t % NTM]
                st = tm["st"]; mv = tm["mv"]; rs = tm["rs"]; nb = tm["nb"]
                for hh in range(2):
                    k.I("dve", "bn_stats", out=st[:, hh, :], in_=t_[:, hh * 512:(hh + 1) * 512])
                k.I("dve", "bn_aggr", out=mv[:], in_=st[:].rearrange("p a b -> p (a b)"))
                yield
                k.I("act", "activation", out=rs[:], in_=mv[:, 1:2], func=AF.Ln, bias=self.eps_ln[:, 0:1])
                k.I("act", "activation", out=rs[:], in_=rs[:], func=AF.Exp, scale=-0.5)
                yield
                k.I("dve", "scalar_tensor_tensor", out=nb[:], in0=mv[:, 0:1], scalar=-1.0, in1=rs[:], op0=ALU.mult, op1=ALU.mult)
                yield
                k.I("act", "activation", out=t_[:], in_=t_[:], func=AF.Identity, scale=rs[:, 0:1], bias=nb[:, 0:1])
                yield
                k.I("pool", "tensor_tensor", out=t_[:], in0=t_[:], in1=g[:], op=ALU.mult)
                yield
                k.I("dve", "tensor_tensor", out=t_[:], in0=t_[:], in1=b[:], op=ALU.add)
                k.dma("sp", self.X1.reg(t)[sl, :], t_[:])
                yield
                hf = h2f[t % 3]
                k.I("pool", "tensor_tensor", out=hf[:], in0=t_[:], in1=mod[:, 4 * D:5 * D], op=ALU.mult)
                yield
                k.I("dve", "tensor_tensor", out=hf[:], in0=hf[:], in1=mod[:, 3 * D:4 * D], op=ALU.add)
                yield
                k.I("act", "activation", out=h2[t % 3][:], in_=hf[:], func=AF.Copy)
                k.dma("act", self.H2.reg(t)[sl, :], h2[t % 3][:])
                hT_ = h2T[t % 2]
                for g0 in range(2):
                    p = psr[g0]
                    for i in range(4):
                        kc = g0 * 4 + i
                        k.tp(p[:, i * 128:(i + 1) * 128], hf[:, kc * 128:(kc + 1) * 128], self.ident_f[:])
                    k.cp(g0, hT_[:, g0 * 4:(g0 + 1) * 4, :], p[:].rearrange("p (a b) -> p a b", b=128))
                yield
                p = psr[t % 2]
                for kc in range(8):
                    k.mm(p[:, 0:NE], hT_[:, kc, :], rw[:, kc, :], start=(kc == 0), stop=(kc == 7))
                lg = lgs[t % 4]; mxr = mxs[t % 4]; smr = sms[t % 4]
                k.I("dve", "tensor_copy", out=lg[:], in_=p[:, 0:NE])
                k.I("dve", "tensor_reduce", out=mxr[:], in_=lg[:], axis=AX.X, op=ALU.max, negate=True)
                yield
                k.I("act", "activation", out=lg[:], in_=lg[:], func=AF.Exp, bias=mxr[:, 0:1], accum_out=smr[:])
                yield
                k.I("dve", "reciprocal", out=smr[:], in_=smr[:])
                k.I("dve", "tensor_scalar", out=affs[:, t, :], in0=lg[:], scalar1=smr[:, 0:1], scalar2=None, op0=ALU.mult)
                yield

            run_pipelined(NT, tile, cat_stages + 17)
            k.dma("sp", self.AFF[:], affs[:])

    def p5_finish0(self, mod):
        k = self.k
        with k.scope():
            dl = [k.sb("dl", [128, 3, 260]) for _ in range(4)]
            of = [k.sb("of", [128, 512]) for _ in range(7)]
            ob = [k.sb("ob5", [128, 512]) for _ in range(3)]
            zz = [k.sb("zz", [128, 512], BF16) for _ in range(7)]
            sqs = [k.sb("sq5", [128, 512]) for _ in range(3)]
            mss = [k.sb("ms5", [128, 4]) for _ in range(5)]
            rds = [k.sb("rd5", [128, 4]) for _ in range(3)]
            dnn = k.sb("dnn", [128, 128])
            k.dma("sp", dnn[:], self.ab_dn_norm[0:1, :].broadcast_to([128, 128]))

            def build_cat(t, c_):
                sl = slice(t * 128, (t + 1) * 128)
                d_ = dl[t % 4]; f_ = of[t % 7]; b_ = ob[t % 3]; z_ = zz[t % 7]
                sq = sqs[t % 3]; ms = mss[t % 5]; rd = rds[t % 3]
                k.dma("sp", d_[:], V(self.DIL.t[:, sl, :].rearrange("g p c -> p g c"), self.DIL))
                k.dma("act", f_[:], self.OF[sl, :])
                k.dma("act", b_[:], self.OB[sl, :])
                k.dma("sp", z_[:], self.Z[sl, :])
                yield
                k.I("pool", "tensor_tensor", out=d_[:, 0, :], in0=d_[:, 0, :], in1=d_[:, 1, :], op=ALU.add)
                k.I("pool", "tensor_tensor", out=d_[:, 0, :], in0=d_[:, 0, :], in1=d_[:, 2, :], op=ALU.add)
                k.I("dve", "tensor_tensor", out=f_[:], in0=f_[:], in1=b_[:], op=ALU.add)
                yield
                dv = d_[:, 0, :].rearrange("p (h e) -> p h e", e=65)
                k.I("dve", "reciprocal", out=rd[:], in_=dv[:, :, 64])
                k.I("dve", "tensor_tensor", out=c_[:, 512:768].rearrange("p (h e) -> p h e", e=64), in0=dv[:, :, 0:64],
                    in1=rd[:].unsqueeze(2).broadcast_to([128, 4, 64]), op=ALU.mult)
                k.I("pool", "tensor_tensor", out=sq[:], in0=f_[:], in1=f_[:], op=ALU.mult)
                yield
                k.I("dve", "tensor_reduce", out=ms[:], in_=sq[:].rearrange("p (h e) -> p h e", e=128), axis=AX.X, op=ALU.add)
                yield
                k.I("act", "activation", out=ms[:], in_=ms[:], func=AF.Ln, scale=1.0 / 128.0, bias=self.eps_norm[:, 0:1])
                k.I("act", "activation", out=ms[:], in_=ms[:], func=AF.Exp, scale=-0.5)
                yield
                fv = f_[:].rearrange("p (h e) -> p h e", e=128)
                k.I("dve", "tensor_tensor", out=fv, in0=fv, in1=ms[:].unsqueeze(2).broadcast_to([128, 4, 128]), op=ALU.mult)
                k.I("dve", "tensor_tensor", out=fv, in0=fv, in1=dnn[:].unsqueeze(1).broadcast_to([128, 4, 128]), op=ALU.mult)
                k.I("dve", "tensor_tensor", out=c_[:, 0:512], in0=f_[:], in1=z_[:], op=ALU.mult)

            self.mixer_finish(0, mod, self.x, self.ab_w_out, 768, build_cat, 5)

    def p5_finish1(self, mod):
        k = self.k
        with k.scope():
            at = [k.sb("at5", [128, 16, 65]) for _ in range(3)]
            sk = k.sb("sk5", [128, 16])
            dens = [k.sb("den5", [128, 16]) for _ in range(3)]
            k.dma("sp", sk[:], self.swa_sinks[0:1, :].broadcast_to([128, 16]))
            k.I("act", "activation", out=sk[:], in_=sk[:], func=AF.Exp)

            def build_cat(t, c_):
                sl = slice(t * 128, (t + 1) * 128)
                a_ = at[t % 3]; den = dens[t % 3]
                k.dma("sp", a_[:].rearrange("p h e -> p (h e)"), self.ATT[sl, :])
                yield
                k.I("dve", "tensor_tensor", out=den[:], in0=a_[:, :, 64], in1=sk[:], op=ALU.add)
                k.I("dve", "reciprocal", out=den[:], in_=den[:])
                k.I("dve", "tensor_tensor", out=c_[:].rearrange("p (h e) -> p h e", e=64), in0=a_[:, :, 0:64],
                    in1=den[:].unsqueeze(2).broadcast_to([128, 16, 64]), op=ALU.mult)

            self.mixer_finish(1, mod, self.X2, self.swa_w_out, 1024, build_cat, 1)

    def moe(self, layer, mod, OUTX):
        k, S, NT = self.k, self.S, self.NT
        cap = S // 8
        NA = cap // 128
        nc = self.nc
        with k.scope():
            idx_i = k.sb("idx_i", [128, NE, NA], I32)
            gate_s = k.sb("gate_s", [128, NE, NA])
            with k.scope():
                affs = k.sb("affs6", [128, NT, NE])
                k.dma("sp", affs[:], self.AFF[:])
                affT = k.sb("affT", [NE, S])
                scr = k.sb("scr6", [NE, S], BF16)
                pt = [k.ps("p6t", [128, 512]) for _ in range(2)]
                for t0 in range(0, NT, 4):
                    p = pt[(t0 // 4) % 2]
                    for i in range(4):
                        k.tp(p[0:NE, i * 128:(i + 1) * 128], affs[:, t0 + i, :], self.ident_f[:])
                    k.cp(t0 // 4, affT[:, t0 * 128:(t0 + 4) * 128], p[0:NE, :])
                lo = k.sb("lo6", [NE, 1]); mid = k.sb("mid6", [NE, 1]); cnt = k.sb("cnt6", [NE, 1]); ge = k.sb("ge6", [NE, 1])
                k.I("dve", "memset", ap=lo[:], constant=0.0)
                for itn in range(34):
                    dlt = 2.0 ** (-(itn + 1))
                    k.I("dve", "tensor_scalar", out=mid[:], in0=lo[:], scalar1=dlt, scalar2=None, op0=ALU.add)
                    k.I("dve", "tensor_scalar", out=scr[:], in0=affT[:], scalar1=mid[:, 0:1], scalar2=None, op0=ALU.is_gt, op1=ALU.add,
                        accum_out=cnt[:])
                    k.I("dve", "tensor_scalar", out=ge[:], in0=cnt[:], scalar1=float(cap) - 0.5, scalar2=dlt, op0=ALU.is_gt, op1=ALU.mult)
                    k.I("dve", "tensor_tensor", out=lo[:], in0=lo[:], in1=ge[:], op=ALU.add)
                maskT = k.sb("maskT", [NE, S])
                posT = affT
                k.I("dve", "tensor_scalar", out=maskT[:], in0=affT[:], scalar1=lo[:, 0:1], scalar2=None, op0=ALU.is_gt)
                k.I("dve", "tensor_tensor_scan", out=posT[:], data0=maskT[:], data1=maskT[:], initial=0.0, op0=ALU.add, op1=ALU.max)
                k.I("dve", "tensor_tensor", out=posT[:], in0=posT[:], in1=maskT[:], op=ALU.subtract)
                posk = k.sb("posk", [128, NT, NE]); mskk = k.sb("mskk", [128, NT, NE])
                for src, dst in ((posT, posk), (maskT, mskk)):
                    for t0 in range(0, NT, 32):
                        n = min(32, NT - t0)
                        p = pt[(t0 // 32) % 2]
                        for i in range(n):
                            k.tp(p[:, i * NE:(i + 1) * NE], src[:, (t0 + i) * 128:(t0 + i + 1) * 128], self.ident_f[0:NE, 0:NE])
                        k.I("dve", "tensor_copy", out=dst[:, t0:t0 + n, :].rearrange("p a b -> p (a b)"), in_=p[:, 0:n * NE])
                posi = k.sb("posi6", [128, NT, NE], I32); hii = k.sb("hii6", [128, NT, NE], I32); loi = k.sb("loi6", [128, NT, NE], I32)
                hif = k.sb("hif6", [128, NT, NE]); lof = k.sb("lof6", [128, NT, NE])
                k.I("dve", "tensor_copy", out=posi[:], in_=posk[:])
                k.I("dve", "tensor_single_scalar", out=hii[:], in_=posi[:], scalar=7, op=ALU.arith_shift_right)
                k.I("dve", "tensor_single_scalar", out=loi[:], in_=posi[:], scalar=127, op=ALU.bitwise_and)
                k.I("dve", "tensor_copy", out=hif[:], in_=hii[:])
                k.I("dve", "tensor_copy", out=lof[:], in_=loi[:])
                io128i = k.sb("io128i", [128, 128], I32); io128 = k.sb("io128", [128, 128])
                k.I("pool", "iota", out=io128i[:], pattern=[[1, 128]], base=0, channel_multiplier=0)
                k.I("dve", "tensor_copy", out=io128[:], in_=io128i[:])
                toki = k.sb("toki", [128, NT], I32); tokf = k.sb("tokf", [128, NT])
                k.I("pool", "iota", out=toki[:], pattern=[[128, NT]], base=0, channel_multiplier=1)
                k.I("dve", "tensor_copy", out=tokf[:], in_=toki[:])
                io128b = k.sb("io128b", [128, 128], BF16)
                k.I("dve", "tensor_copy", out=io128b[:], in_=io128[:])
                lofb = k.sb("lofb", [128, NT, NE], BF16)
                k.I("dve", "tensor_copy", out=lofb[:], in_=lof[:])
                thii = k.sb("thii", [128, NT], I32); tloi = k.sb("tloi", [128, NT], I32)
                thif = k.sb("thif", [128, NT]); tlof = k.sb("tlof", [128, NT])
                k.I("dve", "tensor_single_scalar", out=thii[:], in_=toki[:], scalar=6, op=ALU.arith_shift_right)
                k.I("dve", "tensor_single_scalar", out=tloi[:], in_=toki[:], scalar=63, op=ALU.bitwise_and)
                k.I("dve", "tensor_copy", out=thif[:], in_=thii[:])
                k.I("dve", "tensor_copy", out=tlof[:], in_=tloi[:])
                ghb = k.sb("ghb", [128, NT, NE], BF16); ghf = k.sb("ghf", [128, NT, NE]); glf = k.sb("glf", [128, NT, NE])
                k.I("dve", "tensor_copy", out=ghb[:], in_=affs[:])
                k.I("dve", "tensor_copy", out=ghf[:], in_=ghb[:])
                k.I("dve", "tensor_tensor", out=glf[:], in0=affs[:], in1=ghf[:], op=ALU.subtract)
                A1 = [k.sb("A61", [128, NT, NA]) for _ in range(2)]
                A2 = [k.sb("A62", [128, NT, 4 * NA], BF16) for _ in range(2)]
                TB = 8
                Bblk = [k.sb("B6", [128, TB, 128], BF16) for _ in range(3)]
                pacc = [k.ps("pacc", [128, 512]) for _ in range(2)]
                racc = k.sb("racc", [128, 4 * NA])
                bi = 0
                for e in range(NE):
                    a1 = A1[e % 2]; a2 = A2[e % 2]
                    k.I("dve", "tensor_tensor", out=a1[:], in0=io128[:, 0:NA].unsqueeze(1).broadcast_to([128, NT, NA]),
                        in1=hif[:, :, e].unsqueeze(2).broadcast_to([128, NT, NA]), op=ALU.is_equal)
                    k.I("dve", "tensor_tensor", out=a1[:], in0=a1[:], in1=mskk[:, :, e].unsqueeze(2).broadcast_to([128, NT, NA]), op=ALU.mult)
                    k.I("dve", "tensor_tensor", out=a2[:, :, 0:NA], in0=a1[:], in1=thif[:].unsqueeze(2).broadcast_to([128, NT, NA]), op=ALU.mult)
                    k.I("dve", "tensor_tensor", out=a2[:, :, NA:2 * NA], in0=a1[:], in1=tlof[:].unsqueeze(2).broadcast_to([128, NT, NA]), op=ALU.mult)
                    k.I("dve", "tensor_tensor", out=a2[:, :, 2 * NA:3 * NA], in0=a1[:], in1=ghf[:, :, e].unsqueeze(2).broadcast_to([128, NT, NA]), op=ALU.mult)
                    k.I("dve", "tensor_tensor", out=a2[:, :, 3 * NA:4 * NA], in0=a1[:], in1=glf[:, :, e].unsqueeze(2).broadcast_to([128, NT, NA]), op=ALU.mult)
                    pa = pacc[e % 2]
                    for t0 in range(0, NT, TB):
                        bb = Bblk[bi % 3]; bi += 1
                        k.I("dve", "tensor_tensor", out=bb[:], in0=io128b[:].unsqueeze(1).broadcast_to([128, TB, 128]),
                            in1=lofb[:, t0:t0 + TB, e].unsqueeze(2).broadcast_to([128, TB, 128]), op=ALU.is_equal)
                        for j in range(TB):
                            t = t0 + j
                            k.mm(pa[:, 0:4 * NA], bb[:, j, :], a2[:, t, :], start=(t == 0), stop=(t == NT - 1))
                    k.I("act", "activation", out=racc[:], in_=pa[:, 0:4 * NA], func=AF.Copy)
                    k.I("dve", "scalar_tensor_tensor", out=racc[:, 0:NA], in0=racc[:, 0:NA], scalar=64.0, in1=racc[:, NA:2 * NA], op0=ALU.mult, op1=ALU.add)
                    k.I("dve", "tensor_copy", out=idx_i[:, e, :], in_=racc[:, 0:NA])
                    k.I("dve", "tensor_tensor", out=gate_s[:, e, :], in0=racc[:, 2 * NA:3 * NA], in1=racc[:, 3 * NA:4 * NA], op=ALU.add)
                if "IDX" in self.dbg:
                    self.IDX = k.dram("IDX", [128, NE, NA], I32, kind="ExternalOutput")
                    k.dma("sp", self.IDX[:], idx_i[:])
                    self.GTS = k.dram("GTS", [128, NE, NA], F32, kind="ExternalOutput")
                    k.dma("sp", self.GTS[:], gate_s[:])
            if getattr(self, "stop", None) == "route":
                return
            with k.scope():
                zf = k.sb("zf6", [128, D])
                k.I("pool", "memset", ap=zf[:], constant=0.0)
                for t in range(NT):
                    k.dma("sp" if t % 2 == 0 else "act", self.Y.reg(t)[t * 128:(t + 1) * 128, :], zf[:])
            with k.scope():
                wg = [k.sb("wg", [128, 8, D], BF16) for _ in range(2)]
                wu = [k.sb("wu", [128, 8, D], BF16) for _ in range(2)]
                wd = [k.sb("wd", [128, 8, D], BF16) for _ in range(2)]
                st = [k.sb("wst6", [128, D]) for _ in range(4)]
                xg = [k.sb("xg6", [128, D], BF16) for _ in range(8)]
                xgT = k.sb("xgT", [128, 8, cap], BF16)
                hT = k.sb("hT6", [128, 8, cap], BF16)
                sg = [k.sb("sg6", [128, 512]) for _ in range(2)]
                yst = [k.sb("yst", [128, D]) for _ in range(2)]
                pst = [k.ps("p6x", [128, 512], BF16) for _ in range(2)]
                pgu = [k.ps("p6g", [128, 512]) for _ in range(4)]
                pdn = [k.ps("p6d", [128, 512]) for _ in range(2)]
                self._wi = 0

                def loadw1(dst, src, kc):
                    s_ = st[self._wi % len(st)]
                    k.dma("sp", s_[:], src[kc * 128:(kc + 1) * 128, :])
                    r = self._wi % 4
                    if r == 3:
                        k.I("pool", "tensor_copy", out=dst[:, kc, :], in_=s_[:])
                    elif r == 1:
                        k.I("dve", "tensor_copy", out=dst[:, kc, :], in_=s_[:])
                    else:
                        k.I("act", "activation", out=dst[:, kc, :], in_=s_[:], func=AF.Copy)
                    self._wi += 1

                def loadw_chunk(e, kc):
                    loadw1(wg[e % 2], self.moe_w_gate[layer, e], kc)
                    loadw1(wu[e % 2], self.moe_w_up[layer, e], kc)
                    loadw1(wd[e % 2], self.moe_w_down[layer, e], kc)

                for kc in range(8):
                    loadw_chunk(0, kc)
                self._ci = 0

                def gather(e, a):
                    k.dma("pool", xg[a % 8][:], self.H2[:, :], method="indirect_dma_start", out_offset=None,
                          in_offset=bass.IndirectOffsetOnAxis(ap=idx_i.t[:, e, a:a + 1], axis=0), R=[idx_i])

                def xpose(e, a):
                    x_ = xg[a % 8]
                    for g0 in range(2):
                        p = pst[g0]
                        for i in range(4):
                            kc = g0 * 4 + i
                            k.tp(p[:, i * 128:(i + 1) * 128], x_[:, kc * 128:(kc + 1) * 128], self.ident_b[:])
                        k.cp(self._ci, xgT[:, g0 * 4:(g0 + 1) * 4, a * 128:(a + 1) * 128], p[:].rearrange("p (a b) -> p a b", b=128)); self._ci += 1

                ci = 0
                for e in range(NE):
                    G, U, Dn = wg[e % 2], wu[e % 2], wd[e % 2]
                    if e == 0:
                        for a in range(NA):
                            gather(0, a); xpose(0, a)
                    if e + 1 < NE:
                        for a in range(NA):
                            gather(e + 1, a)
                    for fc in range(8):
                        for n0 in range(0, cap, 512):
                            nw = min(512, cap - n0)
                            p1 = pgu[ci % 4]; p2 = pgu[(ci + 1) % 4]; s_ = sg[(ci // 2) % 2]; ci += 2
                            for kc in range(8):
                                k.mm(p1[:, 0:nw], G[:, kc, fc * 128:(fc + 1) * 128], xgT[:, kc, n0:n0 + nw], start=(kc == 0), stop=(kc == 7))
                            for kc in range(8):
                                k.mm(p2[:, 0:nw], U[:, kc, fc * 128:(fc + 1) * 128], xgT[:, kc, n0:n0 + nw], start=(kc == 0), stop=(kc == 7))
                            k.I("act", "activation", out=s_[:, 0:nw], in_=p1[:, 0:nw], func=AF.Silu)
                            k.I("dve", "tensor_tensor", out=hT[:, fc, n0:n0 + nw], in0=s_[:, 0:nw], in1=p2[:, 0:nw], op=ALU.mult)
                        if e + 1 < NE:
                            loadw_chunk(e + 1, fc)
                    for a in range(NA):
                        if e + 1 < NE:
                            if a >= 1:
                                xpose(e + 1, a - 1)
                        y_ = yst[a % 2]
                        for hh in range(2):
                            p = pdn[hh]
                            for fc in range(8):
                                k.mm(p[:], hT[:, fc, a * 128:(a + 1) * 128], Dn[:, fc, hh * 512:(hh + 1) * 512], start=(fc == 0), stop=(fc == 7))
                            if hh == 0:
                                k.I("dve", "tensor_scalar", out=y_[:, 0:512], in0=p[:], scalar1=gate_s[:, e, a:a + 1], scalar2=None, op0=ALU.mult)
                            else:
                                k.I("act", "activation", out=y_[:, 512:1024], in_=p[:], func=AF.Copy, scale=gate_s[:, e, a:a + 1])
                        k.dma("pool", self.Y[:, :], y_[:], method="indirect_dma_start",
                              out_offset=bass.IndirectOffsetOnAxis(ap=idx_i.t[:, e, a:a + 1], axis=0), in_offset=None,
                              compute_op=ALU.add, R=[idx_i])
                    if e + 1 < NE:
                        xpose(e + 1, NA - 1)
            with k.scope():
                g, b = self.ln_setup(self.ln_ffn_g, self.ln_ffn_b, layer)
                NR = 8
                tmps = [self.ln_tmp() for _ in range(NR)]
                xt = [k.sb("xt7", [128, D]) for _ in range(4)]
                yt = [k.sb("yt7", [128, D]) for _ in range(NR)]

                def tile(t):
                    sl = slice(t * 128, (t + 1) * 128)
                    x_ = xt[t % 4]; y_ = yt[t % NR]; tm = tmps[t % NR]
                    st = tm["st"]; mv = tm["mv"]; rs = tm["rs"]; nb = tm["nb"]
                    k.dma("sp", x_[:], self.X1[sl, :])
                    k.dma("act", y_[:], self.Y[sl, :])
                    yield
                    k.I("pool", "tensor_tensor", out=y_[:], in0=y_[:], in1=mod[:, 5 * D:6 * D], op=ALU.mult)
                    yield
                    k.I("dve", "scalar_tensor_tensor", out=y_[:], in0=x_[:], scalar=float(ALPHA), in1=y_[:], op0=ALU.mult, op1=ALU.add)
                    for hh in range(2):
                        k.I("dve", "bn_stats", out=st[:, hh, :], in_=y_[:, hh * 512:(hh + 1) * 512])
                    k.I("dve", "bn_aggr", out=mv[:], in_=st[:].rearrange("p a b -> p (a b)"))
                    yield
                    k.I("act", "activation", out=rs[:], in_=mv[:, 1:2], func=AF.Ln, bias=self.eps_ln[:, 0:1])
                    k.I("act", "activation", out=rs[:], in_=rs[:], func=AF.Exp, scale=-0.5)
                    yield
                    k.I("dve", "scalar_tensor_tensor", out=nb[:], in0=mv[:, 0:1], scalar=-1.0, in1=rs[:], op0=ALU.mult, op1=ALU.mult)
                    yield
                    k.I("act", "activation", out=y_[:], in_=y_[:], func=AF.Identity, scale=rs[:, 0:1], bias=nb[:, 0:1])
                    yield
                    k.I("pool", "tensor_tensor", out=y_[:], in0=y_[:], in1=g[:], op=ALU.mult)
                    yield
                    k.I("dve", "tensor_tensor", out=y_[:], in0=y_[:], in1=b[:], op=ALU.add)
                    k.dma("sp", OUTX.reg(t)[sl, :], y_[:])
                    yield
                run_pipelined(NT, tile, 8)


    def p1_proj1(self, mod):
        k, S, NT = self.k, self.S, self.NT
        MT = 4
        MW = MT * 128
        with k.scope():
            w = k.sb("w_in1", [128, 8, 1536], BF16)
            self.load_w(w, self.swa_w_in, 1536)
            xs = [k.sb("xs1", [128, MT, D]) for _ in range(2)]
            hb = k.sb("hb1", [128, MT, D], BF16)
            hTs = [k.sb("hT1", [128, 8, MW], BF16) for _ in range(2)]
            qb = [k.sb("qb1", [128, 1536]) for _ in range(3)]
            qbb = [k.sb("qbb1", [128, 1536], BF16) for _ in range(2)]
            kdup = [k.sb("kdup", [128, 4, 2, 64], BF16) for _ in range(2)]
            qkT = [k.sb("qkT1", [128, 12, 128], BF16) for _ in range(2)]
            rts = [[k.sb("rtmp1", [128, 24, 8]) for _ in range(4)] for _ in range(2)]
            pst = [k.ps("pst1", [128, 512], BF16) for _ in range(2)]
            psm = [k.ps("psm1", [128, 512]) for _ in range(4)]
            self._ei = 0
            self._pi = 0

            def nextp():
                p = psm[self._pi % 4]; self._pi += 1
                return p

            def cpy(out, in_):
                k.cp(self._ei, out, in_); self._ei += 1

            def tile(t):
                m, j = t // MT, t % MT
                hT = hTs[m % 2]
                if j == 0:
                    x_ = xs[m % 2]
                    k.dma("sp", x_[:], self.X2[m * MW:(m + 1) * MW, :].rearrange("(j p) d -> p j d", p=128))
                    for jj in range(MT):
                        k.I("dve", "tensor_tensor", out=x_[:, jj, :], in0=x_[:, jj, :], in1=mod[:, D:2 * D], op=ALU.mult)
                        k.I("pool", "tensor_tensor", out=hb[:, jj, :], in0=x_[:, jj, :], in1=mod[:, 0:D], op=ALU.add)
                yield
                if j == 0:
                    for kc in range(8):
                        p = pst[kc % 2]
                        for jj in range(MT):
                            k.tp(p[:, jj * 128:(jj + 1) * 128], hb[:, jj, kc * 128:(kc + 1) * 128], self.ident_b[:])
                        cpy(hT[:, kc, :], p[:])
                yield
                q_ = qb[t % 3]
                for g in range(3):
                    p = nextp()
                    for kc in range(8):
                        k.mm(p[:], hT[:, kc, j * 128:(j + 1) * 128], w[:, kc, g * 512:(g + 1) * 512], start=(kc == 0), stop=(kc == 7))
                    cpy(q_[:, g * 512:(g + 1) * 512], p[:])
                yield
                self.rope(q_, 20, t, rts[t % 2])
                yield
                qq = qbb[t % 2]
                k.I("act", "activation", out=qq[:], in_=q_[:], func=AF.Copy)
                k.dma("sp", self.V1.reg(t)[t * 128:(t + 1) * 128, :], qq[:, 1280:1536])
                kd_ = kdup[t % 2]
                kv = qq[:, 1024:1280].rearrange("p (h d) -> p h d", d=64)
                k.I("pool", "tensor_copy", out=kd_[:, :, 0, :], in_=kv)
                k.I("pool", "tensor_copy", out=kd_[:, :, 1, :], in_=kv)
                yield
                qt = qkT[t % 2]
                for g3 in range(3):
                    p = pst[g3 % 2]
                    for i4 in range(4):
                        cc = g3 * 4 + i4
                        src = qq[:, cc * 128:(cc + 1) * 128] if cc < 8 else kd_[:, cc - 8, :, :].rearrange("p a b -> p (a b)")
                        k.tp(p[:, i4 * 128:(i4 + 1) * 128], src, self.ident_b[:])
                    cpy(qt[:, g3 * 4:(g3 + 1) * 4, :], p[:].rearrange("p (a b) -> p a b", b=128))
                k.dma("act", V(self.Q1T.t.rearrange("(cc p) t -> p cc t", p=128)[:, :, t * 128:(t + 1) * 128], self.Q1T.reg(t)),
                      qt[:, 0:8, :])
                k.dma("act", V(self.K1T.t.rearrange("(cc p) t -> p cc t", p=128)[:, :, t * 128:(t + 1) * 128], self.K1T.reg(t)),
                      qt[:, 8:12, :])
                yield

            run_pipelined(NT, tile)

    def p4_swa(self):
        with self.k.scope():
            masks = self.attn_masks(128)
            heads = [(h // 2, h // 4, (h % 2) * 64, h // 4) for h in range(16)]
            self.attn(self.Q1T, self.K1T, self.V1, heads, 8, 4, 256, 0, 1, masks, self.ATT)


def build(S, dbg=None, stop=None):
    P = Prog(S, dbg)
    k = P.k
    P.declare()
    with k.scope():
        P.consts()
        k.mark("P.consts()")
        P.rope_tables()
        k.mark("P.rope_tables()")
        mod = k.sb("mod", [128, 6 * D])
        P.ada(0, mod)
        k.mark("P.ada(0, mod)")
        if "MOD" in (dbg or []):
            P.MOD = k.dram("MOD", [128, 6 * D], F32, kind="ExternalOutput")
            k.dma("sp", P.MOD[:], mod[:])
        P.p1_proj0(mod)
        k.mark("P.p1_proj0(mod)")
        P.p2_dnprep()
        k.mark("P.p2_dnprep()")
        P.p3_deltanet()
        k.mark("P.p3_deltanet()")
        P.p4_dilated()
        k.mark("P.p4_dilated()")
        P.p5_finish0(mod)
        k.mark("P.p5_finish0(mod)")
        P.stop = stop
        if stop == "p5":
            k.barrier()
            return P
        P.moe(0, mod, P.X2)
        k.mark("P.moe(0, mod, P.X2)")
        if stop:
            k.barrier()
            return P
        P.ada(1, mod)
        k.mark("P.ada(1, mod)")
        P.p1_proj1(mod)
        k.mark("P.p1_proj1(mod)")
        P.p4_swa()
        k.mark("P.p4_swa()")
        P.p5_finish1(mod)
        k.mark("P.p5_finish1(mod)")
        P.moe(1, mod, P.out)
        k.mark("P.moe(1, mod, P.out)")
    return P


def host_inputs(inputs, b, S):
    f = lambda a: np.ascontiguousarray(np.asarray(a))
    m = {}
    m["x"] = f(inputs["x"][b, :S])
    m["c"] = f(np.asarray(inputs["c"][b]).reshape(8, 128).T)
    m["pos"] = f(np.asarray(inputs["positions"][b, :S]).reshape(S // 128, 128).T.astype(np.int32))
    m["ada_w"] = f(inputs["ada_w"])
    m["ada_b"] = f(inputs["ada_b"])
    m["ab_w_in"] = f(inputs["ab_w_in"][0])
    m["ab_conv_w"] = f(np.asarray(inputs["ab_conv_w"][0]).reshape(5, 12, 128).transpose(2, 1, 0))
    m["ab_a_log"] = f(np.asarray(inputs["ab_a_log"][0]).reshape(1, 8))
    m["ab_dt_bias"] = f(np.asarray(inputs["ab_dt_bias"][0]).reshape(1, 8))
    m["ab_dn_norm"] = f(np.asarray(inputs["ab_dn_norm"][0]).reshape(1, 128))
    m["ab_w_out"] = f(inputs["ab_w_out"][0])
    m["swa_w_in"] = f(inputs["swa_w_in"][0])
    m["swa_sinks"] = f(np.asarray(inputs["swa_sinks"][0]).reshape(1, 16))
    m["swa_w_out"] = f(inputs["swa_w_out"][0])
    for n in ("ln_mix_g", "ln_mix_b", "router_w", "moe_w_gate", "moe_w_up", "moe_w_down", "ln_ffn_g", "ln_ffn_b"):
        m[n] = f(inputs[n])
    return m


def kernel(**inputs):
    S = 8192
    P = build(S)
    in_maps = [host_inputs(inputs, b, S) for b in range(8)]
    res = run_bass_kernel_spmd(P.nc, in_maps, core_ids=list(range(8)))
    return np.stack([r["out"] for r in res.results], axis=0)
```
